# Optimizing a Trainium2 kernel written in Bass

```python
import math
import jax, jax.numpy as jnp
from jax import lax
import numpy as np

D_MODEL = 1024
BATCH = 8
SEQ = 2048
DEPTH = 2

HEAD_DIM = 64
ROPE_THETA = 10000.0
Q_BLOCK = 128
EPS = 1e-6
NEG = -1e30
FORCE = 1e4

NSA_HEADS = 4
NSA_CMP_LEN = 32
NSA_CMP_STRIDE = 16
NSA_SEL_LEN = 64
NSA_TOP_N = 16
NSA_WINDOW = 512

DIFF_HEADS = 4
DIFF_QK_DIM = 32
DIFF_V_DIM = 64

MLA_HEADS = 4
MLA_Q_RANK = 256
MLA_KV_RANK = 128
MLA_NOPE_DIM = 64
MLA_ROPE_DIM = 32
MLA_V_DIM = 64

SWA_HEADS = 4
SWA_KV_HEADS = 2
SWA_WINDOW = 128

MIX_WIDTH = NSA_HEADS * HEAD_DIM + DIFF_HEADS * DIFF_V_DIM + MLA_HEADS * MLA_V_DIM + SWA_HEADS * HEAD_DIM

NSA_COLS = NSA_HEADS * HEAD_DIM + 6 * HEAD_DIM + 3 * NSA_HEADS
DIFF_COLS = 2 * DIFF_HEADS * 2 * DIFF_QK_DIM + DIFF_HEADS * DIFF_V_DIM
MLA_COLS = MLA_Q_RANK + MLA_KV_RANK + MLA_ROPE_DIM
SWA_COLS = SWA_HEADS * HEAD_DIM + 2 * SWA_KV_HEADS * HEAD_DIM
IN_COLS = NSA_COLS + DIFF_COLS + MLA_COLS + SWA_COLS

PEER_HEADS = 8
PEER_N_KEYS = 128
PEER_TOPK = 16
PEER_QUERY_DIM = 256
PEER_EXPERTS = PEER_N_KEYS * PEER_N_KEYS
PEER_CHUNK = 128

kernel_name = "hybrid_nsa_diff_mla_swa_peer_adaln"


def rms_norm(x, g):
    xf = x.astype(jnp.float32)
    y = xf * lax.rsqrt(jnp.mean(xf * xf, axis=-1, keepdims=True) + EPS)
    return (y * g.astype(jnp.float32)).astype(x.dtype)


def split_cols(t, sizes):
    idx = [int(i) for i in np.cumsum(sizes)[:-1]]
    return jnp.split(t, idx, axis=-1)


def rope_tables(seq, dim):
    inv = 1.0 / (ROPE_THETA ** (jnp.arange(0, dim, 2, dtype=jnp.float32) / dim))
    ang = jnp.arange(seq, dtype=jnp.float32)[:, None] * inv[None, :]
    return jnp.cos(ang), jnp.sin(ang)


def apply_rope(x, cos, sin):
    x1, x2 = jnp.split(x, 2, axis=-1)
    c = cos[None, :, None, :].astype(x.dtype)
    s = sin[None, :, None, :].astype(x.dtype)
    return jnp.concatenate([x1 * c - x2 * s, x1 * s + x2 * c], axis=-1)


def to_blocks(t):
    b, s = t.shape[:2]
    t = t.reshape((b, s // Q_BLOCK, Q_BLOCK) + t.shape[2:])
    return jnp.moveaxis(t, 1, 0)


def from_blocks(t):
    t = jnp.moveaxis(t, 0, 1)
    return t.reshape((t.shape[0], t.shape[1] * t.shape[2]) + t.shape[3:])


def band_keys(t, window):
    s = t.shape[1]
    tp = jnp.pad(t, [(0, 0), (window, 0)] + [(0, 0)] * (t.ndim - 2))
    idx = jnp.arange(s // Q_BLOCK)[:, None] * Q_BLOCK + jnp.arange(window + Q_BLOCK)[None, :]
    return jnp.moveaxis(tp[:, idx], 1, 0)


def banded_attention(q, k, v, window, sinks=None):
    b, s, h, d = q.shape
    g = k.shape[2]
    r = h // g
    scale = d ** -0.5
    nblk = s // Q_BLOCK
    qb = to_blocks(q.reshape(b, s, g, r, d))
    kb = band_keys(k, window)
    vb = band_keys(v, window)

    def blk(args):
        qq, kk, vv, i = args
        qpos = i * Q_BLOCK + jnp.arange(Q_BLOCK)
        kpos = i * Q_BLOCK - window + jnp.arange(window + Q_BLOCK)
        dist = qpos[:, None] - kpos[None, :]
        mask = (dist >= 0) & (dist < window) & (kpos[None, :] >= 0)
        sc = jnp.einsum('bqgrd,bkgd->bgrqk', qq, kk).astype(jnp.float32) * scale
        sc = jnp.where(mask, sc, NEG)
        if sinks is None:
            p = jax.nn.softmax(sc, axis=-1)
        else:
            sk = jnp.broadcast_to(sinks.astype(jnp.float32).reshape(1, g, r, 1, 1), sc.shape[:-1] + (1,))
            p = jax.nn.softmax(jnp.concatenate([sc, sk], axis=-1), axis=-1)[..., :-1]
        return jnp.einsum('bgrqk,bkgd->bqgrd', p.astype(vv.dtype), vv)

    o = lax.map(blk, (qb, kb, vb, jnp.arange(nblk)))
    return from_blocks(o).reshape(b, s, h, d)


def nsa_mixer(cols, pos_k, pos_v, wk, wv, cos, sin):
    b, s, _ = cols.shape
    h, d = NSA_HEADS, HEAD_DIM
    q, kc, vc, ksl, vsl, kw, vw, gt = split_cols(cols, [h * d, d, d, d, d, d, d, 3 * h])
    q = apply_rope(q.reshape(b, s, h, d), cos, sin)
    kc = apply_rope(kc[:, :, None], cos, sin)[:, :, 0]
    ksl = apply_rope(ksl[:, :, None], cos, sin)[:, :, 0]
    kw = apply_rope(kw[:, :, None], cos, sin)
    gates = jax.nn.sigmoid(gt.reshape(b, s, h, 3))
    scale = d ** -0.5
    tpos = jnp.arange(s)

    n_c = (s - NSA_CMP_LEN) // NSA_CMP_STRIDE + 1
    cidx = jnp.arange(n_c)[:, None] * NSA_CMP_STRIDE + jnp.arange(NSA_CMP_LEN)[None, :]
    k_cmp = (kc[:, cidx] + pos_k).reshape(b, n_c, NSA_CMP_LEN * d) @ wk
    v_cmp = (vc[:, cidx] + pos_v).reshape(b, n_c, NSA_CMP_LEN * d) @ wv
    cmask = cidx[:, -1][None, :] <= tpos[:, None]
    sc = jnp.einsum('bqhd,bcd->bhqc', q, k_cmp).astype(jnp.float32) * scale
    p_cmp = jax.nn.softmax(jnp.where(cmask, sc, NEG), axis=-1) * cmask
    o_cmp = jnp.einsum('bhqc,bcd->bqhd', p_cmp.astype(v_cmp.dtype), v_cmp)

    n_sel = s // NSA_SEL_LEN
    top_n = min(NSA_TOP_N, n_sel)
    overlap = jax.nn.one_hot(cidx // NSA_SEL_LEN, n_sel, dtype=jnp.float32).mean(axis=1)
    imp = jnp.einsum('bhqc,cj->bqj', p_cmp, overlap)
    qblk = tpos // NSA_SEL_LEN
    jj = jnp.arange(n_sel)
    allowed = jj[None, :] <= qblk[:, None]
    forced = (jj[None, :] == 0) | (jj[None, :] == qblk[:, None]) | (jj[None, :] == qblk[:, None] - 1)
    imp = jnp.where(allowed, jnp.where(forced, FORCE, imp), NEG)
    _, sel_idx = lax.top_k(imp, top_n)
    sel_valid = sel_idx <= qblk[None, :, None]
    kb = ksl.reshape(b, n_sel, NSA_SEL_LEN, d)
    vb = vsl.reshape(b, n_sel, NSA_SEL_LEN, d)

    def sel_block(args):
        qq, ii, ok, bi = args
        qpos = bi * Q_BLOCK + jnp.arange(Q_BLOCK)
        kg = jax.vmap(lambda kk, ix: kk[ix])(kb, ii).reshape(b, Q_BLOCK, top_n * NSA_SEL_LEN, d)
        vg = jax.vmap(lambda vv, ix: vv[ix])(vb, ii).reshape(b, Q_BLOCK, top_n * NSA_SEL_LEN, d)
        kpos = (ii[..., None] * NSA_SEL_LEN + jnp.arange(NSA_SEL_LEN)).reshape(b, Q_BLOCK, -1)
        m = (kpos <= qpos[None, :, None]) & jnp.repeat(ok, NSA_SEL_LEN, axis=-1)
        ss = jnp.einsum('bqhd,bqkd->bqhk', qq, kg).astype(jnp.float32) * scale
        pp = jax.nn.softmax(jnp.where(m[:, :, None, :], ss, NEG), axis=-1)
        return jnp.einsum('bqhk,bqkd->bqhd', pp.astype(vg.dtype), vg)

    o_sel = from_blocks(lax.map(sel_block, (to_blocks(q), to_blocks(sel_idx), to_blocks(sel_valid),
                                           jnp.arange(s // Q_BLOCK))))

    o_win = banded_attention(q, kw, vw[:, :, None], NSA_WINDOW)

    o = gates[..., 0:1] * o_cmp + gates[..., 1:2] * o_sel + gates[..., 2:3] * o_win
    return o.reshape(b, s, h * d)


def diff_mixer(cols, layer, lq1, lk1, lq2, lk2, sub_g, cos, sin):
    b, s, _ = cols.shape
    h, dq, dv = DIFF_HEADS, DIFF_QK_DIM, DIFF_V_DIM
    q, k, v = split_cols(cols, [h * 2 * dq, h * 2 * dq, h * dv])
    q = apply_rope(q.reshape(b, s, h * 2, dq), cos, sin).reshape(b, s, h, 2, dq)
    k = apply_rope(k.reshape(b, s, h * 2, dq), cos, sin).reshape(b, s, h, 2, dq)
    v = v.reshape(b, s, h, dv)
    lam_init = 0.8 - 0.6 * math.exp(-0.3 * layer)
    f32 = jnp.float32
    lam = (jnp.exp(jnp.sum(lq1.astype(f32) * lk1.astype(f32)))
           - jnp.exp(jnp.sum(lq2.astype(f32) * lk2.astype(f32))) + lam_init)
    scale = dq ** -0.5
    kpos = jnp.arange(s)

    def blk(args):
        qq, i = args
        qpos = i * Q_BLOCK + jnp.arange(Q_BLOCK)
        mask = kpos[None, :] <= qpos[:, None]
        sc = jnp.einsum('bqhcd,bkhcd->bhcqk', qq, k).astype(f32) * scale
        p = jax.nn.softmax(jnp.where(mask, sc, NEG), axis=-1)
        a = p[:, :, 0] - lam * p[:, :, 1]
        return jnp.einsum('bhqk,bkhd->bqhd', a.astype(v.dtype), v)

    o = from_blocks(lax.map(blk, (to_blocks(q), jnp.arange(s // Q_BLOCK))))
    o = rms_norm(o, sub_g) * (1.0 - lam_init)
    return o.reshape(b, s, h * dv)


def mla_mixer(cols, q_norm_g, w_uq, kv_norm_g, w_ukv, cos, sin):
    b, s, _ = cols.shape
    h = MLA_HEADS
    c_q, c_kv, k_rope = split_cols(cols, [MLA_Q_RANK, MLA_KV_RANK, MLA_ROPE_DIM])
    q = (rms_norm(c_q, q_norm_g) @ w_uq).reshape(b, s, h, MLA_NOPE_DIM + MLA_ROPE_DIM)
    q_nope, q_rope = split_cols(q, [MLA_NOPE_DIM, MLA_ROPE_DIM])
    q_rope = apply_rope(q_rope, cos, sin)
    kv = (rms_norm(c_kv, kv_norm_g) @ w_ukv).reshape(b, s, h, MLA_NOPE_DIM + MLA_V_DIM)
    k_nope, v = split_cols(kv, [MLA_NOPE_DIM, MLA_V_DIM])
    k_rope = apply_rope(k_rope[:, :, None], cos, sin)[:, :, 0]
    scale = (MLA_NOPE_DIM + MLA_ROPE_DIM) ** -0.5
    kpos = jnp.arange(s)

    def blk(args):
        qn, qr, i = args
        qpos = i * Q_BLOCK + jnp.arange(Q_BLOCK)
        mask = kpos[None, :] <= qpos[:, None]
        sc = (jnp.einsum('bqhd,bkhd->bhqk', qn, k_nope)
              + jnp.einsum('bqhd,bkd->bhqk', qr, k_rope)).astype(jnp.float32) * scale
        p = jax.nn.softmax(jnp.where(mask, sc, NEG), axis=-1)
        return jnp.einsum('bhqk,bkhd->bqhd', p.astype(v.dtype), v)

    o = from_blocks(lax.map(blk, (to_blocks(q_nope), to_blocks(q_rope), jnp.arange(s // Q_BLOCK))))
    return o.reshape(b, s, h * MLA_V_DIM)


def swa_mixer(cols, sinks, cos, sin):
    b, s, _ = cols.shape
    d = HEAD_DIM
    q, k, v = split_cols(cols, [SWA_HEADS * d, SWA_KV_HEADS * d, SWA_KV_HEADS * d])
    q = apply_rope(q.reshape(b, s, SWA_HEADS, d), cos, sin)
    k = apply_rope(k.reshape(b, s, SWA_KV_HEADS, d), cos, sin)
    v = v.reshape(b, s, SWA_KV_HEADS, d)
    o = banded_attention(q, k, v, SWA_WINDOW, sinks)
    return o.reshape(b, s, SWA_HEADS * d)


def hybrid_mixer(h, layer, w_in, nsa_cmp_pos_k, nsa_cmp_pos_v, nsa_cmp_wk, nsa_cmp_wv,
                 diff_lam_q1, diff_lam_k1, diff_lam_q2, diff_lam_k2, diff_sub_g,
                 mla_q_norm_g, mla_w_uq, mla_kv_norm_g, mla_w_ukv, swa_sinks, w_out):
    s = h.shape[1]
    cols = h @ w_in
    nsa_c, diff_c, mla_c, swa_c = split_cols(cols, [NSA_COLS, DIFF_COLS, MLA_COLS, SWA_COLS])
    cos_h, sin_h = rope_tables(s, HEAD_DIM)
    cos_d, sin_d = rope_tables(s, DIFF_QK_DIM)
    cos_m, sin_m = rope_tables(s, MLA_ROPE_DIM)
    o_a = nsa_mixer(nsa_c, nsa_cmp_pos_k, nsa_cmp_pos_v, nsa_cmp_wk, nsa_cmp_wv, cos_h, sin_h)
    o_b = diff_mixer(diff_c, layer, diff_lam_q1, diff_lam_k1, diff_lam_q2, diff_lam_k2, diff_sub_g, cos_d, sin_d)
    o_c = mla_mixer(mla_c, mla_q_norm_g, mla_w_uq, mla_kv_norm_g, mla_w_ukv, cos_m, sin_m)
    o_d = swa_mixer(swa_c, swa_sinks, cos_h, sin_h)
    o = jnp.concatenate([o_a, o_b, o_c, o_d], axis=-1)
    return o @ w_out


def peer_ffn(h, w_q, sub_k1, sub_k2, u, v):
    b, s, d = h.shape
    t = b * s
    hk = PEER_HEADS * PEER_TOPK
    hf = h.reshape(t, d)
    q = (hf @ w_q).reshape(t, PEER_HEADS, 2, PEER_QUERY_DIM // 2)
    s1 = jnp.einsum('thd,kd->thk', q[:, :, 0], sub_k1).astype(jnp.float32)
    s2 = jnp.einsum('thd,kd->thk', q[:, :, 1], sub_k2).astype(jnp.float32)
    v1, i1 = lax.top_k(s1, PEER_TOPK)
    v2, i2 = lax.top_k(s2, PEER_TOPK)
    cand = (v1[..., :, None] + v2[..., None, :]).reshape(t, PEER_HEADS, PEER_TOPK * PEER_TOPK)
    cid = (i1[..., :, None] * PEER_N_KEYS + i2[..., None, :]).reshape(t, PEER_HEADS, PEER_TOPK * PEER_TOPK)
    top_s, top_j = lax.top_k(cand, PEER_TOPK)
    eid = jnp.take_along_axis(cid, top_j, axis=-1)
    gate = jax.nn.softmax(top_s, axis=-1)
    nch = t // PEER_CHUNK

    def chunk(args):
        hc, ec, gc = args
        act = jax.nn.gelu(jnp.einsum('cd,ced->ce', hc, u[ec]).astype(jnp.float32))
        w = (gc * act).astype(hc.dtype)
        return jnp.einsum('ce,ced->cd', w, v[ec])

    out = lax.map(chunk, (hf.reshape(nch, PEER_CHUNK, d), eid.reshape(nch, PEER_CHUNK, hk),
                          gate.reshape(nch, PEER_CHUNK, hk)))
    return out.reshape(b, s, d)


def setup_inputs(seed: int = 0) -> dict:
    key = jax.random.key(seed)
    ks = iter(jax.random.split(key, 40))

    def nrm(shape, scale):
        return jax.random.normal(next(ks), shape, jnp.float32) * scale

    def gain(shape):
        return 1.0 + 0.02 * jax.random.normal(next(ks), shape, jnp.float32)

    L, D = DEPTH, D_MODEL
    return {
        "x": nrm((BATCH, SEQ, D), 1.0),
        "c": nrm((BATCH, D), 1.0),
        "ada_w": nrm((L, D, 6 * D), 0.5 * D ** -0.5),
        "ada_b": nrm((L, 6 * D), 0.02),
        "norm_mix_g": gain((L, D)),
        "norm_ffn_g": gain((L, D)),
        "w_in": nrm((L, D, IN_COLS), D ** -0.5),
        "nsa_cmp_pos_k": nrm((L, NSA_CMP_LEN, HEAD_DIM), 0.1),
        "nsa_cmp_pos_v": nrm((L, NSA_CMP_LEN, HEAD_DIM), 0.1),
        "nsa_cmp_wk": nrm((L, NSA_CMP_LEN * HEAD_DIM, HEAD_DIM), (NSA_CMP_LEN * HEAD_DIM) ** -0.5),
        "nsa_cmp_wv": nrm((L, NSA_CMP_LEN * HEAD_DIM, HEAD_DIM), (NSA_CMP_LEN * HEAD_DIM) ** -0.5),
        "diff_lam_q1": nrm((L, DIFF_QK_DIM), 0.1),
        "diff_lam_k1": nrm((L, DIFF_QK_DIM), 0.1),
        "diff_lam_q2": nrm((L, DIFF_QK_DIM), 0.1),
        "diff_lam_k2": nrm((L, DIFF_QK_DIM), 0.1),
        "diff_sub_g": gain((L, DIFF_V_DIM)),
        "mla_q_norm_g": gain((L, MLA_Q_RANK)),
        "mla_w_uq": nrm((L, MLA_Q_RANK, MLA_HEADS * (MLA_NOPE_DIM + MLA_ROPE_DIM)), MLA_Q_RANK ** -0.5),
        "mla_kv_norm_g": gain((L, MLA_KV_RANK)),
        "mla_w_ukv": nrm((L, MLA_KV_RANK, MLA_HEADS * (MLA_NOPE_DIM + MLA_V_DIM)), MLA_KV_RANK ** -0.5),
        "swa_sinks": nrm((L, SWA_HEADS), 0.5),
        "w_out": nrm((L, MIX_WIDTH, D), MIX_WIDTH ** -0.5),
        "peer_w_q": nrm((L, D, PEER_HEADS * PEER_QUERY_DIM), D ** -0.5),
        "peer_sub_k1": nrm((L, PEER_N_KEYS, PEER_QUERY_DIM // 2), (PEER_QUERY_DIM // 2) ** -0.5),
        "peer_sub_k2": nrm((L, PEER_N_KEYS, PEER_QUERY_DIM // 2), (PEER_QUERY_DIM // 2) ** -0.5),
        "peer_u": nrm((L, PEER_EXPERTS, D), D ** -0.5),
        "peer_v": nrm((L, PEER_EXPERTS, D), PEER_HEADS ** -0.5),
        "final_g": gain((D,)),
    }


def reference(x, c, ada_w, ada_b, norm_mix_g, norm_ffn_g, w_in, nsa_cmp_pos_k, nsa_cmp_pos_v,
              nsa_cmp_wk, nsa_cmp_wv, diff_lam_q1, diff_lam_k1, diff_lam_q2, diff_lam_k2, diff_sub_g,
              mla_q_norm_g, mla_w_uq, mla_kv_norm_g, mla_w_ukv, swa_sinks, w_out,
              peer_w_q, peer_sub_k1, peer_sub_k2, peer_u, peer_v, final_g):
    for l in range(DEPTH):
        mod = jax.nn.silu(c) @ ada_w[l] + ada_b[l]
        sh1, sc1, g1, sh2, sc2, g2 = [m[:, None, :] for m in jnp.split(mod, 6, axis=-1)]
        h = rms_norm(x, norm_mix_g[l]) * (1.0 + sc1) + sh1
        x = x + g1 * hybrid_mixer(h, l, w_in[l], nsa_cmp_pos_k[l], nsa_cmp_pos_v[l], nsa_cmp_wk[l], nsa_cmp_wv[l],
                                  diff_lam_q1[l], diff_lam_k1[l], diff_lam_q2[l], diff_lam_k2[l], diff_sub_g[l],
                                  mla_q_norm_g[l], mla_w_uq[l], mla_kv_norm_g[l], mla_w_ukv[l], swa_sinks[l], w_out[l])
        h = rms_norm(x, norm_ffn_g[l]) * (1.0 + sc2) + sh2
        x = x + g2 * peer_ffn(h, peer_w_q[l], peer_sub_k1[l], peer_sub_k2[l], peer_u[l], peer_v[l])
    return rms_norm(x, final_g)
```

```python
import math
from contextlib import ExitStack
import numpy as np
import concourse.bass as bass
import concourse.mybir as mybir
from concourse.bass_utils import run_bass_kernel_spmd

F32 = mybir.dt.float32
BF16 = mybir.dt.bfloat16
ALU = mybir.AluOpType
AF = mybir.ActivationFunctionType
AX = mybir.AxisListType

COMPUTE = ("pe", "dve", "act", "pool")
L = 2
S = 2048
D = 1024
NT = 16
EPS = 1e-6
BIG = 1.0e4
GELU = AF.Gelu_apprx_tanh
RELAX = 1 << 30

NSA0, DIF0, MLA0, SWA0 = 0, 652, 1420, 1836


class Prog:
    def __init__(self, nc, es):
        self.nc = nc
        self.es = es
        self.engs = {"pe": nc.tensor, "dve": nc.vector, "act": nc.scalar, "pool": nc.gpsimd, "sp": nc.sync}
        self.q = {k: [] for k in self.engs}
        self.cnt = {k: 0 for k in COMPUTE}
        self.sem = {k: es.enter_context(nc.semaphore("s_" + k)) for k in COMPUTE}
        self.seen = {k: {} for k in self.engs}
        self.lastw = {}
        self.readers = {}
        self.dma_sem = {}
        self.n_inst = 0
        self.out_waits = []

    def _deps(self, reads, writes):
        deps = []
        for k in reads:
            t = self.lastw.get(k)
            if t is not None:
                deps.append(t)
        for k in writes:
            t = self.lastw.get(k)
            if t is not None:
                deps.append(t)
            deps.extend(self.readers.get(k, ()))
        return deps

    def _emit_waits(self, eng, deps, self_sync=True):
        best = {}
        for (name, sem, val) in deps:
            if name == eng and (not self_sync or self.cnt[eng] - val >= RELAX):
                continue
            if name not in best or best[name][1] < val:
                best[name] = (sem, val)
        for name, (sem, val) in best.items():
            if self.seen[eng].get(name, 0) >= val:
                continue
            self.seen[eng][name] = val
            self.q[eng].append(lambda e, sem=sem, val=val: e.wait_ge(sem, val))

    def _commit(self, tok, reads, writes):
        for k in writes:
            self.lastw[k] = tok
            self.readers[k] = []
        for k in reads:
            if k in writes:
                continue
            self.readers.setdefault(k, []).append(tok)

    def op(self, eng, fn, reads=(), writes=(), self_sync=True, sig=True):
        deps = self._deps(reads, writes)
        self._emit_waits(eng, deps, self_sync)
        sem = self.sem[eng]
        if sig:
            self.cnt[eng] += 1
            val = self.cnt[eng]
            self.q[eng].append(lambda e, fn=fn, sem=sem: fn(e).then_inc(sem, 1))
        else:
            val = self.cnt[eng] + 1
            self.q[eng].append(lambda e, fn=fn: fn(e))
        self._commit((eng, sem, val), reads, writes)
        self.n_inst += 1

    def dma(self, out, in_, reads=(), writes=(), queue="sp", is_output=False):
        deps = self._deps(reads, writes)
        self._emit_waits(queue, deps)
        key = writes[0] if writes else ("rd", reads[0])
        if key not in self.dma_sem:
            self.dma_sem[key] = [self.es.enter_context(self.nc.semaphore("d%d" % len(self.dma_sem))), 0]
        ent = self.dma_sem[key]
        ent[1] += 16
        sem, val = ent[0], ent[1]
        self.q[queue].append(lambda e, out=out, in_=in_, sem=sem: e.dma_start(out=out, in_=in_).then_inc(sem, 16))
        tok = ("dma:%s" % str(key), sem, val)
        self._commit(tok, reads, writes)
        if is_output:
            self.out_waits.append(tok)
        self.n_inst += 1

    def barrier(self):
        deps = [(k, self.sem[k], self.cnt[k]) for k in COMPUTE if self.cnt[k] > 0]
        for key, (sem, val) in self.dma_sem.items():
            if val > 0:
                deps.append(("dma:%s" % str(key), sem, val))
        for eng in self.engs:
            self._emit_waits(eng, deps, self_sync=False)
        self.lastw = {}
        self.readers = {}

    def finish(self):
        self._emit_waits("sp", self.out_waits)
        with self.nc.Block() as block:
            @block.sync
            def _(e):
                for f in self.q["sp"]:
                    f(e)

            @block.tensor
            def _(e):
                for f in self.q["pe"]:
                    f(e)

            @block.vector
            def _(e):
                for f in self.q["dve"]:
                    f(e)

            @block.scalar
            def _(e):
                for f in self.q["act"]:
                    f(e)

            @block.gpsimd
            def _(e):
                for f in self.q["pool"]:
                    f(e)


INPUT_SPECS = [
    ("x", [S, D]), ("cT", [128, 8]),
    ("ada_w", [L, D, 6 * D]), ("ada_b", [L, 6 * D]),
    ("gmix", [L, D]), ("gffn", [L, D]), ("final_g", [1, D]),
    ("w_in", [L, D, 2348]), ("w_out", [L, D, D]),
    ("wkD", [L, 2048, 128]), ("wv", [L, 2048, 64]), ("poskT", [L, 64, 32]), ("posvT", [L, 64, 32]),
    ("lam4", [L, 4, 32]), ("sub_g", [L, 64]),
    ("mla_qg", [L, 256]), ("mla_w_uq", [L, 256, 384]), ("mla_kvg", [L, 128]), ("mla_w_ukv", [L, 128, 512]),
    ("swa_sinks", [L, 4]),
    ("peer_w_q", [L, D, 2048]), ("skT", [L, 2, 128, 128]),
    ("uT", [L, D, 16384]), ("pv", [L, 16384, D]),
    ("ident", [128, 128]), ("tri", [128, 2, 128]),
    ("cs64", [S, 2, 32]), ("cs32", [S, 2, 16]),
    ("cmaskT", [128, S]), ("selM", [S, 3, 32]), ("expand", [32, S]), ("ovl1", [128, 33]),
]


def build(layers=(0, 1), dbg=None, do_mixers=(0, 1, 2, 3), do_ffn=True):
    nc = bass.Bass("TRN2", target_bir_lowering=False)
    dr = {}
    for name, shape in INPUT_SPECS:
        dr[name] = nc.dram_tensor(name, shape, F32, kind="ExternalInput").ap()
    out_d = nc.dram_tensor("out", [S, D], F32, kind="ExternalOutput").ap()
    xmid_d = nc.dram_tensor("xmid", [S, D], F32, kind="ExternalOutput").ap() if dbg == "x" else None

    with ExitStack() as es:
        P = Prog(nc, es)

        uid = [0]

        def sbuf(st, name, shape, dt):
            uid[0] += 1
            return st.enter_context(nc.sbuf_tensor("%s_s%d" % (name, uid[0]), shape, dt))

        def psum(st, name, shape, dt):
            uid[0] += 1
            return st.enter_context(nc.psum_tensor("%s_p%d" % (name, uid[0]), shape, dt))

        def psum_tb(st, name):
            t_ = psum(st, name, [128, 512], F32)
            return t_[:].bitcast(BF16).rearrange("p (k t) -> p k t", t=128)

        def mm(o, l, r, st, sp, R, W, sig=None):
            P.op("pe", lambda e: e.matmul(o, l, r, start=st, stop=sp, skip_group_check=True), reads=R, writes=W, self_sync=False,
                 sig=(sp if sig is None else sig))

        def tr(o, i, idn, R, W):
            P.op("pe", lambda e: e.transpose(o, i, idn), reads=R, writes=W, self_sync=False)

        def tt(eng, o, a, b, op, R, W):
            P.op(eng, lambda e: e.tensor_tensor(o, a, b, op), reads=R, writes=W)

        def ts(eng, o, a, s1, s2, op0, op1, R, W):
            if s2 is None:
                P.op(eng, lambda e: e.tensor_scalar(o, a, s1, None, op0), reads=R, writes=W)
            else:
                P.op(eng, lambda e: e.tensor_scalar(o, a, s1, s2, op0, op1), reads=R, writes=W)

        def stt(o, a, s, b, op0, op1, R, W):
            P.op("dve", lambda e: e.scalar_tensor_tensor(o, a, s, b, op0, op1), reads=R, writes=W)

        def act(o, i, func, R, W, bias=None, scale=None, accum=None):
            kw = {}
            if bias is not None:
                kw["bias"] = bias
            if scale is not None:
                kw["scale"] = scale
            if accum is not None:
                kw["accum_out"] = accum
            P.op("act", lambda e: e.activation(o, i, func, **kw), reads=R, writes=W)

        def cp(eng, o, i, R, W):
            if eng == "act":
                P.op("act", lambda e: e.copy(o, i), reads=R, writes=W)
            else:
                P.op(eng, lambda e: e.tensor_copy(o, i), reads=R, writes=W)

        def memset(eng, o, val, W):
            P.op(eng, lambda e: e.memset(o, val), writes=W)

        def red(o, i, op, R, W):
            P.op("dve", lambda e: e.tensor_reduce(o, i, AX.X, op), reads=R, writes=W)

        def recip(o, i, R, W):
            P.op("dve", lambda e: e.reciprocal(o, i), reads=R, writes=W)

        X = sbuf(es, "X", [128, NT, D], F32)
        MOD = sbuf(es, "MOD", [128, 3, D], F32)
        identf = sbuf(es, "identf", [128, 128], F32)
        identb = sbuf(es, "identb", [128, 128], BF16)
        tri = sbuf(es, "tri", [128, 2, 128], BF16)
        identb2 = sbuf(es, "identb2", [128, 64], BF16)
        zrow = sbuf(es, "zrow", [1, 512], BF16)
        onesb = sbuf(es, "onesb", [1, 128], BF16)
        scb = sbuf(es, "scb", [128, 8, 128], F32)
        SSQ = sbuf(es, "SSQ", [128, NT], F32)
        RSTD = sbuf(es, "RSTD", [128, NT], F32)
        epsc = sbuf(es, "epsc", [128, 1], F32)

        with ExitStack() as st0:
            trif = sbuf(st0, "trif", [128, 2, 128], F32)
            ct = sbuf(st0, "ct", [128, 8], F32)
            P.dma(identf[:], dr["ident"], writes=["identf"])
            P.dma(trif[:], dr["tri"], writes=["trif"])
            P.dma(ct[:], dr["cT"], writes=["ct"])
            for i in range(NT):
                P.dma(X[:, i, :], dr["x"][i * 128:(i + 1) * 128, :], writes=[("x", i)])
            cp("dve", identb[:], identf[:], ["identf"], ["identb"])
            cp("dve", tri[:], trif[:], ["trif"], ["tri"])
            tt("pool", identb2[:], identb[:, 0:64], identb[:, 64:128], ALU.add, ["identb"], ["identb2"])
            memset("pool", zrow[:], 0.0, ["zrow"])
            memset("pool", onesb[:], 1.0, ["onesb"])
            memset("pool", epsc[:], EPS, ["epsc"])
            act(ct[:], ct[:], AF.Silu, ["ct"], ["ct"])
            cp("dve", scb[:], ct[:].unsqueeze(2).to_broadcast([128, 8, 128]), ["ct"], ["scb"])
            P.barrier()

        def load_mod(l, half):
            with ExitStack() as st:
                AW = [sbuf(st, "AW%d" % j, [128, 8, 512], F32) for j in range(2)]
                gbc = sbuf(st, "gbc", [128, D], F32)
                pm = [psum(st, "pm%d" % j, [128, 512], F32) for j in range(2)]
                MODf = MOD[:].rearrange("p a d -> p (a d)")
                c0 = half * 3 * D
                P.dma(MODf, dr["ada_b"][l:l + 1, c0:c0 + 3 * D].to_broadcast([128, 3 * D]), writes=["mod"])
                gsrc = dr["gmix"] if half == 0 else dr["gffn"]
                P.dma(gbc[:], gsrc[l:l + 1, :].to_broadcast([128, D]), writes=["gbc"])
                for n in range(6):
                    stg = AW[n % 2]
                    P.dma(stg[:], dr["ada_w"][l][:, c0 + n * 512:c0 + (n + 1) * 512].rearrange("(k p) n -> p k n", p=128),
                          writes=[("AW", n % 2)])
                    for k in range(8):
                        mm(pm[n % 2][:], scb[:, k, :], stg[:, k, :], k == 0, k == 7, [("AW", n % 2), "scb"], [("pm", n % 2)])
                    tt("dve", MODf[:, n * 512:(n + 1) * 512], pm[n % 2][:], MODf[:, n * 512:(n + 1) * 512], ALU.add,
                       [("pm", n % 2), "mod"], ["mod"])
                stt(MOD[:, 1, :], MOD[:, 1, :], 1.0, gbc[:], ALU.add, ALU.mult, ["mod", "gbc"], ["mod"])
                P.barrier()

        def norm_to_T(i, dstT, col0, dkey, wk):
            junk, t1, hb, pT = wk
            xi = X[:, i, :]
            act(junk[:], xi, AF.Square, [("x", i)], ["junk", ("ssq", i)], accum=SSQ[:, i:i + 1])
            act(RSTD[:, i:i + 1], SSQ[:, i:i + 1], AF.Sqrt, [("ssq", i), "epsc"], [("rstd", i)], bias=epsc[:], scale=1.0 / D)
            recip(RSTD[:, i:i + 1], RSTD[:, i:i + 1], [("rstd", i)], [("rstd", i)])
            stt(t1[:], xi, RSTD[:, i:i + 1], MOD[:, 1, :], ALU.mult, ALU.mult, [("x", i), ("rstd", i), "mod"], ["t1"])
            tt("pool", hb[:], t1[:], MOD[:, 0, :], ALU.add, ["t1", "mod"], ["hb"])
            for k in range(8):
                tr(pT[:, k, :], hb[:, k * 128:(k + 1) * 128], identb[:], ["hb", "identb"], ["pT"])
            cp("act", dstT[:, :, col0:col0 + 128], pT[:], ["pT"], [dkey])

        def load_w_cols(W, wkey, src2d, ncols, stg, sname):
            j = 0
            for c0 in range(0, ncols, 256):
                n = min(256, ncols - c0)
                s_ = stg[j % len(stg)]
                sk = (sname, j % len(stg))
                P.dma(s_[:, :, 0:n], src2d[:, c0:c0 + n].rearrange("(k p) n -> p k n", p=128), writes=[sk])
                cp("act" if j % 2 == 0 else "pool", W[:, :, c0:c0 + n], s_[:, :, 0:n], [sk], [wkey])
                j += 1

        def rope(dst4, src4, G, half, cs_i, tC, tS, R, W):
            cb = cs_i[:, 0, :].unsqueeze(1).unsqueeze(1).to_broadcast([128, G, 2, half])
            sb_ = cs_i[:, 1, :].unsqueeze(1).unsqueeze(1).to_broadcast([128, G, 2, half])
            tC4 = tC[:, 0:G * 2 * half].rearrange("p (g two h) -> p g two h", two=2, h=half)
            tS4 = tS[:, 0:G * 2 * half].rearrange("p (g two h) -> p g two h", two=2, h=half)
            tt("dve", tC4, src4, cb, ALU.mult, R + ["cs"], ["tC"])
            tt("dve", tS4, src4, sb_, ALU.mult, R + ["cs"], ["tS"])
            tt("pool", dst4[:, :, 0, :], tC4[:, :, 0, :], tS4[:, :, 1, :], ALU.subtract, ["tC", "tS"], W)
            tt("pool", dst4[:, :, 1, :], tS4[:, :, 0, :], tC4[:, :, 1, :], ALU.add, ["tC", "tS"], W)

        def v4(ap, G, half):
            return ap.rearrange("p (g two h) -> p g two h", two=2, h=half)

        def flash(qc, units, kt_range, edge, scale, ACC, SC, PEX, shared_mask=None):
            qts = [4 * qc + j for j in range(4)]
            lo = min(kt_range(q)[0] for q in qts)
            hi = max(kt_range(q)[1] for q in qts)
            for u in range(len(units)):
                mm(ACC[u][:, 0:512], onesb[0:1, 0:128], zrow[0:1, 0:512], True, False, ["onesb", "zrow"], [("acc", u)])
            steps = []
            for kt in range(lo, hi + 1):
                need = [j for j, q in enumerate(qts) if kt_range(q)[0] <= kt <= kt_range(q)[1]]
                j0, j1 = need[0], need[-1]
                c0 = (4 * qc + j0) * 128
                n = (j1 - j0 + 1) * 128
                for u, un in enumerate(units):
                    steps.append((kt, u, un, need, j0, c0, n))
            smc = {}

            def emit_scores(idx):
                kt, u, un, need, j0, c0, n = steps[idx]
                if shared_mask is not None and kt not in smc:
                    smc[kt] = shared_mask(kt, c0, n, j0)
                sc = SC[idx % 2]
                sk = ("sc", idx % 2)
                np_ = len(un["parts"])
                for pi, (kf, qf) in enumerate(un["parts"]):
                    mm(sc[:, 0:n], kf(kt), qf(c0, n), pi == 0, pi == np_ - 1, un["R"], [sk])

            def emit_rest(idx):
                kt, u, un, need, j0, c0, n = steps[idx]
                sc = SC[idx % 2]
                sk = ("sc", idx % 2)
                pe = PEX[idx % len(PEX)]
                pk = ("pex", idx % len(PEX))
                act(pe[:, 0:n], sc[:, 0:n], AF.Exp, [sk], [pk], scale=scale)
                if shared_mask is not None:
                    sm = smc[kt]
                    tt("dve", pe[:, 0:n], pe[:, 0:n], sm[0], ALU.mult, [pk, sm[1]], [pk])
                else:
                    for j in need:
                        ed = edge(kt, 4 * qc + j)
                        if ed is not None:
                            sl = pe[:, (j - j0) * 128:(j - j0 + 1) * 128]
                            tt("pool", sl, sl, tri[:, ed, :], ALU.mult, [pk, "tri"], [pk])
                for j in need:
                    last = (kt == kt_range(4 * qc + j)[1])
                    mm(ACC[u][:, j * 65:(j + 1) * 65], pe[:, (j - j0) * 128:(j - j0 + 1) * 128], un["vf"](kt),
                       False, last, [pk] + un["R"], [("acc", u)])

            emit_scores(0)
            for idx in range(len(steps)):
                if idx + 1 < len(steps):
                    emit_scores(idx + 1)
                emit_rest(idx)

        def apply_wout(qc, O, okey, WO, SC, MS, wk):
            OT, tmpx = wk
            for j in range(4):
                i = 4 * qc + j
                pT = MS[j % 2]
                pk = ("ms", j % 2)
                pTf = pT[:, 0:256].rearrange("p (k t) -> p k t", k=2)
                for k in range(2):
                    tr(pTf[:, k, :], O[:, j, k * 128:(k + 1) * 128], identf[:], [okey, "identf"], [pk])
                cp("act", OT[:], pTf, [pk], ["OT"])
                for hf in range(2):
                    for k in range(2):
                        mm(SC[hf][:, 0:512], OT[:, k, :], WO[:, k, hf * 512:(hf + 1) * 512], k == 0, k == 1, ["OT", "WO"], [("sc", hf)])
                for hf in range(2):
                    tt("dve", tmpx[:, hf * 512:(hf + 1) * 512], SC[hf][:, 0:512], MOD[:, 2, hf * 512:(hf + 1) * 512], ALU.mult,
                       [("sc", hf), "mod"], ["tmpx"])
                tt("pool", X[:, i, :], X[:, i, :], tmpx[:], ALU.add, [("x", i), "tmpx"], [("x", i)])

        def load_wout(st, l, m):
            WOs = sbuf(st, "WOs", [128, 2, 512], F32)
            WO = sbuf(st, "WO", [128, 2, D], BF16)
            for hf in range(2):
                P.dma(WOs[:], dr["w_out"][l][m * 256:(m + 1) * 256, hf * 512:(hf + 1) * 512].rearrange("(k p) n -> p k n", p=128),
                      writes=["WOs"])
                cp("act", WO[:, :, hf * 512:(hf + 1) * 512], WOs[:], ["WOs"], ["WO"])
            return WO

        def causal(qt):
            return (0, qt)

        def causal_edge(kt, qt):
            return 0 if kt == qt else None

        def window(wt):
            return (lambda qt: (max(0, qt - wt), qt)), (lambda kt, qt: 0 if kt == qt else (1 if kt == qt - wt else None))

        def mixer_phase(l):
            load_mod(l, 0)
            with ExitStack() as sm:
                HT = sbuf(sm, "HT", [128, 8, S], BF16)
                CS64 = sbuf(sm, "CS64", [128, NT, 2, 32], F32)
                CS32 = sbuf(sm, "CS32", [128, NT, 2, 16], F32)
                P.dma(CS64[:], dr["cs64"].rearrange("(i p) c h -> p i c h", p=128), writes=["cs"])
                P.dma(CS32[:], dr["cs32"].rearrange("(i p) c h -> p i c h", p=128), writes=["cs"])
                with ExitStack() as st:
                    junk = sbuf(st, "junk", [128, D], BF16)
                    t1 = sbuf(st, "t1", [128, D], F32)
                    hb = sbuf(st, "hb", [128, D], BF16)
                    pT = psum_tb(st, "pT")
                    for i in range(NT):
                        norm_to_T(i, HT, i * 128, ("hT", i), (junk, t1, hb, pT))
                    P.barrier()
                if 0 in do_mixers:
                    nsa_mixer(l, HT, CS64)
                if 1 in do_mixers:
                    diff_mixer(l, HT, CS32)
                if 2 in do_mixers:
                    mla_mixer(l, HT, CS32)
                if 3 in do_mixers:
                    swa_mixer(l, HT, CS64)
                P.barrier()

        def cols_for_tile(i, HT, W, ncols, banks, bkeys):
            for b, c0 in enumerate(range(0, ncols, 512)):
                n = min(512, ncols - c0)
                for k in range(8):
                    mm(banks[b][:, 0:n], HT[:, k, i * 128:(i + 1) * 128], W[:, k, c0:c0 + n], k == 0, k == 7,
                       [("hT", i), "W"], [bkeys[b]])

        def attn_scope(st, npex=3):
            ACC = [psum(st, "ACC%d" % j, [128, 512], F32) for j in range(4)]
            SC = [psum(st, "SC%d" % j, [128, 512], F32) for j in range(2)]
            MS = [psum(st, "MS%d" % j, [128, 512], F32) for j in range(2)]
            PEX = [sbuf(st, "PEX%d" % j, [128, 512], BF16) for j in range(npex)]
            OT = sbuf(st, "OT", [128, 2, 128], BF16)
            tmpx = sbuf(st, "tmpx", [128, D], F32)
            return ACC, SC, MS, PEX, (OT, tmpx)

        def nsa_mixer(l, HT, CS64):
            with ExitStack() as sa:
                NA = sbuf(sa, "NA", [128, 6, S], BF16)
                VS = sbuf(sa, "VS", [128, NT, 2, 65], BF16)
                GT = sbuf(sa, "GT", [128, NT, 12], F32)
                memset("pool", VS[:], 1.0, ["VS"])
                with ExitStack() as st:
                    W = sbuf(st, "W", [128, 8, 768], BF16)
                    stg = [sbuf(st, "stg%d" % j, [128, 8, 256], F32) for j in range(2)]
                    tC = sbuf(st, "tC", [128, 512], F32)
                    tS = sbuf(st, "tS", [128, 512], F32)
                    R = sbuf(st, "R", [128, 768], BF16)
                    pb = [psum(st, "pb%d" % j, [128, 512], F32) for j in range(4)]
                    pT = [psum_tb(st, "pTn%d" % j)[:, 0:6, :] for j in range(2)]
                    load_w_cols(W, "W", dr["w_in"][l][:, NSA0:NSA0 + 652], 652, stg, "stg")
                    cols_for_tile(0, HT, W, 652, [pb[0], pb[1]], [("pb", 0), ("pb", 1)])
                    for i in range(NT):
                        b = [pb[(2 * i) % 4], pb[(2 * i + 1) % 4]]
                        bk = [("pb", (2 * i) % 4), ("pb", (2 * i + 1) % 4)]
                        if i + 1 < NT:
                            cols_for_tile(i + 1, HT, W, 652, [pb[(2 * i + 2) % 4], pb[(2 * i + 3) % 4]], [("pb", (2 * i + 2) % 4), ("pb", (2 * i + 3) % 4)])
                        cs = CS64[:, i, :, :]
                        rope(v4(R[:, 0:256], 4, 32), v4(b[0][:, 0:256], 4, 32), 4, 32, cs, tC, tS, [bk[0]], ["R"])
                        Rk = R[:, 256:640].rearrange("p (g r d) -> p g r d", r=2, d=64)
                        rope(Rk[:, :, 0, :].rearrange("p g (two h) -> p g two h", two=2), v4(b[0][:, 256:448], 3, 32), 3, 32, cs, tC, tS, [bk[0]], ["R"])
                        cp("pool", Rk[:, :, 1, :], Rk[:, :, 0, :], ["R"], ["R"])
                        cp("dve", R[:, 640:768].rearrange("p (r d) -> p r d", r=2), b[0][:, 448:512].unsqueeze(1).to_broadcast([128, 2, 64]), [bk[0]], ["R"])
                        cp("dve", VS[:, i, :, 0:64], b[1][:, 0:128].rearrange("p (g d) -> p g d", g=2), [bk[1]], ["VS"])
                        act(GT[:, i, :], b[1][:, 128:140], AF.Sigmoid, [bk[1]], ["GT"])
                        p_ = pT[i % 2]
                        for k in range(6):
                            tr(p_[:, k, :], R[:, k * 128:(k + 1) * 128], identb[:], ["R", "identb"], [("pTn", i % 2)])
                        cp("act", NA[:, :, i * 128:(i + 1) * 128], p_[:], [("pTn", i % 2)], [("na", i)])
                    P.barrier()
                NAR = [("na", i) for i in range(NT)]
                with ExitStack() as st:
                    ACC, SC, MS, PEX, wk = attn_scope(st)
                    WO = load_wout(st, l, 0)
                    KCMP = sbuf(st, "KCMP", [128, 128], BF16)
                    VC1 = sbuf(st, "VC1", [128, 97], BF16)
                    CM = sbuf(st, "CM", [128, S], BF16)
                    SELM = sbuf(st, "SELM", [128, NT, 3, 32], F32)
                    EXP = sbuf(st, "EXP", [32, S], BF16)
                    selT = sbuf(st, "selT", [32, 512], BF16)
                    P.dma(SELM[:], dr["selM"].rearrange("(i p) c j -> p i c j", p=128), writes=["SELM"])
                    with ExitStack() as sp_:
                        wstg = sbuf(sp_, "wstg", [64, 8, 128], F32)
                        WKb = sbuf(sp_, "WKb", [64, 32, 128], BF16)
                        WVb = sbuf(sp_, "WVb", [64, 32, 64], BF16)
                        posf = sbuf(sp_, "posf", [64, 2, 32], F32)
                        posb = sbuf(sp_, "posb", [64, 2, 32], BF16)
                        ovf = sbuf(sp_, "ovf", [128, 33], F32)
                        kb = sbuf(sp_, "kb", [128, 1], F32)
                        vb = sbuf(sp_, "vb", [1, 64], BF16)
                        CMf = sbuf(sp_, "CMf", [128, 512], F32)
                        EXs = sbuf(sp_, "EXs", [32, 512], F32)
                        wk3 = dr["wkD"][l].rearrange("(j d) o -> d j o", d=64)
                        wv3 = dr["wv"][l].rearrange("(j d) o -> d j o", d=64)
                        for c in range(4):
                            P.dma(wstg[:], wk3[:, 8 * c:8 * c + 8, :], writes=["wstg"])
                            cp("act", WKb[:, 8 * c:8 * c + 8, :], wstg[:], ["wstg"], ["WKb"])
                        for c in range(4):
                            P.dma(wstg[:, :, 0:64], wv3[:, 8 * c:8 * c + 8, :], writes=["wstg"])
                            cp("act", WVb[:, 8 * c:8 * c + 8, :], wstg[:, :, 0:64], ["wstg"], ["WVb"])
                        P.dma(posf[:, 0, :], dr["poskT"][l], writes=["posf"])
                        P.dma(posf[:, 1, :], dr["posvT"][l], writes=["posf"])
                        P.dma(ovf[:], dr["ovl1"], writes=["ovf"])
                        for c in range(4):
                            P.dma(CMf[:], dr["cmaskT"][:, c * 512:(c + 1) * 512], writes=["CMf"])
                            cp("pool", CM[:, c * 512:(c + 1) * 512], CMf[:], ["CMf"], ["CM"])
                            P.dma(EXs[:], dr["expand"][:, c * 512:(c + 1) * 512], writes=["EXs"])
                            cp("pool", EXP[:, c * 512:(c + 1) * 512], EXs[:], ["EXs"], ["EXP"])
                        cp("dve", posb[:], posf[:], ["posf"], ["posb"])
                        memset("pool", VC1[:], 0.0, ["VC1"])
                        memset("pool", KCMP[:], 0.0, ["KCMP"])
                        cp("dve", VC1[:, 64:97], ovf[:], ["ovf", "VC1"], ["VC1"])
                        for j in range(32):
                            mm(MS[0][:, 0:1], WKb[0:64, j, :], posb[0:64, 0, j:j + 1], j == 0, j == 31, ["WKb", "posb"], [("ms", 0)])
                        cp("dve", kb[:], MS[0][:, 0:1], [("ms", 0)], ["kb"])
                        for j in range(32):
                            mm(MS[1][0:1, 0:64], posb[0:64, 1, j:j + 1], WVb[0:64, j, :], j == 0, j == 31, ["WVb", "posb"], [("ms", 1)])
                        cp("dve", vb[:], MS[1][0:1, 0:64], [("ms", 1)], ["vb"])
                        for j in range(32):
                            mm(SC[0][:, 0:127], WKb[0:64, j, :], NA[0:64, 2, j:j + 16 * 126 + 1:16], j == 0, j == 31, ["WKb"] + NAR, [("sc", 0)])
                        ts("dve", KCMP[:, 0:127], SC[0][:, 0:127], kb[:, 0:1], None, ALU.add, None, [("sc", 0), "kb", "KCMP"], ["KCMP"])
                        for j in range(32):
                            mm(SC[1][0:127, 0:64], NA[0:64, 5, j:j + 16 * 126 + 1:16], WVb[0:64, j, :], j == 0, False, ["WVb"] + NAR, [("sc", 1)])
                        mm(SC[1][0:127, 0:64], onesb[0:1, 0:127], vb[0:1, :], False, True, ["onesb", "vb"], [("sc", 1)])
                        cp("dve", VC1[0:127, 0:64], SC[1][0:127, 0:64], [("sc", 1), "VC1"], ["VC1"])
                        P.barrier()
                    MK = [sbuf(st, "MK%d" % j, [128, 512], BF16) for j in range(2)]
                    O = sbuf(st, "O", [128, 4, 256], F32)
                    sm_ = sbuf(st, "nsa_small", [128, 256], F32)
                    impw = sbuf(st, "impw", [128, 4, 32], F32)
                    imr = sbuf(st, "imr", [128, 32], F32)
                    selb = sbuf(st, "selb", [128, 32], BF16)
                    PC = [sbuf(st, "PC%d" % j, [128, 512], BF16) for j in range(4)]
                    scale = 64 ** -0.5
                    for qc in range(4):
                        q0 = qc * 512
                        pcs = []
                        for h in range(4):
                            hp, blk = 64 * (h % 2), h // 2
                            sc = SC[h % 2]
                            mm(sc[0:127, :], KCMP[hp:hp + 64, 0:127], NA[hp:hp + 64, blk, q0:q0 + 512], True, True, ["KCMP"] + NAR, [("sc", h % 2)])
                            pc = PC[h]
                            act(pc[0:127, :], sc[0:127, :], AF.Exp, [("sc", h % 2)], [("pc", h)], scale=scale)
                            tt("dve", pc[0:127, :], pc[0:127, :], CM[0:127, q0:q0 + 512], ALU.mult, [("pc", h), "CM"], [("pc", h)])
                            pcs.append(pc)
                        for j in range(4):
                            i = 4 * qc + j
                            psc = MS[j % 2]
                            pck = ("ms", j % 2)
                            ps3 = psc[:, 0:388].rearrange("p (h c) -> p h c", h=4)
                            for h in range(4):
                                mm(ps3[:, h, :], pcs[h][0:127, j * 128:(j + 1) * 128], VC1[0:127, :], True, True, [("pc", h), "VC1"], [pck])
                            den = sm_[:, 0:4]
                            g1 = sm_[:, 4:8]
                            ts("dve", den, ps3[:, :, 64], 1e-30, None, ALU.max, None, [pck], ["nsm"])
                            recip(den, den, ["nsm"], ["nsm"])
                            tt("dve", g1, den, GT[:, i, 0:12:3], ALU.mult, ["nsm", "GT"], ["nsm"])
                            tt("dve", O[:, j, :].rearrange("p (h d) -> p h d", h=4), ps3[:, :, 0:64],
                               g1.unsqueeze(2).to_broadcast([128, 4, 64]), ALU.mult, [pck, "nsm"], ["O"])
                            tt("dve", impw[:], ps3[:, :, 65:97], den.unsqueeze(2).to_broadcast([128, 4, 32]), ALU.mult, [pck, "nsm"], ["impw"])
                            red(imr[:], impw[:].rearrange("p h j -> p j h"), ALU.add, ["impw"], ["imr"])
                            tt("dve", imr[:], imr[:], SELM[:, i, 0, :], ALU.mult, ["imr", "SELM"], ["imr"])
                            tt("dve", imr[:], imr[:], SELM[:, i, 1, :], ALU.add, ["imr", "SELM"], ["imr"])
                            m8 = sm_[:, 8:24]
                            imr2 = sm_[:, 32:64]
                            P.op("dve", lambda e, m8=m8: e.max(m8[:, 0:8], imr[:]), reads=["imr"], writes=["nsm"])
                            P.op("dve", lambda e, m8=m8, imr2=imr2: e.match_replace(imr2, m8[:, 0:8], imr[:], -3.0e38), reads=["imr", "nsm"], writes=["nsm2"])
                            P.op("dve", lambda e, m8=m8, imr2=imr2: e.max(m8[:, 8:16], imr2), reads=["nsm2"], writes=["nsm"])
                            stt(selb[:], imr[:], m8[:, 15:16], SELM[:, i, 2, :], ALU.is_ge, ALU.mult, ["imr", "nsm", "SELM"], ["selb"])
                            pst = MS[j % 2][:, 256:384].bitcast(BF16)
                            tr(pst[0:32, 0:128], selb[:], identb[:], ["selb", "identb"], [pck])
                            cp("act", selT[:, j * 128:(j + 1) * 128], pst[0:32, 0:128], [pck], ["selT"])

                        def smask(kt, c0, n, j0, qc=qc, q0=q0):
                            mk = MK[kt % 2]
                            mkk = ("mk", kt % 2)
                            pm_ = MS[kt % 2]
                            mm(pm_[:, 0:n], EXP[0:32, kt * 128:(kt + 1) * 128], selT[0:32, c0 - q0:c0 - q0 + n], True, True, ["EXP", "selT"], [("ms", kt % 2)])
                            cp("act", mk[:, 0:n], pm_[:, 0:n], [("ms", kt % 2)], [mkk])
                            if kt >= 4 * qc:
                                tt("pool", mk[:, 0:128], mk[:, 0:128], tri[:, 0, :], ALU.mult, [mkk, "tri"], [mkk])
                            return (mk[:, 0:n], mkk)

                        units = []
                        for h in range(4):
                            hp, blk = 64 * (h % 2), h // 2
                            units.append(dict(
                                parts=[((lambda kt, hp=hp: NA[hp:hp + 64, 3, kt * 128:(kt + 1) * 128]),
                                        (lambda c0, n, hp=hp, blk=blk: NA[hp:hp + 64, blk, c0:c0 + n]))],
                                vf=(lambda kt: VS[:, kt, 0, :]), R=NAR + ["VS"]))
                        flash(qc, units, causal, causal_edge, scale, ACC, SC, PEX, shared_mask=smask)
                        nsa_fin(qc, ACC, GT, O, sm_, 1)
                        kr, ed = window(4)
                        units = []
                        for h in range(4):
                            hp, blk = 64 * (h % 2), h // 2
                            units.append(dict(
                                parts=[((lambda kt, hp=hp: NA[hp:hp + 64, 4, kt * 128:(kt + 1) * 128]),
                                        (lambda c0, n, hp=hp, blk=blk: NA[hp:hp + 64, blk, c0:c0 + n]))],
                                vf=(lambda kt: VS[:, kt, 1, :]), R=NAR + ["VS"]))
                        flash(qc, units, kr, ed, scale, ACC, SC, PEX)
                        nsa_fin(qc, ACC, GT, O, sm_, 2)
                        if dbg == ("o", 0):
                            for j in range(4):
                                P.dma(out_d[(4 * qc + j) * 128:(4 * qc + j + 1) * 128, 0:256], O[:, j, :], reads=["O"], is_output=True)
                        apply_wout(qc, O, "O", WO, SC, MS, wk)
                    P.barrier()

        def nsa_fin(qc, ACC, GT, O, sm_, gi):
            for h in range(4):
                a3 = ACC[h][:, 0:260].rearrange("p (j c) -> p j c", j=4)
                rd = sm_[:, 64 + 4 * h:68 + 4 * h]
                recip(rd, a3[:, :, 64], [("acc", h)], [("rd", h)])
                tt("dve", rd, rd, GT[:, 4 * qc:4 * qc + 4, 3 * h + gi], ALU.mult, [("rd", h), "GT"], [("rd", h)])
                for j in range(4):
                    stt(O[:, j, h * 64:(h + 1) * 64], a3[:, j, 0:64], rd[:, j:j + 1], O[:, j, h * 64:(h + 1) * 64], ALU.mult, ALU.add,
                        [("acc", h), ("rd", h), "O"], ["O"])

        def diff_mixer(l, HT, CS32):
            lam_init = 0.8 - 0.6 * math.exp(-0.3 * l)
            with ExitStack() as sa:
                DA = sbuf(sa, "DA", [128, 6, S], BF16)
                VD = sbuf(sa, "VD", [128, NT, 4, 65], BF16)
                memset("pool", VD[:], 1.0, ["VD"])
                with ExitStack() as st:
                    W = sbuf(st, "W", [128, 8, 768], BF16)
                    stg = [sbuf(st, "stg%d" % j, [128, 8, 256], F32) for j in range(2)]
                    tC = sbuf(st, "tC", [128, 512], F32)
                    tS = sbuf(st, "tS", [128, 512], F32)
                    R = sbuf(st, "R", [128, 768], BF16)
                    RT = sbuf(st, "RT", [128, 512], BF16)
                    memset("pool", R[:], 0.0, ["R"])
                    pb = [psum(st, "pb%d" % j, [128, 512], F32) for j in range(4)]
                    pT = [psum_tb(st, "pTn%d" % j)[:, 0:6, :] for j in range(2)]
                    load_w_cols(W, "W", dr["w_in"][l][:, DIF0:DIF0 + 768], 768, stg, "stg")
                    cols_for_tile(0, HT, W, 768, [pb[0], pb[1]], [("pb", 0), ("pb", 1)])
                    for i in range(NT):
                        b = [pb[(2 * i) % 4], pb[(2 * i + 1) % 4]]
                        bk = [("pb", (2 * i) % 4), ("pb", (2 * i + 1) % 4)]
                        if i + 1 < NT:
                            cols_for_tile(i + 1, HT, W, 768, [pb[(2 * i + 2) % 4], pb[(2 * i + 3) % 4]], [("pb", (2 * i + 2) % 4), ("pb", (2 * i + 3) % 4)])
                        rope(v4(RT[:, 0:512], 16, 16), v4(b[0][:, 0:512], 16, 16), 16, 16, CS32[:, i, :, :], tC, tS, [bk[0]], ["RT"])
                        for side in range(2):
                            for gb_ in range(3):
                                nu = 3 if gb_ < 2 else 2
                                cp("pool" if gb_ % 2 == 0 else "act", R[:, (3 * side + gb_) * 128:(3 * side + gb_) * 128 + 32 * nu],
                                   RT[:, side * 256 + gb_ * 96:side * 256 + gb_ * 96 + 32 * nu], ["RT"], ["R"])
                        cp("dve", VD[:, i, :, 0:64], b[1][:, 0:256].rearrange("p (g d) -> p g d", g=4), [bk[1]], ["VD"])
                        p_ = pT[i % 2]
                        for k in range(6):
                            tr(p_[:, k, :], R[:, k * 128:(k + 1) * 128], identb[:], ["R", "identb"], [("pTn", i % 2)])
                        cp("act", DA[:, :, i * 128:(i + 1) * 128], p_[:], [("pTn", i % 2)], [("da", i)])
                    P.barrier()
                DAR = [("da", i) for i in range(NT)]
                with ExitStack() as st:
                    ACC, SC, MS, PEX, wk = attn_scope(st)
                    WO = load_wout(st, l, 1)
                    O = sbuf(st, "O", [128, 4, 256], F32)
                    O2 = sbuf(st, "O2", [128, 4, 256], F32)
                    lamt = sbuf(st, "lamt", [128, 4, 32], F32)
                    sgb = sbuf(st, "sgb", [128, 64], F32)
                    sm_ = sbuf(st, "dsm", [128, 64], F32)
                    P.dma(lamt[:].rearrange("p a b -> p (a b)"), dr["lam4"][l:l + 1].rearrange("o a b -> o (a b)").to_broadcast([128, 128]), writes=["lamt"])
                    P.dma(sgb[:], dr["sub_g"][l:l + 1, :].to_broadcast([128, 64]), writes=["sgb"])
                    tt("dve", lamt[:, 0, :], lamt[:, 0, :], lamt[:, 1, :], ALU.mult, ["lamt"], ["lamt"])
                    tt("dve", lamt[:, 2, :], lamt[:, 2, :], lamt[:, 3, :], ALU.mult, ["lamt"], ["lamt"])
                    red(sm_[:, 0:1], lamt[:, 0, :], ALU.add, ["lamt"], ["dl"])
                    red(sm_[:, 1:2], lamt[:, 2, :], ALU.add, ["lamt"], ["dl"])
                    act(sm_[:, 0:2], sm_[:, 0:2], AF.Exp, ["dl"], ["dl"])
                    stt(sm_[:, 0:1], sm_[:, 1:2], -lam_init, sm_[:, 0:1], ALU.add, ALU.subtract, ["dl"], ["dl"])
                    ts("dve", sgb[:], sgb[:], 1.0 - lam_init, None, ALU.mult, None, ["sgb"], ["sgb"])
                    scale = 32 ** -0.5
                    for qc in range(4):
                        for grp in range(2):
                            units = []
                            for uu in range(4):
                                u = grp * 4 + uu
                                blk, po = u // 3, 32 * (u % 3)
                                h = u // 2
                                units.append(dict(
                                    parts=[((lambda kt, po=po, blk=blk: DA[po:po + 32, 3 + blk, kt * 128:(kt + 1) * 128]),
                                            (lambda c0, n, po=po, blk=blk: DA[po:po + 32, blk, c0:c0 + n]))],
                                    vf=(lambda kt, h=h: VD[:, kt, h, :]), R=DAR + ["VD"]))
                            flash(qc, units, causal, causal_edge, scale, ACC, SC, PEX)
                            for uu in range(4):
                                u = grp * 4 + uu
                                h, c = u // 2, u % 2
                                a3 = ACC[uu][:, 0:260].rearrange("p (j c) -> p j c", j=4)
                                rd = sm_[:, 8 + 4 * uu:12 + 4 * uu]
                                recip(rd, a3[:, :, 64], [("acc", uu)], [("rd", uu)])
                                if c == 1:
                                    ts("dve", rd, rd, sm_[:, 0:1], None, ALU.mult, None, [("rd", uu), "dl"], [("rd", uu)])
                                dst = (O if c == 0 else O2)[:, :, h * 64:(h + 1) * 64]
                                tt("dve", dst, a3[:, :, 0:64], rd.unsqueeze(2).to_broadcast([128, 4, 64]), ALU.mult,
                                   [("acc", uu), ("rd", uu)], ["O" if c == 0 else "O2"])
                        tt("pool", O[:], O[:], O2[:], ALU.add, ["O", "O2"], ["O"])
                        tt("pool", O2[:], O[:], O[:], ALU.mult, ["O"], ["O2"])
                        ss16 = sm_[:, 32:48]
                        red(ss16, O2[:].rearrange("p j (h d) -> p (j h) d", h=4), ALU.add, ["O2"], ["ss16"])
                        act(ss16, ss16, AF.Sqrt, ["ss16", "epsc"], ["ss16"], bias=epsc[:], scale=1.0 / 64)
                        recip(ss16, ss16, ["ss16"], ["ss16"])
                        O3 = O[:].rearrange("p j (h d) -> p (j h) d", h=4)
                        tt("dve", O3, O3, ss16.unsqueeze(2).to_broadcast([128, 16, 64]), ALU.mult, ["O", "ss16"], ["O"])
                        tt("dve", O3, O3, sgb[:].unsqueeze(1).to_broadcast([128, 16, 64]), ALU.mult, ["O", "sgb"], ["O"])
                        if dbg == ("o", 1):
                            for j in range(4):
                                P.dma(out_d[(4 * qc + j) * 128:(4 * qc + j + 1) * 128, 0:256], O[:, j, :], reads=["O"], is_output=True)
                        apply_wout(qc, O, "O", WO, SC, MS, wk)
                    P.barrier()

        def mla_mixer(l, HT, CS32):
            with ExitStack() as sa:
                MA = sbuf(sa, "MA", [128, 7, S], BF16)
                VM = sbuf(sa, "VM", [128, NT, 4, 65], BF16)
                memset("pool", VM[:], 1.0, ["VM"])
                with ExitStack() as st:
                    W = sbuf(st, "W", [128, 8, 416], BF16)
                    stg = [sbuf(st, "stg%d" % j, [128, 8, 256], F32) for j in range(2)]
                    tC = sbuf(st, "tC", [128, 512], F32)
                    tS = sbuf(st, "tS", [128, 512], F32)
                    R = sbuf(st, "R", [128, 896], BF16)
                    memset("pool", R[:], 0.0, ["R"])
                    WUQ = sbuf(st, "WUQ", [128, 2, 384], BF16)
                    WUKV = sbuf(st, "WUKV", [128, 512], BF16)
                    wst = sbuf(st, "wst", [128, 2, 512], F32)
                    qgb = sbuf(st, "qgb", [128, 384], F32)
                    cqn = sbuf(st, "cqn", [128, 384], BF16)
                    CT = sbuf(st, "CT", [128, 3, 128], BF16)
                    junk = sbuf(st, "junk", [128, 256], BF16)
                    kr = sbuf(st, "kr", [128, 32], BF16)
                    msm = sbuf(st, "msm", [128, 8], F32)
                    pb = [psum(st, "pb%d" % j, [128, 512], F32) for j in range(2)]
                    pu = [psum(st, "pu%d" % j, [128, 512], F32) for j in range(4)]
                    pT = [psum_tb(st, "pTn%d" % j)[:, 0:7, :] for j in range(2)]
                    load_w_cols(W, "W", dr["w_in"][l][:, MLA0:MLA0 + 416], 416, stg, "stg")
                    P.dma(wst[:, :, 0:384], dr["mla_w_uq"][l].rearrange("(k p) n -> p k n", p=128), writes=["wst"])
                    cp("act", WUQ[:], wst[:, :, 0:384], ["wst"], ["WUQ"])
                    P.dma(wst[:, 0, :], dr["mla_w_ukv"][l], writes=["wst"])
                    cp("act", WUKV[:], wst[:, 0, :], ["wst"], ["WUKV"])
                    P.dma(qgb[:, 0:256], dr["mla_qg"][l:l + 1, :].to_broadcast([128, 256]), writes=["qgb"])
                    P.dma(qgb[:, 256:384], dr["mla_kvg"][l:l + 1, :].to_broadcast([128, 128]), writes=["qgb"])
                    cols_for_tile(0, HT, W, 416, [pb[0]], [("pb", 0)])
                    for i in range(NT):
                        b = pb[i % 2]
                        bk = ("pb", i % 2)
                        if i + 1 < NT:
                            cols_for_tile(i + 1, HT, W, 416, [pb[(i + 1) % 2]], [("pb", (i + 1) % 2)])
                        act(junk[:, 0:256], b[:, 0:256], AF.Square, [bk], ["junk", "msm"], accum=msm[:, 0:1])
                        act(junk[:, 0:128], b[:, 256:384], AF.Square, [bk], ["junk", "msm"], accum=msm[:, 1:2])
                        act(msm[:, 0:1], msm[:, 0:1], AF.Sqrt, ["msm", "epsc"], ["msm"], bias=epsc[:], scale=1.0 / 256)
                        act(msm[:, 1:2], msm[:, 1:2], AF.Sqrt, ["msm", "epsc"], ["msm"], bias=epsc[:], scale=1.0 / 128)
                        recip(msm[:, 0:2], msm[:, 0:2], ["msm"], ["msm"])
                        stt(cqn[:, 0:256], b[:, 0:256], msm[:, 0:1], qgb[:, 0:256], ALU.mult, ALU.mult, [bk, "msm", "qgb"], ["cqn"])
                        stt(cqn[:, 256:384], b[:, 256:384], msm[:, 1:2], qgb[:, 256:384], ALU.mult, ALU.mult, [bk, "msm", "qgb"], ["cqn"])
                        p_ = pT[i % 2]
                        pk = ("pTn", i % 2)
                        for k in range(3):
                            tr(p_[:, k, :], cqn[:, k * 128:(k + 1) * 128], identb[:], ["cqn", "identb"], [pk])
                        cp("act", CT[:], p_[:, 0:3, :], [pk], ["CT"])
                        pq = pu[(2 * i) % 4]
                        pkv = pu[(2 * i + 1) % 4]
                        pqk, pkvk = ("pu", (2 * i) % 4), ("pu", (2 * i + 1) % 4)
                        mm(pq[:, 0:384], CT[:, 0, :], WUQ[:, 0, :], True, False, ["CT", "WUQ"], [pqk])
                        mm(pq[:, 0:384], CT[:, 1, :], WUQ[:, 1, :], False, True, ["CT", "WUQ"], [pqk])
                        mm(pkv[:, 0:512], CT[:, 2, :], WUKV[:], True, True, ["CT", "WUKV"], [pkvk])
                        q3 = pq[:, 0:384].rearrange("p (g x) -> p g x", x=96)
                        kv3 = pkv[:, 0:512].rearrange("p (g x) -> p g x", x=128)
                        cs = CS32[:, i, :, :]
                        cp("dve", R[:, 0:256].rearrange("p (g d) -> p g d", g=4), q3[:, :, 0:64], [pqk], ["R"])
                        rope(v4(R[:, 256:352], 3, 16), q3[:, 0:3, 64:96].rearrange("p g (two h) -> p g two h", two=2), 3, 16, cs, tC, tS, [pqk], ["R"])
                        rope(v4(R[:, 768:800], 1, 16), q3[:, 3:4, 64:96].rearrange("p g (two h) -> p g two h", two=2), 1, 16, cs, tC, tS, [pqk], ["R"])
                        cp("dve", R[:, 384:640].rearrange("p (g d) -> p g d", g=4), kv3[:, :, 0:64], [pkvk], ["R"])
                        rope(v4(kr[:, 0:32], 1, 16), v4(b[:, 384:416], 1, 16), 1, 16, cs, tC, tS, [bk], ["kr"])
                        cp("pool", R[:, 640:768].rearrange("p (g d) -> p g d", g=4), kr[:].unsqueeze(1).to_broadcast([128, 4, 32]), ["kr"], ["R"])
                        cp("dve", VM[:, i, :, 0:64], kv3[:, :, 64:128], [pkvk], ["VM"])
                        for k in range(7):
                            tr(p_[:, k, :], R[:, k * 128:(k + 1) * 128], identb[:], ["R", "identb"], [pk])
                        cp("act", MA[:, :, i * 128:(i + 1) * 128], p_[:], [pk], [("ma", i)])
                    P.barrier()
                MAR = [("ma", i) for i in range(NT)]
                with ExitStack() as st:
                    ACC, SC, MS, PEX, wk = attn_scope(st)
                    WO = load_wout(st, l, 2)
                    O = sbuf(st, "O", [128, 4, 256], F32)
                    sm_ = sbuf(st, "msm2", [128, 16], F32)
                    scale = 96 ** -0.5
                    for qc in range(4):
                        units = []
                        for h in range(4):
                            hp, blk = 64 * (h % 2), h // 2
                            units.append(dict(
                                parts=[((lambda kt, hp=hp, blk=blk: MA[hp:hp + 64, 3 + blk, kt * 128:(kt + 1) * 128]),
                                        (lambda c0, n, hp=hp, blk=blk: MA[hp:hp + 64, blk, c0:c0 + n])),
                                       ((lambda kt, h=h: MA[32 * (h % 3):32 * (h % 3) + 32, 5, kt * 128:(kt + 1) * 128]),
                                        (lambda c0, n, h=h: MA[32 * (h % 3):32 * (h % 3) + 32, 2 if h < 3 else 6, c0:c0 + n]))],
                                vf=(lambda kt, h=h: VM[:, kt, h, :]), R=MAR + ["VM"]))
                        flash(qc, units, causal, causal_edge, scale, ACC, SC, PEX)
                        for h in range(4):
                            a3 = ACC[h][:, 0:260].rearrange("p (j c) -> p j c", j=4)
                            rd = sm_[:, 4 * h:4 * h + 4]
                            recip(rd, a3[:, :, 64], [("acc", h)], [("rd", h)])
                            tt("dve", O[:, :, h * 64:(h + 1) * 64], a3[:, :, 0:64], rd.unsqueeze(2).to_broadcast([128, 4, 64]), ALU.mult,
                               [("acc", h), ("rd", h)], ["O"])
                        if dbg == ("o", 2):
                            for j in range(4):
                                P.dma(out_d[(4 * qc + j) * 128:(4 * qc + j + 1) * 128, 0:256], O[:, j, :], reads=["O"], is_output=True)
                        apply_wout(qc, O, "O", WO, SC, MS, wk)
                    P.barrier()

        def swa_mixer(l, HT, CS64):
            with ExitStack() as sa:
                SA = sbuf(sa, "SA", [128, 4, S], BF16)
                VW = sbuf(sa, "VW", [128, NT, 2, 65], BF16)
                memset("pool", VW[:], 1.0, ["VW"])
                with ExitStack() as st:
                    W = sbuf(st, "W", [128, 8, 512], BF16)
                    stg = [sbuf(st, "stg%d" % j, [128, 8, 256], F32) for j in range(2)]
                    tC = sbuf(st, "tC", [128, 512], F32)
                    tS = sbuf(st, "tS", [128, 512], F32)
                    R = sbuf(st, "R", [128, 512], BF16)
                    pb = [psum(st, "pb%d" % j, [128, 512], F32) for j in range(2)]
                    pT = [psum_tb(st, "pTn%d" % j)[:, 0:4, :] for j in range(2)]
                    load_w_cols(W, "W", dr["w_in"][l][:, SWA0:SWA0 + 512], 512, stg, "stg")
                    cols_for_tile(0, HT, W, 512, [pb[0]], [("pb", 0)])
                    for i in range(NT):
                        b = pb[i % 2]
                        bk = ("pb", i % 2)
                        if i + 1 < NT:
                            cols_for_tile(i + 1, HT, W, 512, [pb[(i + 1) % 2]], [("pb", (i + 1) % 2)])
                        cs = CS64[:, i, :, :]
                        rope(v4(R[:, 0:256], 4, 32), v4(b[:, 0:256], 4, 32), 4, 32, cs, tC, tS, [bk], ["R"])
                        Rk = R[:, 256:512].rearrange("p (g r d) -> p g r d", r=2, d=64)
                        rope(Rk[:, :, 0, :].rearrange("p g (two h) -> p g two h", two=2), v4(b[:, 256:384], 2, 32), 2, 32, cs, tC, tS, [bk], ["R"])
                        cp("pool", Rk[:, :, 1, :], Rk[:, :, 0, :], ["R"], ["R"])
                        cp("dve", VW[:, i, :, 0:64], b[:, 384:512].rearrange("p (g d) -> p g d", g=2), [bk], ["VW"])
                        p_ = pT[i % 2]
                        for k in range(4):
                            tr(p_[:, k, :], R[:, k * 128:(k + 1) * 128], identb[:], ["R", "identb"], [("pTn", i % 2)])
                        cp("act", SA[:, :, i * 128:(i + 1) * 128], p_[:], [("pTn", i % 2)], [("sa", i)])
                    P.barrier()
                SAR = [("sa", i) for i in range(NT)]
                with ExitStack() as st:
                    ACC, SC, MS, PEX, wk = attn_scope(st)
                    WO = load_wout(st, l, 3)
                    O = sbuf(st, "O", [128, 4, 256], F32)
                    sm_ = sbuf(st, "ssm", [128, 16], F32)
                    esk = sbuf(st, "esk", [128, 4], F32)
                    P.dma(esk[:], dr["swa_sinks"][l:l + 1, :].to_broadcast([128, 4]), writes=["esk"])
                    act(esk[:], esk[:], AF.Exp, ["esk"], ["esk"])
                    scale = 64 ** -0.5
                    kr, ed = window(1)
                    for qc in range(4):
                        units = []
                        for h in range(4):
                            hp, blk = 64 * (h % 2), h // 2
                            units.append(dict(
                                parts=[((lambda kt, hp=hp, blk=blk: SA[hp:hp + 64, 2 + blk, kt * 128:(kt + 1) * 128]),
                                        (lambda c0, n, hp=hp, blk=blk: SA[hp:hp + 64, blk, c0:c0 + n]))],
                                vf=(lambda kt, g=h // 2: VW[:, kt, g, :]), R=SAR + ["VW"]))
                        flash(qc, units, kr, ed, scale, ACC, SC, PEX)
                        for h in range(4):
                            a3 = ACC[h][:, 0:260].rearrange("p (j c) -> p j c", j=4)
                            rd = sm_[:, 4 * h:4 * h + 4]
                            ts("dve", rd, a3[:, :, 64], esk[:, h:h + 1], None, ALU.add, None, [("acc", h), "esk"], [("rd", h)])
                            recip(rd, rd, [("rd", h)], [("rd", h)])
                            tt("dve", O[:, :, h * 64:(h + 1) * 64], a3[:, :, 0:64], rd.unsqueeze(2).to_broadcast([128, 4, 64]), ALU.mult,
                               [("acc", h), ("rd", h)], ["O"])
                        if dbg == ("o", 3):
                            for j in range(4):
                                P.dma(out_d[(4 * qc + j) * 128:(4 * qc + j + 1) * 128, 0:256], O[:, j, :], reads=["O"], is_output=True)
                        apply_wout(qc, O, "O", WO, SC, MS, wk)
                    P.barrier()

        def ffn_phase(l):
            load_mod(l, 1)
            with ExitStack() as sf:
                ST = sbuf(sf, "ST", [128, 4, 8, 4, 128], BF16)
                LNR = sbuf(sf, "LNR", [128, 4, 8], F32)
                H2T = sbuf(sf, "H2T", [128, 8, 512], BF16)
                SKs = sbuf(sf, "SKs", [128, 2, 128], F32)
                SK = sbuf(sf, "SK", [128, 2, 128], BF16)
                P.dma(SKs[:], dr["skT"][l].rearrange("c d k -> d c k"), writes=["SKs"])
                cp("dve", SK[:], SKs[:], ["SKs"], ["SK"])
                for g in range(4):
                    with ExitStack() as st:
                        junk = sbuf(st, "junk", [128, D], BF16)
                        t1 = sbuf(st, "t1", [128, D], F32)
                        hb = sbuf(st, "hb", [128, D], BF16)
                        pT = psum_tb(st, "pT")
                        QP = sbuf(st, "QP", [128, 16, 512], BF16)
                        wqs = [sbuf(st, "wqs%d" % j, [128, 8, 128], F32) for j in range(2)]
                        wqb = [sbuf(st, "wqb%d" % j, [128, 8, 128], BF16) for j in range(2)]
                        SSs = sbuf(st, "SSs", [128, 16, 128], F32)
                        pq = [psum(st, "pq%d" % j, [128, 512], F32) for j in range(2)]
                        pss = [psum(st, "pss%d" % j, [128, 512], F32) for j in range(4)]
                        m1 = sbuf(st, "m1", [128, 16], F32)
                        m2 = sbuf(st, "m2", [128, 16], F32)
                        mc = sbuf(st, "mc", [128, 16], F32)
                        rr = sbuf(st, "rr", [128, 256], F32)
                        cand = sbuf(st, "cand", [128, 256], F32)
                        sm_ = sbuf(st, "psm", [128, 32], F32)
                        am = sbuf(st, "am", [128, 128], F32)
                        sfl = sbuf(st, "sfl", [128, 2, 128], F32)
                        shl = sbuf(st, "shl", [128, 4, 128], BF16)
                        pst_ = psum_tb(st, "pst")
                        for tt_ in range(4):
                            norm_to_T(4 * g + tt_, H2T, tt_ * 128, ("h2t", tt_), (junk, t1, hb, pT))
                        H2R = [("h2t", j) for j in range(4)]
                        for n in range(16):
                            s_ = wqs[n % 2]
                            P.dma(s_[:], dr["peer_w_q"][l][:, n * 128:(n + 1) * 128].rearrange("(k p) n -> p k n", p=128), writes=[("wqs", n % 2)])
                            cp("pool", wqb[n % 2][:], s_[:], [("wqs", n % 2)], [("wqb", n % 2)])
                            for k in range(8):
                                mm(pq[n % 2][:], wqb[n % 2][:, k, :], H2T[:, k, :], k == 0, k == 7, [("wqb", n % 2)] + H2R, [("pq", n % 2)])
                            cp("act", QP[:, n, :], pq[n % 2][:], [("pq", n % 2)], [("qp", n)])
                        for tt_ in range(4):
                            for n in range(16):
                                b = pss[n // 4]
                                mm(b[:, (n % 4) * 128:(n % 4 + 1) * 128], QP[:, n, tt_ * 128:(tt_ + 1) * 128], SK[:, n % 2, :], True, True,
                                   [("qp", n), "SK"], [("pss", n // 4)])
                            for q4 in range(4):
                                cp("act", SSs[:, 4 * q4:4 * q4 + 4, :], pss[q4][:].rearrange("p (a b) -> p a b", a=4), [("pss", q4)], [("sss", q4)])
                            for h in range(8):
                                s1 = SSs[:, 2 * h, :]
                                s2 = SSs[:, 2 * h + 1, :]
                                sk_ = [("sss", h // 2)]
                                for (sx, mx) in ((s1, m1), (s2, m2)):
                                    P.op("dve", lambda e, sx=sx, mx=mx: e.max(mx[:, 0:8], sx), reads=sk_, writes=["mx"])
                                    P.op("dve", lambda e, sx=sx, mx=mx: e.match_replace(rr[:, 0:128], mx[:, 0:8], sx, -1.0e30), reads=sk_ + ["mx"], writes=["rr"])
                                    P.op("dve", lambda e, mx=mx: e.max(mx[:, 8:16], rr[:, 0:128]), reads=["rr"], writes=["mx"])
                                c3 = cand[:].rearrange("p (a b) -> p a b", a=16)
                                tt("dve", c3, m1[:].unsqueeze(2).to_broadcast([128, 16, 16]), m2[:].unsqueeze(1).to_broadcast([128, 16, 16]), ALU.add,
                                   ["mx"], ["cand"])
                                P.op("dve", lambda e: e.max(mc[:, 0:8], cand[:]), reads=["cand"], writes=["mc"])
                                P.op("dve", lambda e: e.match_replace(rr[:], mc[:, 0:8], cand[:], -1.0e30), reads=["cand", "mc"], writes=["rr"])
                                P.op("dve", lambda e: e.max(mc[:, 8:16], rr[:]), reads=["rr"], writes=["mc"])
                                negM = sm_[:, 0:1]
                                Z = sm_[:, 1:2]
                                nthr = sm_[:, 2:3]
                                ts("dve", negM, mc[:, 0:1], -1.0, None, ALU.mult, None, ["mc"], ["psm"])
                                act(sm_[:, 8:24], mc[:], AF.Exp, ["mc", "psm"], ["psm2", "psmZ"], bias=negM, accum=Z)
                                act(Z, Z, AF.Ln, ["psmZ"], ["psmZ"])
                                ts("dve", nthr, mc[:, 15:16], -1.0, 1.0e-3, ALU.mult, ALU.add, ["mc"], ["psm"])
                                tt("dve", Z, negM, Z, ALU.subtract, ["psm", "psmZ"], ["psmZ"])
                                tt("dve", LNR[:, tt_, h:h + 1], Z, nthr, ALU.subtract, ["psm", "psmZ"], [("lnr", tt_)])
                                ts("dve", am[:], s1, m1[:, 15:16], -1.0, ALU.is_ge, ALU.add, sk_ + ["mx"], ["am"])
                                stt(am[:], am[:], BIG, s1, ALU.mult, ALU.add, ["am"] + sk_, ["am"])
                                ts("dve", sfl[:, 0, :], am[:], nthr, None, ALU.add, None, ["am", "psm"], ["sfl"])
                                ts("dve", am[:], s2, m2[:, 15:16], -1.0, ALU.is_ge, ALU.add, sk_ + ["mx"], ["am"])
                                stt(sfl[:, 1, :], am[:], BIG, s2, ALU.mult, ALU.add, ["am"] + sk_, ["sfl"])
                                shl6 = shl[:].rearrange("p (c hf) (q j) -> p c hf q j", hf=2, q=2)
                                sfl4 = sfl[:].rearrange("p c (hf j) -> p c hf j", hf=2)
                                for c_ in range(2):
                                    cp("pool", shl6[:, c_, :, 0, :], sfl4[:, c_, :, :], ["sfl"], ["shl"])
                                    tt("pool", shl6[:, c_, :, 1, :], sfl4[:, c_, :, :], shl6[:, c_, :, 0, :], ALU.subtract, ["sfl", "shl"], ["shl"])
                                for q_ in range(4):
                                    tr(pst_[:, q_, :], shl[:, q_, :], identb[:], ["shl", "identb"], ["pst"])
                                cp("act", ST[:, tt_, h, :, :], pst_[:, 0:4, :], ["pst"], [("st", tt_)])
                        P.barrier()
                    with ExitStack() as st:
                        Us = sbuf(st, "Us", [128, 8, 256], F32)
                        Vs = sbuf(st, "Vs", [128, 2, D], F32)
                        UB = [sbuf(st, "UB%d" % j, [128, 8, 512], BF16) for j in range(2)]
                        VB = [sbuf(st, "VB%d" % j, [128, 4, D], BF16) for j in range(2)]
                        NR = 6
                        Rb = [sbuf(st, "Rb%d" % j, [128, 512], BF16) for j in range(NR)]
                        Wb = [sbuf(st, "Wb%d" % j, [128, 512], BF16) for j in range(NR)]
                        GtT = sbuf(st, "GtT", [128, 8, 512], BF16)
                        Gt = [GtT[:, j, :] for j in range(8)]
                        WAT = sbuf(st, "WAT", [128, 4, 512], BF16)
                        WAm = [sbuf(st, "WAm%d" % j, [128, 512], BF16) for j in range(2)]
                        wT = [psum(st, "wT%d" % j, [128, 512], F32) for j in range(2)]
                        MC = [psum(st, "MC%d" % j, [128, 512], F32) for j in range(4)]
                        PP = [psum(st, "PP%d" % j, [128, 512], F32) for j in range(2)]
                        H2R = [("h2t", j) for j in range(4)]
                        ppc = [0]

                        def next_pp():
                            j = ppc[0] % 2
                            ppc[0] += 1
                            return PP[j], ("pp", j)

                        def load_steps(ck):
                            e0 = ck * 512
                            ub, vbf = UB[ck % 2], VB[ck % 2]
                            uk, vk = ("ub", ck % 2), ("vb", ck % 2)

                            def dma_h(hh):
                                P.dma(Us[:], dr["uT"][l][:, e0 + hh * 256:e0 + (hh + 1) * 256].rearrange("(k p) e -> p k e", p=128), writes=["Us"])
                                P.dma(Vs[:], dr["pv"][l][e0 + hh * 256:e0 + (hh + 1) * 256, :].rearrange("(s p) d -> p s d", p=128), writes=["Vs"])

                            def cast_h(hh):
                                cp("act", ub[:, :, hh * 256:(hh + 1) * 256], Us[:], ["Us"], [uk])
                                tt("pool", vbf[:, 2 * hh:2 * hh + 2, :], Vs[:], MOD[:, 2, :].unsqueeze(1).to_broadcast([128, 2, D]), ALU.mult, ["Vs", "mod"], [vk])
                            return [lambda: dma_h(0), lambda: (cast_h(0), dma_h(1)), lambda: cast_h(1)]

                        def front(ck, subs=(0, 1, 2, 3)):
                            ub, uk = UB[ck % 2], ("ub", ck % 2)
                            for t4 in subs:
                                p_, pk_ = next_pp()
                                for k in range(8):
                                    mm(p_[:], H2T[:, k, t4 * 128:(t4 + 1) * 128], ub[:, k, :], k == 0, k == 7, H2R + [uk], [pk_])
                                gj = (ck % 2) * 4 + t4
                                act(Gt[gj], p_[:], GELU, [pk_], [("gt", gj)])

                        def tail_v(ck):
                            vbf, vk = VB[ck % 2], ("vb", ck % 2)
                            outl = []
                            for tt_ in range(4):
                                for hf in range(2):
                                    def vgrp(tt_=tt_, hf=hf, vbf=vbf, vk=vk):
                                        i = 4 * g + tt_
                                        o_, ok = next_pp()
                                        for s_ in range(4):
                                            mm(o_[:], WAT[:, s_, tt_ * 128:(tt_ + 1) * 128], vbf[:, s_, hf * 512:(hf + 1) * 512], s_ == 0, s_ == 3, [("wat", tt_), vk], [ok])
                                        tt("dve", X[:, i, hf * 512:(hf + 1) * 512], X[:, i, hf * 512:(hf + 1) * 512], o_[:], ALU.add, [("x", i), ok], [("x", i)])
                                    outl.append(vgrp)
                            return outl

                        cn = 0
                        for f_ in load_steps(0):
                            f_()
                        front(0)
                        deferred = []
                        sel2 = identb2[:, :].unsqueeze(1).to_broadcast([128, 4, 64])
                        wtc = 0
                        pendq = []
                        midq = []
                        watq = [None]
                        for ck in range(32):
                            un_i = 0
                            half_, ckk = ck // 16, ck % 16
                            sel1 = identb2[:, 4 * ckk:4 * ckk + 4].unsqueeze(2).to_broadcast([128, 4, 128])
                            for tt_ in range(4):
                                wtb = wT[wtc % 2]
                                wtk = ("wt", wtc % 2)
                                wtc += 1
                                wt3 = wtb[:].rearrange("p (s t) -> p s t", s=4)
                                for h in range(8):
                                    kk = cn % NR
                                    rb, wb = Rb[kk], Wb[kk]
                                    mcb = MC[cn % 4]
                                    mk_ = ("mc", cn % 4)
                                    mc3 = mcb[:].rearrange("p (a b) -> p a b", a=4)
                                    stk = [("st", tt_), "identb2"]
                                    mm(mc3, ST[:, tt_, h, half_, :], sel1, True, False, stk, [mk_])
                                    mm(mc3[:, :, 0:64], ST[:, tt_, h, 2, :], sel2, False, False, stk, [mk_])
                                    mm(mc3[:, :, 64:128], ST[:, tt_, h, 3, :], sel2, False, False, stk, [mk_], sig=True)
                                    ts("dve", rb[:], mcb[:], 1.0e6, 0.0, ALU.mult, ALU.min, [mk_], [("rb", kk)])
                                    cn += 1

                                    def fin_unit(kk=kk, h=h, wb=wb, wtb=wtb, wtk=wtk):
                                        mm(wtb[:], identb[:], wb[:], h == 0, h == 7, [("wb", kk), "identb"], [wtk], sig=True)

                                    def mid_unit(kk=kk, tt_=tt_, h=h, rb=rb, wb=wb, mcb=mcb, mk_=mk_, fin_unit=fin_unit):
                                        mm(mcb[:], identb[:], rb[:], False, True, ["identb", ("rb", kk)], [mk_])
                                        act(wb[:], mcb[:], AF.Exp, [mk_, ("lnr", tt_)], [("wb", kk)], bias=LNR[:, tt_, h:h + 1])
                                        pendq.append(fin_unit)
                                        if len(pendq) > 2:
                                            pendq.pop(0)()
                                    midq.append(mid_unit)
                                    if len(midq) > 1:
                                        midq.pop(0)()
                                    if h == 3 and watq[0] is not None:
                                        watq[0]()
                                        watq[0] = None
                                    if deferred:
                                        deferred.pop(0)()
                                    un_i += 1
                                    if ck + 1 < 32:
                                        if un_i == 9:
                                            while deferred:
                                                deferred.pop(0)()
                                            lsteps = load_steps(ck + 1)
                                            lsteps[0]()
                                        elif un_i == 16:
                                            lsteps[1]()
                                        elif un_i == 24:
                                            lsteps[2]()
                                        elif un_i == 27:
                                            front(ck + 1, (0, 1))
                                        elif un_i == 31:
                                            front(ck + 1, (2, 3))
                                    if h == 7:
                                        def wat_fn(tt_=tt_, gj=(ck % 2) * 4 + tt_, wtb=wtb, wtk=wtk, wslot=(wtc % 2)):
                                            wam = WAm[wslot]
                                            tt("dve", wam[:], Gt[gj], wtb[:], ALU.mult, [("gt", gj), wtk], [("wam", wslot)])
                                            p_, pk_ = next_pp()
                                            pb_ = p_[:].bitcast(BF16).rearrange("p (k t) -> p k t", t=128)
                                            for s_ in range(4):
                                                tr(pb_[:, s_, :], wam[:, s_ * 128:(s_ + 1) * 128], identb[:], [("wam", wslot), "identb"], [pk_])
                                            cp("act", WAT[:, :, tt_ * 128:(tt_ + 1) * 128], pb_[:, 0:4, :], [pk_], [("wat", tt_)])
                                        watq[0] = wat_fn
                            while deferred:
                                deferred.pop(0)()
                            deferred = tail_v(ck)
                        while midq:
                            midq.pop(0)()
                        while pendq:
                            pendq.pop(0)()
                        watq[0]()
                        while deferred:
                            deferred.pop(0)()
                        P.barrier()

        for l in layers:
            mixer_phase(l)
            if xmid_d is not None:
                for i in range(NT):
                    P.dma(xmid_d[i * 128:(i + 1) * 128, :], X[:, i, :], reads=[("x", i)], is_output=True)
            if do_ffn:
                ffn_phase(l)

        with ExitStack() as st:
            fgb = sbuf(st, "fgb", [128, D], F32)
            junk = sbuf(st, "junk", [128, D], BF16)
            ob = [sbuf(st, "ob%d" % j, [128, D], F32) for j in range(2)]
            P.dma(fgb[:], dr["final_g"].to_broadcast([128, D]), writes=["fgb"])
            if dbg is None or dbg == "final":
                for i in range(NT):
                    xi = X[:, i, :]
                    act(junk[:], xi, AF.Square, [("x", i)], ["junk", ("ssq", i)], accum=SSQ[:, i:i + 1])
                    act(RSTD[:, i:i + 1], SSQ[:, i:i + 1], AF.Sqrt, [("ssq", i), "epsc"], [("rstd", i)], bias=epsc[:], scale=1.0 / D)
                    recip(RSTD[:, i:i + 1], RSTD[:, i:i + 1], [("rstd", i)], [("rstd", i)])
                    stt(ob[i % 2][:], xi, RSTD[:, i:i + 1], fgb[:], ALU.mult, ALU.mult, [("x", i), ("rstd", i), "fgb"], [("ob", i % 2)])
                    P.dma(out_d[i * 128:(i + 1) * 128, :], ob[i % 2][:], reads=[("ob", i % 2)], is_output=True)
            elif dbg == "x":
                for i in range(NT):
                    P.dma(out_d[i * 128:(i + 1) * 128, :], X[:, i, :], reads=[("x", i)], is_output=True)
            P.barrier()
        P.finish()
        print("instructions:", P.n_inst, {k: len(v) for k, v in P.q.items()}, "dma sems:", len(P.dma_sem), flush=True)
    return nc


def _consts():
    c = {}
    c["ident"] = np.eye(128, dtype=np.float32)
    k = np.arange(128)[:, None]
    q = np.arange(128)[None, :]
    c["tri"] = np.stack([(k <= q), (k > q)], axis=1).astype(np.float32)

    def rt(dim):
        inv = (1.0 / (np.float32(10000.0) ** (np.arange(0, dim, 2, dtype=np.float32) / np.float32(dim)))).astype(np.float32)
        ang = (np.arange(S, dtype=np.float32)[:, None] * inv[None, :]).astype(np.float32)
        return np.stack([np.cos(ang), np.sin(ang)], axis=1).astype(np.float32)
    c["cs64"] = rt(64)
    c["cs32"] = rt(32)
    cidx_last = np.arange(127) * 16 + 31
    cm = np.zeros((128, S), np.float32)
    cm[:127] = (cidx_last[:, None] <= np.arange(S)[None, :])
    c["cmaskT"] = cm
    t = np.arange(S)
    qblk = t // 64
    jj = np.arange(32)
    allowed = jj[None, :] <= qblk[:, None]
    forced = (jj[None, :] == 0) | (jj[None, :] == qblk[:, None]) | (jj[None, :] == qblk[:, None] - 1)
    M1 = (allowed & ~forced).astype(np.float32)
    M2 = np.where(allowed, np.where(forced, 1e4, 0.0), -1e30).astype(np.float32)
    c["selM"] = np.stack([M1, M2, allowed.astype(np.float32)], axis=1)
    ex = np.zeros((32, S), np.float32)
    ex[np.arange(S) // 64, np.arange(S)] = 1.0
    c["expand"] = ex
    cidx = np.arange(127)[:, None] * 16 + np.arange(32)[None, :]
    ov = np.zeros((128, 33), np.float32)
    ov[:127, 0] = 1.0
    for cc in range(127):
        for p in cidx[cc]:
            ov[cc, 1 + p // 64] += 1.0 / 32.0
    c["ovl1"] = ov
    return c


def _prep_shared(inp):
    f = lambda a: np.ascontiguousarray(np.asarray(a, dtype=np.float32))
    w_in = f(inp["w_in"])
    perm = np.concatenate([np.arange(0, 256), np.arange(256, 320), np.arange(384, 448), np.arange(512, 576),
                           np.arange(320, 384), np.arange(448, 512), np.arange(576, 640), np.arange(640, 652),
                           np.arange(652, 2348)])
    sh = {
        "ada_w": f(inp["ada_w"]), "ada_b": f(inp["ada_b"]),
        "gmix": f(inp["norm_mix_g"]), "gffn": f(inp["norm_ffn_g"]), "final_g": f(inp["final_g"]).reshape(1, D),
        "w_in": f(w_in[:, :, perm]), "w_out": f(inp["w_out"]),
        "wkD": f(np.concatenate([inp["nsa_cmp_wk"], inp["nsa_cmp_wk"]], axis=-1)), "wv": f(inp["nsa_cmp_wv"]),
        "poskT": f(np.transpose(inp["nsa_cmp_pos_k"], (0, 2, 1))), "posvT": f(np.transpose(inp["nsa_cmp_pos_v"], (0, 2, 1))),
        "lam4": f(np.stack([inp["diff_lam_q1"], inp["diff_lam_k1"], inp["diff_lam_q2"], inp["diff_lam_k2"]], axis=1)),
        "sub_g": f(inp["diff_sub_g"]),
        "mla_qg": f(inp["mla_q_norm_g"]), "mla_w_uq": f(inp["mla_w_uq"]), "mla_kvg": f(inp["mla_kv_norm_g"]), "mla_w_ukv": f(inp["mla_w_ukv"]),
        "swa_sinks": f(inp["swa_sinks"]),
        "peer_w_q": f(inp["peer_w_q"]),
        "skT": f(np.stack([np.transpose(inp["peer_sub_k1"], (0, 2, 1)), np.transpose(inp["peer_sub_k2"], (0, 2, 1))], axis=1)),
        "uT": f(np.transpose(inp["peer_u"], (0, 2, 1))), "pv": f(inp["peer_v"]),
    }
    sh.update(_consts())
    return sh


def make_in_maps(inp):
    sh = _prep_shared(inp)
    x = np.asarray(inp["x"], dtype=np.float32)
    c = np.asarray(inp["c"], dtype=np.float32)
    maps = []
    for b in range(8):
        m = dict(sh)
        m["x"] = np.ascontiguousarray(x[b])
        m["cT"] = np.ascontiguousarray(c[b].reshape(8, 128).T)
        maps.append(m)
    return maps


_NC_CACHE = {}


def kernel(**inputs):
    if "nc" not in _NC_CACHE:
        _NC_CACHE["nc"] = build()
    nc = _NC_CACHE["nc"]
    in_maps = make_in_maps(inputs)
    res = run_bass_kernel_spmd(nc, in_maps, core_ids=list(range(8)))
    return np.stack([np.asarray(r["out"], dtype=np.float32) for r in res.results], axis=0)
```

```python
import math
from contextlib import ExitStack
import numpy as np
import concourse.bass as bass
import concourse.mybir as mybir
from concourse.bass_utils import run_bass_kernel_spmd

F32 = mybir.dt.float32
BF16 = mybir.dt.bfloat16
ALU = mybir.AluOpType
AF = mybir.ActivationFunctionType
AX = mybir.AxisListType

COMPUTE = ("pe", "dve", "act", "pool")
L = 2
S = 2048
D = 1024
NT = 16
EPS = 1e-6
BIG = 1.0e4
GELU = AF.Gelu_apprx_tanh
RELAX = 1 << 30

NSA0, DIF0, MLA0, SWA0 = 0, 652, 1420, 1836


class Prog:
    def __init__(self, nc, es):
        self.nc = nc
        self.es = es
        self.engs = {"pe": nc.tensor, "dve": nc.vector, "act": nc.scalar, "pool": nc.gpsimd, "sp": nc.sync}
        self.q = {k: [] for k in self.engs}
        self.cnt = {k: 0 for k in COMPUTE}
        self.sem = {k: es.enter_context(nc.semaphore("s_" + k)) for k in COMPUTE}
        self.seen = {k: {} for k in self.engs}
        self.lastw = {}
        self.readers = {}
        self.dma_sem = {}
        self.n_inst = 0
        self.out_waits = []

    def _deps(self, reads, writes):
        deps = []
        for k in reads:
            t = self.lastw.get(k)
            if t is not None:
                deps.append(t)
        for k in writes:
            t = self.lastw.get(k)
            if t is not None:
                deps.append(t)
            deps.extend(self.readers.get(k, ()))
        return deps

    def _emit_waits(self, eng, deps, self_sync=True):
        best = {}
        for (name, sem, val) in deps:
            if name == eng and (not self_sync or self.cnt[eng] - val >= RELAX):
                continue
            if name not in best or best[name][1] < val:
                best[name] = (sem, val)
        for name, (sem, val) in best.items():
            if self.seen[eng].get(name, 0) >= val:
                continue
            self.seen[eng][name] = val
            self.q[eng].append(lambda e, sem=sem, val=val: e.wait_ge(sem, val))

    def _commit(self, tok, reads, writes):
        for k in writes:
            self.lastw[k] = tok
            self.readers[k] = []
        for k in reads:
            if k in writes:
                continue
            self.readers.setdefault(k, []).append(tok)

    def op(self, eng, fn, reads=(), writes=(), self_sync=True, sig=True):
        deps = self._deps(reads, writes)
        self._emit_waits(eng, deps, self_sync)
        sem = self.sem[eng]
        if sig:
            self.cnt[eng] += 1
            val = self.cnt[eng]
            self.q[eng].append(lambda e, fn=fn, sem=sem: fn(e).then_inc(sem, 1))
        else:
            val = self.cnt[eng] + 1
            self.q[eng].append(lambda e, fn=fn: fn(e))
        self._commit((eng, sem, val), reads, writes)
        self.n_inst += 1

    def dma(self, out, in_, reads=(), writes=(), queue="sp", is_output=False):
        deps = self._deps(reads, writes)
        self._emit_waits(queue, deps)
        key = writes[0] if writes else ("rd", reads[0])
        if key not in self.dma_sem:
            self.dma_sem[key] = [self.es.enter_context(self.nc.semaphore("d%d" % len(self.dma_sem))), 0]
        ent = self.dma_sem[key]
        ent[1] += 16
        sem, val = ent[0], ent[1]
        self.q[queue].append(lambda e, out=out, in_=in_, sem=sem: e.dma_start(out=out, in_=in_).then_inc(sem, 16))
        tok = ("dma:%s" % str(key), sem, val)
        self._commit(tok, reads, writes)
        if is_output:
            self.out_waits.append(tok)
        self.n_inst += 1

    def barrier(self):
        deps = [(k, self.sem[k], self.cnt[k]) for k in COMPUTE if self.cnt[k] > 0]
        for key, (sem, val) in self.dma_sem.items():
            if val > 0:
                deps.append(("dma:%s" % str(key), sem, val))
        for eng in self.engs:
            self._emit_waits(eng, deps, self_sync=False)
        self.lastw = {}
        self.readers = {}

    def finish(self):
        self._emit_waits("sp", self.out_waits)
        with self.nc.Block() as block:
            @block.sync
            def _(e):
                for f in self.q["sp"]:
                    f(e)

            @block.tensor
            def _(e):
                for f in self.q["pe"]:
                    f(e)

            @block.vector
            def _(e):
                for f in self.q["dve"]:
                    f(e)

            @block.scalar
            def _(e):
                for f in self.q["act"]:
                    f(e)

            @block.gpsimd
            def _(e):
                for f in self.q["pool"]:
                    f(e)


INPUT_SPECS = [
    ("x", [S, D]), ("cT", [128, 8]),
    ("ada_w", [L, D, 6 * D]), ("ada_b", [L, 6 * D]),
    ("gmix", [L, D]), ("gffn", [L, D]), ("final_g", [1, D]),
    ("w_in", [L, D, 2348]), ("w_out", [L, D, D]),
    ("wkD", [L, 2048, 128]), ("wv", [L, 2048, 64]), ("poskT", [L, 64, 32]), ("posvT", [L, 64, 32]),
    ("lam4", [L, 4, 32]), ("sub_g", [L, 64]),
    ("mla_qg", [L, 256]), ("mla_w_uq", [L, 256, 384]), ("mla_kvg", [L, 128]), ("mla_w_ukv", [L, 128, 512]),
    ("swa_sinks", [L, 4]),
    ("peer_w_q", [L, D, 2048]), ("skT", [L, 2, 128, 128]),
    ("uT", [L, D, 16384]), ("pv", [L, 16384, D]),
    ("ident", [128, 128]), ("tri", [128, 2, 128]),
    ("cs64", [S, 2, 32]), ("cs32", [S, 2, 16]),
    ("cmaskT", [128, S]), ("selM", [S, 3, 32]), ("expand", [32, S]), ("ovl1", [128, 33]),
]


def build(layers=(0, 1), dbg=None, do_mixers=(0, 1, 2, 3), do_ffn=True):
    nc = bass.Bass("TRN2", target_bir_lowering=False)
    dr = {}
    for name, shape in INPUT_SPECS:
        dr[name] = nc.dram_tensor(name, shape, F32, kind="ExternalInput").ap()
    out_d = nc.dram_tensor("out", [S, D], F32, kind="ExternalOutput").ap()
    xmid_d = nc.dram_tensor("xmid", [S, D], F32, kind="ExternalOutput").ap() if dbg == "x" else None

    with ExitStack() as es:
        P = Prog(nc, es)

        uid = [0]

        def sbuf(st, name, shape, dt):
            uid[0] += 1
            return st.enter_context(nc.sbuf_tensor("%s_s%d" % (name, uid[0]), shape, dt))

        def psum(st, name, shape, dt):
            uid[0] += 1
            return st.enter_context(nc.psum_tensor("%s_p%d" % (name, uid[0]), shape, dt))

        def psum_tb(st, name):
            t_ = psum(st, name, [128, 512], F32)
            return t_[:].bitcast(BF16).rearrange("p (k t) -> p k t", t=128)

        def mm(o, l, r, st, sp, R, W, sig=None):
            P.op("pe", lambda e: e.matmul(o, l, r, start=st, stop=sp, skip_group_check=True), reads=R, writes=W, self_sync=False,
                 sig=(sp if sig is None else sig))

        def tr(o, i, idn, R, W):
            P.op("pe", lambda e: e.transpose(o, i, idn), reads=R, writes=W, self_sync=False)

        def tt(eng, o, a, b, op, R, W):
            P.op(eng, lambda e: e.tensor_tensor(o, a, b, op), reads=R, writes=W)

        def ts(eng, o, a, s1, s2, op0, op1, R, W):
            if s2 is None:
                P.op(eng, lambda e: e.tensor_scalar(o, a, s1, None, op0), reads=R, writes=W)
            else:
                P.op(eng, lambda e: e.tensor_scalar(o, a, s1, s2, op0, op1), reads=R, writes=W)

        def stt(o, a, s, b, op0, op1, R, W):
            P.op("dve", lambda e: e.scalar_tensor_tensor(o, a, s, b, op0, op1), reads=R, writes=W)

        def act(o, i, func, R, W, bias=None, scale=None, accum=None):
            kw = {}
            if bias is not None:
                kw["bias"] = bias
            if scale is not None:
                kw["scale"] = scale
            if accum is not None:
                kw["accum_out"] = accum
            P.op("act", lambda e: e.activation(o, i, func, **kw), reads=R, writes=W)

        def cp(eng, o, i, R, W):
            if eng == "act":
                P.op("act", lambda e: e.copy(o, i), reads=R, writes=W)
            else:
                P.op(eng, lambda e: e.tensor_copy(o, i), reads=R, writes=W)

        def memset(eng, o, val, W):
            P.op(eng, lambda e: e.memset(o, val), writes=W)

        def red(o, i, op, R, W):
            P.op("dve", lambda e: e.tensor_reduce(o, i, AX.X, op), reads=R, writes=W)

        def recip(o, i, R, W):
            P.op("dve", lambda e: e.reciprocal(o, i), reads=R, writes=W)

        X = sbuf(es, "X", [128, NT, D], F32)
        MOD = sbuf(es, "MOD", [128, 3, D], F32)
        identf = sbuf(es, "identf", [128, 128], F32)
        identb = sbuf(es, "identb", [128, 128], BF16)
        tri = sbuf(es, "tri", [128, 2, 128], BF16)
        identb2 = sbuf(es, "identb2", [128, 64], BF16)
        zrow = sbuf(es, "zrow", [1, 512], BF16)
        onesb = sbuf(es, "onesb", [1, 128], BF16)
        scb = sbuf(es, "scb", [128, 8, 128], F32)
        SSQ = sbuf(es, "SSQ", [128, NT], F32)
        RSTD = sbuf(es, "RSTD", [128, NT], F32)
        epsc = sbuf(es, "epsc", [128, 1], F32)

        with ExitStack() as st0:
            trif = sbuf(st0, "trif", [128, 2, 128], F32)
            ct = sbuf(st0, "ct", [128, 8], F32)
            P.dma(identf[:], dr["ident"], writes=["identf"])
            P.dma(trif[:], dr["tri"], writes=["trif"])
            P.dma(ct[:], dr["cT"], writes=["ct"])
            for i in range(NT):
                P.dma(X[:, i, :], dr["x"][i * 128:(i + 1) * 128, :], writes=[("x", i)])
            cp("dve", identb[:], identf[:], ["identf"], ["identb"])
            cp("dve", tri[:], trif[:], ["trif"], ["tri"])
            tt("pool", identb2[:], identb[:, 0:64], identb[:, 64:128], ALU.add, ["identb"], ["identb2"])
            memset("pool", zrow[:], 0.0, ["zrow"])
            memset("pool", onesb[:], 1.0, ["onesb"])
            memset("pool", epsc[:], EPS, ["epsc"])
            act(ct[:], ct[:], AF.Silu, ["ct"], ["ct"])
            cp("dve", scb[:], ct[:].unsqueeze(2).to_broadcast([128, 8, 128]), ["ct"], ["scb"])
            P.barrier()

        def load_mod(l, half):
            with ExitStack() as st:
                AW = [sbuf(st, "AW%d" % j, [128, 8, 512], F32) for j in range(2)]
                gbc = sbuf(st, "gbc", [128, D], F32)
                pm = [psum(st, "pm%d" % j, [128, 512], F32) for j in range(2)]
                MODf = MOD[:].rearrange("p a d -> p (a d)")
                c0 = half * 3 * D
                P.dma(MODf, dr["ada_b"][l:l + 1, c0:c0 + 3 * D].to_broadcast([128, 3 * D]), writes=["mod"])
                gsrc = dr["gmix"] if half == 0 else dr["gffn"]
                P.dma(gbc[:], gsrc[l:l + 1, :].to_broadcast([128, D]), writes=["gbc"])
                for n in range(6):
                    stg = AW[n % 2]
                    P.dma(stg[:], dr["ada_w"][l][:, c0 + n * 512:c0 + (n + 1) * 512].rearrange("(k p) n -> p k n", p=128),
                          writes=[("AW", n % 2)])
                    for k in range(8):
                        mm(pm[n % 2][:], scb[:, k, :], stg[:, k, :], k == 0, k == 7, [("AW", n % 2), "scb"], [("pm", n % 2)])
                    tt("dve", MODf[:, n * 512:(n + 1) * 512], pm[n % 2][:], MODf[:, n * 512:(n + 1) * 512], ALU.add,
                       [("pm", n % 2), "mod"], ["mod"])
                stt(MOD[:, 1, :], MOD[:, 1, :], 1.0, gbc[:], ALU.add, ALU.mult, ["mod", "gbc"], ["mod"])
                P.barrier()

        def norm_to_T(i, dstT, col0, dkey, wk):
            junk, t1, hb, pT = wk
            xi = X[:, i, :]
            act(junk[:], xi, AF.Square, [("x", i)], ["junk", ("ssq", i)], accum=SSQ[:, i:i + 1])
            act(RSTD[:, i:i + 1], SSQ[:, i:i + 1], AF.Sqrt, [("ssq", i), "epsc"], [("rstd", i)], bias=epsc[:], scale=1.0 / D)
            recip(RSTD[:, i:i + 1], RSTD[:, i:i + 1], [("rstd", i)], [("rstd", i)])
            stt(t1[:], xi, RSTD[:, i:i + 1], MOD[:, 1, :], ALU.mult, ALU.mult, [("x", i), ("rstd", i), "mod"], ["t1"])
            tt("pool", hb[:], t1[:], MOD[:, 0, :], ALU.add, ["t1", "mod"], ["hb"])
            for k in range(8):
                tr(pT[:, k, :], hb[:, k * 128:(k + 1) * 128], identb[:], ["hb", "identb"], ["pT"])
            cp("act", dstT[:, :, col0:col0 + 128], pT[:], ["pT"], [dkey])

        def load_w_cols(W, wkey, src2d, ncols, stg, sname):
            j = 0
            for c0 in range(0, ncols, 256):
                n = min(256, ncols - c0)
                s_ = stg[j % len(stg)]
                sk = (sname, j % len(stg))
                P.dma(s_[:, :, 0:n], src2d[:, c0:c0 + n].rearrange("(k p) n -> p k n", p=128), writes=[sk])
                cp("act" if j % 2 == 0 else "pool", W[:, :, c0:c0 + n], s_[:, :, 0:n], [sk], [wkey])
                j += 1

        def rope(dst4, src4, G, half, cs_i, tC, tS, R, W):
            cb = cs_i[:, 0, :].unsqueeze(1).unsqueeze(1).to_broadcast([128, G, 2, half])
            sb_ = cs_i[:, 1, :].unsqueeze(1).unsqueeze(1).to_broadcast([128, G, 2, half])
            tC4 = tC[:, 0:G * 2 * half].rearrange("p (g two h) -> p g two h", two=2, h=half)
            tS4 = tS[:, 0:G * 2 * half].rearrange("p (g two h) -> p g two h", two=2, h=half)
            tt("dve", tC4, src4, cb, ALU.mult, R + ["cs"], ["tC"])
            tt("dve", tS4, src4, sb_, ALU.mult, R + ["cs"], ["tS"])
            tt("pool", dst4[:, :, 0, :], tC4[:, :, 0, :], tS4[:, :, 1, :], ALU.subtract, ["tC", "tS"], W)
            tt("pool", dst4[:, :, 1, :], tS4[:, :, 0, :], tC4[:, :, 1, :], ALU.add, ["tC", "tS"], W)

        def v4(ap, G, half):
            return ap.rearrange("p (g two h) -> p g two h", two=2, h=half)

        def flash(qc, units, kt_range, edge, scale, ACC, SC, PEX, shared_mask=None):
            qts = [4 * qc + j for j in range(4)]
            lo = min(kt_range(q)[0] for q in qts)
            hi = max(kt_range(q)[1] for q in qts)
            for u in range(len(units)):
                mm(ACC[u][:, 0:512], onesb[0:1, 0:128], zrow[0:1, 0:512], True, False, ["onesb", "zrow"], [("acc", u)])
            steps = []
            for kt in range(lo, hi + 1):
                need = [j for j, q in enumerate(qts) if kt_range(q)[0] <= kt <= kt_range(q)[1]]
                j0, j1 = need[0], need[-1]
                c0 = (4 * qc + j0) * 128
                n = (j1 - j0 + 1) * 128
                for u, un in enumerate(units):
                    steps.append((kt, u, un, need, j0, c0, n))
            smc = {}

            def emit_scores(idx):
                kt, u, un, need, j0, c0, n = steps[idx]
                if shared_mask is not None and kt not in smc:
                    smc[kt] = shared_mask(kt, c0, n, j0)
                sc = SC[idx % 2]
                sk = ("sc", idx % 2)
                np_ = len(un["parts"])
                for pi, (kf, qf) in enumerate(un["parts"]):
                    mm(sc[:, 0:n], kf(kt), qf(c0, n), pi == 0, pi == np_ - 1, un["R"], [sk])

            def emit_rest(idx):
                kt, u, un, need, j0, c0, n = steps[idx]
                sc = SC[idx % 2]
                sk = ("sc", idx % 2)
                pe = PEX[idx % len(PEX)]
                pk = ("pex", idx % len(PEX))
                act(pe[:, 0:n], sc[:, 0:n], AF.Exp, [sk], [pk], scale=scale)
                if shared_mask is not None:
                    sm = smc[kt]
                    tt("dve", pe[:, 0:n], pe[:, 0:n], sm[0], ALU.mult, [pk, sm[1]], [pk])
                else:
                    for j in need:
                        ed = edge(kt, 4 * qc + j)
                        if ed is not None:
                            sl = pe[:, (j - j0) * 128:(j - j0 + 1) * 128]
                            tt("pool", sl, sl, tri[:, ed, :], ALU.mult, [pk, "tri"], [pk])
                for j in need:
                    last = (kt == kt_range(4 * qc + j)[1])
                    mm(ACC[u][:, j * 65:(j + 1) * 65], pe[:, (j - j0) * 128:(j - j0 + 1) * 128], un["vf"](kt),
                       False, last, [pk] + un["R"], [("acc", u)])

            emit_scores(0)
            for idx in range(len(steps)):
                if idx + 1 < len(steps):
                    emit_scores(idx + 1)
                emit_rest(idx)

        def apply_wout(qc, O, okey, WO, SC, MS, wk):
            OT, tmpx = wk
            for j in range(4):
                i = 4 * qc + j
                pT = MS[j % 2]
                pk = ("ms", j % 2)
                pTf = pT[:, 0:256].rearrange("p (k t) -> p k t", k=2)
                for k in range(2):
                    tr(pTf[:, k, :], O[:, j, k * 128:(k + 1) * 128], identf[:], [okey, "identf"], [pk])
                cp("act", OT[:], pTf, [pk], ["OT"])
                for hf in range(2):
                    for k in range(2):
                        mm(SC[hf][:, 0:512], OT[:, k, :], WO[:, k, hf * 512:(hf + 1) * 512], k == 0, k == 1, ["OT", "WO"], [("sc", hf)])
                for hf in range(2):
                    tt("dve", tmpx[:, hf * 512:(hf + 1) * 512], SC[hf][:, 0:512], MOD[:, 2, hf * 512:(hf + 1) * 512], ALU.mult,
                       [("sc", hf), "mod"], ["tmpx"])
                tt("pool", X[:, i, :], X[:, i, :], tmpx[:], ALU.add, [("x", i), "tmpx"], [("x", i)])

        def load_wout(st, l, m):
            WOs = sbuf(st, "WOs", [128, 2, 512], F32)
            WO = sbuf(st, "WO", [128, 2, D], BF16)
            for hf in range(2):
                P.dma(WOs[:], dr["w_out"][l][m * 256:(m + 1) * 256, hf * 512:(hf + 1) * 512].rearrange("(k p) n -> p k n", p=128),
                      writes=["WOs"])
                cp("act", WO[:, :, hf * 512:(hf + 1) * 512], WOs[:], ["WOs"], ["WO"])
            return WO

        def causal(qt):
            return (0, qt)

        def causal_edge(kt, qt):
            return 0 if kt == qt else None

        def window(wt):
            return (lambda qt: (max(0, qt - wt), qt)), (lambda kt, qt: 0 if kt == qt else (1 if kt == qt - wt else None))

        def mixer_phase(l):
            load_mod(l, 0)
            with ExitStack() as sm:
                HT = sbuf(sm, "HT", [128, 8, S], BF16)
                CS64 = sbuf(sm, "CS64", [128, NT, 2, 32], F32)
                CS32 = sbuf(sm, "CS32", [128, NT, 2, 16], F32)
                P.dma(CS64[:], dr["cs64"].rearrange("(i p) c h -> p i c h", p=128), writes=["cs"])
                P.dma(CS32[:], dr["cs32"].rearrange("(i p) c h -> p i c h", p=128), writes=["cs"])
                with ExitStack() as st:
                    junk = sbuf(st, "junk", [128, D], BF16)
                    t1 = sbuf(st, "t1", [128, D], F32)
                    hb = sbuf(st, "hb", [128, D], BF16)
                    pT = psum_tb(st, "pT")
                    for i in range(NT):
                        norm_to_T(i, HT, i * 128, ("hT", i), (junk, t1, hb, pT))
                    P.barrier()
                if 0 in do_mixers:
                    nsa_mixer(l, HT, CS64)
                if 1 in do_mixers:
                    diff_mixer(l, HT, CS32)
                if 2 in do_mixers:
                    mla_mixer(l, HT, CS32)
                if 3 in do_mixers:
                    swa_mixer(l, HT, CS64)
                P.barrier()

        def cols_for_tile(i, HT, W, ncols, banks, bkeys):
            for b, c0 in enumerate(range(0, ncols, 512)):
                n = min(512, ncols - c0)
                for k in range(8):
                    mm(banks[b][:, 0:n], HT[:, k, i * 128:(i + 1) * 128], W[:, k, c0:c0 + n], k == 0, k == 7,
                       [("hT", i), "W"], [bkeys[b]])

        def attn_scope(st, npex=3):
            ACC = [psum(st, "ACC%d" % j, [128, 512], F32) for j in range(4)]
            SC = [psum(st, "SC%d" % j, [128, 512], F32) for j in range(2)]
            MS = [psum(st, "MS%d" % j, [128, 512], F32) for j in range(2)]
            PEX = [sbuf(st, "PEX%d" % j, [128, 512], BF16) for j in range(npex)]
            OT = sbuf(st, "OT", [128, 2, 128], BF16)
            tmpx = sbuf(st, "tmpx", [128, D], F32)
            return ACC, SC, MS, PEX, (OT, tmpx)

        def nsa_mixer(l, HT, CS64):
            with ExitStack() as sa:
                NA = sbuf(sa, "NA", [128, 6, S], BF16)
                VS = sbuf(sa, "VS", [128, NT, 2, 65], BF16)
                GT = sbuf(sa, "GT", [128, NT, 12], F32)
                memset("pool", VS[:], 1.0, ["VS"])
                with ExitStack() as st:
                    W = sbuf(st, "W", [128, 8, 768], BF16)
                    stg = [sbuf(st, "stg%d" % j, [128, 8, 256], F32) for j in range(2)]
                    tC = sbuf(st, "tC", [128, 512], F32)
                    tS = sbuf(st, "tS", [128, 512], F32)
                    R = sbuf(st, "R", [128, 768], BF16)
                    pb = [psum(st, "pb%d" % j, [128, 512], F32) for j in range(4)]
                    pT = [psum_tb(st, "pTn%d" % j)[:, 0:6, :] for j in range(2)]
                    load_w_cols(W, "W", dr["w_in"][l][:, NSA0:NSA0 + 652], 652, stg, "stg")
                    cols_for_tile(0, HT, W, 652, [pb[0], pb[1]], [("pb", 0), ("pb", 1)])
                    for i in range(NT):
                        b = [pb[(2 * i) % 4], pb[(2 * i + 1) % 4]]
                        bk = [("pb", (2 * i) % 4), ("pb", (2 * i + 1) % 4)]
                        if i + 1 < NT:
                            cols_for_tile(i + 1, HT, W, 652, [pb[(2 * i + 2) % 4], pb[(2 * i + 3) % 4]], [("pb", (2 * i + 2) % 4), ("pb", (2 * i + 3) % 4)])
                        cs = CS64[:, i, :, :]
                        rope(v4(R[:, 0:256], 4, 32), v4(b[0][:, 0:256], 4, 32), 4, 32, cs, tC, tS, [bk[0]], ["R"])
                        Rk = R[:, 256:640].rearrange("p (g r d) -> p g r d", r=2, d=64)
                        rope(Rk[:, :, 0, :].rearrange("p g (two h) -> p g two h", two=2), v4(b[0][:, 256:448], 3, 32), 3, 32, cs, tC, tS, [bk[0]], ["R"])
                        cp("pool", Rk[:, :, 1, :], Rk[:, :, 0, :], ["R"], ["R"])
                        cp("dve", R[:, 640:768].rearrange("p (r d) -> p r d", r=2), b[0][:, 448:512].unsqueeze(1).to_broadcast([128, 2, 64]), [bk[0]], ["R"])
                        cp("dve", VS[:, i, :, 0:64], b[1][:, 0:128].rearrange("p (g d) -> p g d", g=2), [bk[1]], ["VS"])
                        act(GT[:, i, :], b[1][:, 128:140], AF.Sigmoid, [bk[1]], ["GT"])
                        p_ = pT[i % 2]
                        for k in range(6):
                            tr(p_[:, k, :], R[:, k * 128:(k + 1) * 128], identb[:], ["R", "identb"], [("pTn", i % 2)])
                        cp("act", NA[:, :, i * 128:(i + 1) * 128], p_[:], [("pTn", i % 2)], [("na", i)])
                    P.barrier()
                NAR = [("na", i) for i in range(NT)]
                with ExitStack() as st:
                    ACC, SC, MS, PEX, wk = attn_scope(st)
                    WO = load_wout(st, l, 0)
                    KCMP = sbuf(st, "KCMP", [128, 128], BF16)
                    VC1 = sbuf(st, "VC1", [128, 97], BF16)
                    CM = sbuf(st, "CM", [128, S], BF16)
                    SELM = sbuf(st, "SELM", [128, NT, 3, 32], F32)
                    EXP = sbuf(st, "EXP", [32, S], BF16)
                    selT = sbuf(st, "selT", [32, 512], BF16)
                    P.dma(SELM[:], dr["selM"].rearrange("(i p) c j -> p i c j", p=128), writes=["SELM"])
                    with ExitStack() as sp_:
                        wstg = sbuf(sp_, "wstg", [64, 8, 128], F32)
                        WKb = sbuf(sp_, "WKb", [64, 32, 128], BF16)
                        WVb = sbuf(sp_, "WVb", [64, 32, 64], BF16)
                        posf = sbuf(sp_, "posf", [64, 2, 32], F32)
                        posb = sbuf(sp_, "posb", [64, 2, 32], BF16)
                        ovf = sbuf(sp_, "ovf", [128, 33], F32)
                        kb = sbuf(sp_, "kb", [128, 1], F32)
                        vb = sbuf(sp_, "vb", [1, 64], BF16)
                        CMf = sbuf(sp_, "CMf", [128, 512], F32)
                        EXs = sbuf(sp_, "EXs", [32, 512], F32)
                        wk3 = dr["wkD"][l].rearrange("(j d) o -> d j o", d=64)
                        wv3 = dr["wv"][l].rearrange("(j d) o -> d j o", d=64)
                        for c in range(4):
                            P.dma(wstg[:], wk3[:, 8 * c:8 * c + 8, :], writes=["wstg"])
                            cp("act", WKb[:, 8 * c:8 * c + 8, :], wstg[:], ["wstg"], ["WKb"])
                        for c in range(4):
                            P.dma(wstg[:, :, 0:64], wv3[:, 8 * c:8 * c + 8, :], writes=["wstg"])
                            cp("act", WVb[:, 8 * c:8 * c + 8, :], wstg[:, :, 0:64], ["wstg"], ["WVb"])
                        P.dma(posf[:, 0, :], dr["poskT"][l], writes=["posf"])
                        P.dma(posf[:, 1, :], dr["posvT"][l], writes=["posf"])
                        P.dma(ovf[:], dr["ovl1"], writes=["ovf"])
                        for c in range(4):
                            P.dma(CMf[:], dr["cmaskT"][:, c * 512:(c + 1) * 512], writes=["CMf"])
                            cp("pool", CM[:, c * 512:(c + 1) * 512], CMf[:], ["CMf"], ["CM"])
                            P.dma(EXs[:], dr["expand"][:, c * 512:(c + 1) * 512], writes=["EXs"])
                            cp("pool", EXP[:, c * 512:(c + 1) * 512], EXs[:], ["EXs"], ["EXP"])
                        cp("dve", posb[:], posf[:], ["posf"], ["posb"])
                        memset("pool", VC1[:], 0.0, ["VC1"])
                        memset("pool", KCMP[:], 0.0, ["KCMP"])
                        cp("dve", VC1[:, 64:97], ovf[:], ["ovf", "VC1"], ["VC1"])
                        for j in range(32):
                            mm(MS[0][:, 0:1], WKb[0:64, j, :], posb[0:64, 0, j:j + 1], j == 0, j == 31, ["WKb", "posb"], [("ms", 0)])
                        cp("dve", kb[:], MS[0][:, 0:1], [("ms", 0)], ["kb"])
                        for j in range(32):
                            mm(MS[1][0:1, 0:64], posb[0:64, 1, j:j + 1], WVb[0:64, j, :], j == 0, j == 31, ["WVb", "posb"], [("ms", 1)])
                        cp("dve", vb[:], MS[1][0:1, 0:64], [("ms", 1)], ["vb"])
                        for j in range(32):
                            mm(SC[0][:, 0:127], WKb[0:64, j, :], NA[0:64, 2, j:j + 16 * 126 + 1:16], j == 0, j == 31, ["WKb"] + NAR, [("sc", 0)])
                        ts("dve", KCMP[:, 0:127], SC[0][:, 0:127], kb[:, 0:1], None, ALU.add, None, [("sc", 0), "kb", "KCMP"], ["KCMP"])
                        for j in range(32):
                            mm(SC[1][0:127, 0:64], NA[0:64, 5, j:j + 16 * 126 + 1:16], WVb[0:64, j, :], j == 0, False, ["WVb"] + NAR, [("sc", 1)])
                        mm(SC[1][0:127, 0:64], onesb[0:1, 0:127], vb[0:1, :], False, True, ["onesb", "vb"], [("sc", 1)])
                        cp("dve", VC1[0:127, 0:64], SC[1][0:127, 0:64], [("sc", 1), "VC1"], ["VC1"])
                        P.barrier()
                    MK = [sbuf(st, "MK%d" % j, [128, 512], BF16) for j in range(2)]
                    O = sbuf(st, "O", [128, 4, 256], F32)
                    sm_ = sbuf(st, "nsa_small", [128, 256], F32)
                    impw = sbuf(st, "impw", [128, 4, 32], F32)
                    imr = sbuf(st, "imr", [128, 32], F32)
                    selb = sbuf(st, "selb", [128, 32], BF16)
                    PC = [sbuf(st, "PC%d" % j, [128, 512], BF16) for j in range(4)]
                    scale = 64 ** -0.5
                    for qc in range(4):
                        q0 = qc * 512
                        pcs = []
                        for h in range(4):
                            hp, blk = 64 * (h % 2), h // 2
                            sc = SC[h % 2]
                            mm(sc[0:127, :], KCMP[hp:hp + 64, 0:127], NA[hp:hp + 64, blk, q0:q0 + 512], True, True, ["KCMP"] + NAR, [("sc", h % 2)])
                            pc = PC[h]
                            act(pc[0:127, :], sc[0:127, :], AF.Exp, [("sc", h % 2)], [("pc", h)], scale=scale)
                            tt("dve", pc[0:127, :], pc[0:127, :], CM[0:127, q0:q0 + 512], ALU.mult, [("pc", h), "CM"], [("pc", h)])
                            pcs.append(pc)
                        for j in range(4):
                            i = 4 * qc + j
                            psc = MS[j % 2]
                            pck = ("ms", j % 2)
                            ps3 = psc[:, 0:388].rearrange("p (h c) -> p h c", h=4)
                            for h in range(4):
                                mm(ps3[:, h, :], pcs[h][0:127, j * 128:(j + 1) * 128], VC1[0:127, :], True, True, [("pc", h), "VC1"], [pck])
                            den = sm_[:, 0:4]
                            g1 = sm_[:, 4:8]
                            ts("dve", den, ps3[:, :, 64], 1e-30, None, ALU.max, None, [pck], ["nsm"])
                            recip(den, den, ["nsm"], ["nsm"])
                            tt("dve", g1, den, GT[:, i, 0:12:3], ALU.mult, ["nsm", "GT"], ["nsm"])
                            tt("dve", O[:, j, :].rearrange("p (h d) -> p h d", h=4), ps3[:, :, 0:64],
                               g1.unsqueeze(2).to_broadcast([128, 4, 64]), ALU.mult, [pck, "nsm"], ["O"])
                            tt("dve", impw[:], ps3[:, :, 65:97], den.unsqueeze(2).to_broadcast([128, 4, 32]), ALU.mult, [pck, "nsm"], ["impw"])
                            red(imr[:], impw[:].rearrange("p h j -> p j h"), ALU.add, ["impw"], ["imr"])
                            tt("dve", imr[:], imr[:], SELM[:, i, 0, :], ALU.mult, ["imr", "SELM"], ["imr"])
                            tt("dve", imr[:], imr[:], SELM[:, i, 1, :], ALU.add, ["imr", "SELM"], ["imr"])
                            m8 = sm_[:, 8:24]
                            imr2 = sm_[:, 32:64]
                            P.op("dve", lambda e, m8=m8: e.max(m8[:, 0:8], imr[:]), reads=["imr"], writes=["nsm"])
                            P.op("dve", lambda e, m8=m8, imr2=imr2: e.match_replace(imr2, m8[:, 0:8], imr[:], -3.0e38), reads=["imr", "nsm"], writes=["nsm2"])
                            P.op("dve", lambda e, m8=m8, imr2=imr2: e.max(m8[:, 8:16], imr2), reads=["nsm2"], writes=["nsm"])
                            stt(selb[:], imr[:], m8[:, 15:16], SELM[:, i, 2, :], ALU.is_ge, ALU.mult, ["imr", "nsm", "SELM"], ["selb"])
                            pst = MS[j % 2][:, 256:384].bitcast(BF16)
                            tr(pst[0:32, 0:128], selb[:], identb[:], ["selb", "identb"], [pck])
                            cp("act", selT[:, j * 128:(j + 1) * 128], pst[0:32, 0:128], [pck], ["selT"])

                        def smask(kt, c0, n, j0, qc=qc, q0=q0):
                            mk = MK[kt % 2]
                            mkk = ("mk", kt % 2)
                            pm_ = MS[kt % 2]
                            mm(pm_[:, 0:n], EXP[0:32, kt * 128:(kt + 1) * 128], selT[0:32, c0 - q0:c0 - q0 + n], True, True, ["EXP", "selT"], [("ms", kt % 2)])
                            cp("act", mk[:, 0:n], pm_[:, 0:n], [("ms", kt % 2)], [mkk])
                            if kt >= 4 * qc:
                                tt("pool", mk[:, 0:128], mk[:, 0:128], tri[:, 0, :], ALU.mult, [mkk, "tri"], [mkk])
                            return (mk[:, 0:n], mkk)

                        units = []
                        for h in range(4):
                            hp, blk = 64 * (h % 2), h // 2
                            units.append(dict(
                                parts=[((lambda kt, hp=hp: NA[hp:hp + 64, 3, kt * 128:(kt + 1) * 128]),
                                        (lambda c0, n, hp=hp, blk=blk: NA[hp:hp + 64, blk, c0:c0 + n]))],
                                vf=(lambda kt: VS[:, kt, 0, :]), R=NAR + ["VS"]))
                        flash(qc, units, causal, causal_edge, scale, ACC, SC, PEX, shared_mask=smask)
                        nsa_fin(qc, ACC, GT, O, sm_, 1)
                        kr, ed = window(4)
                        units = []
                        for h in range(4):
                            hp, blk = 64 * (h % 2), h // 2
                            units.append(dict(
                                parts=[((lambda kt, hp=hp: NA[hp:hp + 64, 4, kt * 128:(kt + 1) * 128]),
                                        (lambda c0, n, hp=hp, blk=blk: NA[hp:hp + 64, blk, c0:c0 + n]))],
                                vf=(lambda kt: VS[:, kt, 1, :]), R=NAR + ["VS"]))
                        flash(qc, units, kr, ed, scale, ACC, SC, PEX)
                        nsa_fin(qc, ACC, GT, O, sm_, 2)
                        if dbg == ("o", 0):
                            for j in range(4):
                                P.dma(out_d[(4 * qc + j) * 128:(4 * qc + j + 1) * 128, 0:256], O[:, j, :], reads=["O"], is_output=True)
                        apply_wout(qc, O, "O", WO, SC, MS, wk)
                    P.barrier()

        def nsa_fin(qc, ACC, GT, O, sm_, gi):
            for h in range(4):
                a3 = ACC[h][:, 0:260].rearrange("p (j c) -> p j c", j=4)
                rd = sm_[:, 64 + 4 * h:68 + 4 * h]
                recip(rd, a3[:, :, 64], [("acc", h)], [("rd", h)])
                tt("dve", rd, rd, GT[:, 4 * qc:4 * qc + 4, 3 * h + gi], ALU.mult, [("rd", h), "GT"], [("rd", h)])
                for j in range(4):
                    stt(O[:, j, h * 64:(h + 1) * 64], a3[:, j, 0:64], rd[:, j:j + 1], O[:, j, h * 64:(h + 1) * 64], ALU.mult, ALU.add,
                        [("acc", h), ("rd", h), "O"], ["O"])

        def diff_mixer(l, HT, CS32):
            lam_init = 0.8 - 0.6 * math.exp(-0.3 * l)
            with ExitStack() as sa:
                DA = sbuf(sa, "DA", [128, 6, S], BF16)
                VD = sbuf(sa, "VD", [128, NT, 4, 65], BF16)
                memset("pool", VD[:], 1.0, ["VD"])
                with ExitStack() as st:
                    W = sbuf(st, "W", [128, 8, 768], BF16)
                    stg = [sbuf(st, "stg%d" % j, [128, 8, 256], F32) for j in range(2)]
                    tC = sbuf(st, "tC", [128, 512], F32)
                    tS = sbuf(st, "tS", [128, 512], F32)
                    R = sbuf(st, "R", [128, 768], BF16)
                    RT = sbuf(st, "RT", [128, 512], BF16)
                    memset("pool", R[:], 0.0, ["R"])
                    pb = [psum(st, "pb%d" % j, [128, 512], F32) for j in range(4)]
                    pT = [psum_tb(st, "pTn%d" % j)[:, 0:6, :] for j in range(2)]
                    load_w_cols(W, "W", dr["w_in"][l][:, DIF0:DIF0 + 768], 768, stg, "stg")
                    cols_for_tile(0, HT, W, 768, [pb[0], pb[1]], [("pb", 0), ("pb", 1)])
                    for i in range(NT):
                        b = [pb[(2 * i) % 4], pb[(2 * i + 1) % 4]]
                        bk = [("pb", (2 * i) % 4), ("pb", (2 * i + 1) % 4)]
                        if i + 1 < NT:
                            cols_for_tile(i + 1, HT, W, 768, [pb[(2 * i + 2) % 4], pb[(2 * i + 3) % 4]], [("pb", (2 * i + 2) % 4), ("pb", (2 * i + 3) % 4)])
                        rope(v4(RT[:, 0:512], 16, 16), v4(b[0][:, 0:512], 16, 16), 16, 16, CS32[:, i, :, :], tC, tS, [bk[0]], ["RT"])
                        for side in range(2):
                            for gb_ in range(3):
                                nu = 3 if gb_ < 2 else 2
                                cp("pool" if gb_ % 2 == 0 else "act", R[:, (3 * side + gb_) * 128:(3 * side + gb_) * 128 + 32 * nu],
                                   RT[:, side * 256 + gb_ * 96:side * 256 + gb_ * 96 + 32 * nu], ["RT"], ["R"])
                        cp("dve", VD[:, i, :, 0:64], b[1][:, 0:256].rearrange("p (g d) -> p g d", g=4), [bk[1]], ["VD"])
                        p_ = pT[i % 2]
                        for k in range(6):
                            tr(p_[:, k, :], R[:, k * 128:(k + 1) * 128], identb[:], ["R", "identb"], [("pTn", i % 2)])
                        cp("act", DA[:, :, i * 128:(i + 1) * 128], p_[:], [("pTn", i % 2)], [("da", i)])
                    P.barrier()
                DAR = [("da", i) for i in range(NT)]
                with ExitStack() as st:
                    ACC, SC, MS, PEX, wk = attn_scope(st)
                    WO = load_wout(st, l, 1)
                    O = sbuf(st, "O", [128, 4, 256], F32)
                    O2 = sbuf(st, "O2", [128, 4, 256], F32)
                    lamt = sbuf(st, "lamt", [128, 4, 32], F32)
                    sgb = sbuf(st, "sgb", [128, 64], F32)
                    sm_ = sbuf(st, "dsm", [128, 64], F32)
                    P.dma(lamt[:].rearrange("p a b -> p (a b)"), dr["lam4"][l:l + 1].rearrange("o a b -> o (a b)").to_broadcast([128, 128]), writes=["lamt"])
                    P.dma(sgb[:], dr["sub_g"][l:l + 1, :].to_broadcast([128, 64]), writes=["sgb"])
                    tt("dve", lamt[:, 0, :], lamt[:, 0, :], lamt[:, 1, :], ALU.mult, ["lamt"], ["lamt"])
                    tt("dve", lamt[:, 2, :], lamt[:, 2, :], lamt[:, 3, :], ALU.mult, ["lamt"], ["lamt"])
                    red(sm_[:, 0:1], lamt[:, 0, :], ALU.add, ["lamt"], ["dl"])
                    red(sm_[:, 1:2], lamt[:, 2, :], ALU.add, ["lamt"], ["dl"])
                    act(sm_[:, 0:2], sm_[:, 0:2], AF.Exp, ["dl"], ["dl"])
                    stt(sm_[:, 0:1], sm_[:, 1:2], -lam_init, sm_[:, 0:1], ALU.add, ALU.subtract, ["dl"], ["dl"])
                    ts("dve", sgb[:], sgb[:], 1.0 - lam_init, None, ALU.mult, None, ["sgb"], ["sgb"])
                    scale = 32 ** -0.5
                    for qc in range(4):
                        for grp in range(2):
                            units = []
                            for uu in range(4):
                                u = grp * 4 + uu
                                blk, po = u // 3, 32 * (u % 3)
                                h = u // 2
                                units.append(dict(
                                    parts=[((lambda kt, po=po, blk=blk: DA[po:po + 32, 3 + blk, kt * 128:(kt + 1) * 128]),
                                            (lambda c0, n, po=po, blk=blk: DA[po:po + 32, blk, c0:c0 + n]))],
                                    vf=(lambda kt, h=h: VD[:, kt, h, :]), R=DAR + ["VD"]))
                            flash(qc, units, causal, causal_edge, scale, ACC, SC, PEX)
                            for uu in range(4):
                                u = grp * 4 + uu
                                h, c = u // 2, u % 2
                                a3 = ACC[uu][:, 0:260].rearrange("p (j c) -> p j c", j=4)
                                rd = sm_[:, 8 + 4 * uu:12 + 4 * uu]
                                recip(rd, a3[:, :, 64], [("acc", uu)], [("rd", uu)])
                                if c == 1:
                                    ts("dve", rd, rd, sm_[:, 0:1], None, ALU.mult, None, [("rd", uu), "dl"], [("rd", uu)])
                                dst = (O if c == 0 else O2)[:, :, h * 64:(h + 1) * 64]
                                tt("dve", dst, a3[:, :, 0:64], rd.unsqueeze(2).to_broadcast([128, 4, 64]), ALU.mult,
                                   [("acc", uu), ("rd", uu)], ["O" if c == 0 else "O2"])
                        tt("pool", O[:], O[:], O2[:], ALU.add, ["O", "O2"], ["O"])
                        tt("pool", O2[:], O[:], O[:], ALU.mult, ["O"], ["O2"])
                        ss16 = sm_[:, 32:48]
                        red(ss16, O2[:].rearrange("p j (h d) -> p (j h) d", h=4), ALU.add, ["O2"], ["ss16"])
                        act(ss16, ss16, AF.Sqrt, ["ss16", "epsc"], ["ss16"], bias=epsc[:], scale=1.0 / 64)
                        recip(ss16, ss16, ["ss16"], ["ss16"])
                        O3 = O[:].rearrange("p j (h d) -> p (j h) d", h=4)
                        tt("dve", O3, O3, ss16.unsqueeze(2).to_broadcast([128, 16, 64]), ALU.mult, ["O", "ss16"], ["O"])
                        tt("dve", O3, O3, sgb[:].unsqueeze(1).to_broadcast([128, 16, 64]), ALU.mult, ["O", "sgb"], ["O"])
                        if dbg == ("o", 1):
                            for j in range(4):
                                P.dma(out_d[(4 * qc + j) * 128:(4 * qc + j + 1) * 128, 0:256], O[:, j, :], reads=["O"], is_output=True)
                        apply_wout(qc, O, "O", WO, SC, MS, wk)
                    P.barrier()

        def mla_mixer(l, HT, CS32):
            with ExitStack() as sa:
                MA = sbuf(sa, "MA", [128, 7, S], BF16)
                VM = sbuf(sa, "VM", [128, NT, 4, 65], BF16)
                memset("pool", VM[:], 1.0, ["VM"])
                with ExitStack() as st:
                    W = sbuf(st, "W", [128, 8, 416], BF16)
                    stg = [sbuf(st, "stg%d" % j, [128, 8, 256], F32) for j in range(2)]
                    tC = sbuf(st, "tC", [128, 512], F32)
                    tS = sbuf(st, "tS", [128, 512], F32)
                    R = sbuf(st, "R", [128, 896], BF16)
                    memset("pool", R[:], 0.0, ["R"])
                    WUQ = sbuf(st, "WUQ", [128, 2, 384], BF16)
                    WUKV = sbuf(st, "WUKV", [128, 512], BF16)
                    wst = sbuf(st, "wst", [128, 2, 512], F32)
                    qgb = sbuf(st, "qgb", [128, 384], F32)
                    cqn = sbuf(st, "cqn", [128, 384], BF16)
                    CT = sbuf(st, "CT", [128, 3, 128], BF16)
                    junk = sbuf(st, "junk", [128, 256], BF16)
                    kr = sbuf(st, "kr", [128, 32], BF16)
                    msm = sbuf(st, "msm", [128, 8], F32)
                    pb = [psum(st, "pb%d" % j, [128, 512], F32) for j in range(2)]
                    pu = [psum(st, "pu%d" % j, [128, 512], F32) for j in range(4)]
                    pT = [psum_tb(st, "pTn%d" % j)[:, 0:7, :] for j in range(2)]
                    load_w_cols(W, "W", dr["w_in"][l][:, MLA0:MLA0 + 416], 416, stg, "stg")
                    P.dma(wst[:, :, 0:384], dr["mla_w_uq"][l].rearrange("(k p) n -> p k n", p=128), writes=["wst"])
                    cp("act", WUQ[:], wst[:, :, 0:384], ["wst"], ["WUQ"])
                    P.dma(wst[:, 0, :], dr["mla_w_ukv"][l], writes=["wst"])
                    cp("act", WUKV[:], wst[:, 0, :], ["wst"], ["WUKV"])
                    P.dma(qgb[:, 0:256], dr["mla_qg"][l:l + 1, :].to_broadcast([128, 256]), writes=["qgb"])
                    P.dma(qgb[:, 256:384], dr["mla_kvg"][l:l + 1, :].to_broadcast([128, 128]), writes=["qgb"])
                    cols_for_tile(0, HT, W, 416, [pb[0]], [("pb", 0)])
                    for i in range(NT):
                        b = pb[i % 2]
                        bk = ("pb", i % 2)
                        if i + 1 < NT:
                            cols_for_tile(i + 1, HT, W, 416, [pb[(i + 1) % 2]], [("pb", (i + 1) % 2)])
                        act(junk[:, 0:256], b[:, 0:256], AF.Square, [bk], ["junk", "msm"], accum=msm[:, 0:1])
                        act(junk[:, 0:128], b[:, 256:384], AF.Square, [bk], ["junk", "msm"], accum=msm[:, 1:2])
                        act(msm[:, 0:1], msm[:, 0:1], AF.Sqrt, ["msm", "epsc"], ["msm"], bias=epsc[:], scale=1.0 / 256)
                        act(msm[:, 1:2], msm[:, 1:2], AF.Sqrt, ["msm", "epsc"], ["msm"], bias=epsc[:], scale=1.0 / 128)
                        recip(msm[:, 0:2], msm[:, 0:2], ["msm"], ["msm"])
                        stt(cqn[:, 0:256], b[:, 0:256], msm[:, 0:1], qgb[:, 0:256], ALU.mult, ALU.mult, [bk, "msm", "qgb"], ["cqn"])
                        stt(cqn[:, 256:384], b[:, 256:384], msm[:, 1:2], qgb[:, 256:384], ALU.mult, ALU.mult, [bk, "msm", "qgb"], ["cqn"])
                        p_ = pT[i % 2]
                        pk = ("pTn", i % 2)
                        for k in range(3):
                            tr(p_[:, k, :], cqn[:, k * 128:(k + 1) * 128], identb[:], ["cqn", "identb"], [pk])
                        cp("act", CT[:], p_[:, 0:3, :], [pk], ["CT"])
                        pq = pu[(2 * i) % 4]
                        pkv = pu[(2 * i + 1) % 4]
                        pqk, pkvk = ("pu", (2 * i) % 4), ("pu", (2 * i + 1) % 4)
                        mm(pq[:, 0:384], CT[:, 0, :], WUQ[:, 0, :], True, False, ["CT", "WUQ"], [pqk])
                        mm(pq[:, 0:384], CT[:, 1, :], WUQ[:, 1, :], False, True, ["CT", "WUQ"], [pqk])
                        mm(pkv[:, 0:512], CT[:, 2, :], WUKV[:], True, True, ["CT", "WUKV"], [pkvk])
                        q3 = pq[:, 0:384].rearrange("p (g x) -> p g x", x=96)
                        kv3 = pkv[:, 0:512].rearrange("p (g x) -> p g x", x=128)
                        cs = CS32[:, i, :, :]
                        cp("dve", R[:, 0:256].rearrange("p (g d) -> p g d", g=4), q3[:, :, 0:64], [pqk], ["R"])
                        rope(v4(R[:, 256:352], 3, 16), q3[:, 0:3, 64:96].rearrange("p g (two h) -> p g two h", two=2), 3, 16, cs, tC, tS, [pqk], ["R"])
                        rope(v4(R[:, 768:800], 1, 16), q3[:, 3:4, 64:96].rearrange("p g (two h) -> p g two h", two=2), 1, 16, cs, tC, tS, [pqk], ["R"])
                        cp("dve", R[:, 384:640].rearrange("p (g d) -> p g d", g=4), kv3[:, :, 0:64], [pkvk], ["R"])
                        rope(v4(kr[:, 0:32], 1, 16), v4(b[:, 384:416], 1, 16), 1, 16, cs, tC, tS, [bk], ["kr"])
                        cp("pool", R[:, 640:768].rearrange("p (g d) -> p g d", g=4), kr[:].unsqueeze(1).to_broadcast([128, 4, 32]), ["kr"], ["R"])
                        cp("dve", VM[:, i, :, 0:64], kv3[:, :, 64:128], [pkvk], ["VM"])
                        for k in range(7):
                            tr(p_[:, k, :], R[:, k * 128:(k + 1) * 128], identb[:], ["R", "identb"], [pk])
                        cp("act", MA[:, :, i * 128:(i + 1) * 128], p_[:], [pk], [("ma", i)])
                    P.barrier()
                MAR = [("ma", i) for i in range(NT)]
                with ExitStack() as st:
                    ACC, SC, MS, PEX, wk = attn_scope(st)
                    WO = load_wout(st, l, 2)
                    O = sbuf(st, "O", [128, 4, 256], F32)
                    sm_ = sbuf(st, "msm2", [128, 16], F32)
                    scale = 96 ** -0.5
                    for qc in range(4):
                        units = []
                        for h in range(4):
                            hp, blk = 64 * (h % 2), h // 2
                            units.append(dict(
                                parts=[((lambda kt, hp=hp, blk=blk: MA[hp:hp + 64, 3 + blk, kt * 128:(kt + 1) * 128]),
                                        (lambda c0, n, hp=hp, blk=blk: MA[hp:hp + 64, blk, c0:c0 + n])),
                                       ((lambda kt, h=h: MA[32 * (h % 3):32 * (h % 3) + 32, 5, kt * 128:(kt + 1) * 128]),
                                        (lambda c0, n, h=h: MA[32 * (h % 3):32 * (h % 3) + 32, 2 if h < 3 else 6, c0:c0 + n]))],
                                vf=(lambda kt, h=h: VM[:, kt, h, :]), R=MAR + ["VM"]))
                        flash(qc, units, causal, causal_edge, scale, ACC, SC, PEX)
                        for h in range(4):
                            a3 = ACC[h][:, 0:260].rearrange("p (j c) -> p j c", j=4)
                            rd = sm_[:, 4 * h:4 * h + 4]
                            recip(rd, a3[:, :, 64], [("acc", h)], [("rd", h)])
                            tt("dve", O[:, :, h * 64:(h + 1) * 64], a3[:, :, 0:64], rd.unsqueeze(2).to_broadcast([128, 4, 64]), ALU.mult,
                               [("acc", h), ("rd", h)], ["O"])
                        if dbg == ("o", 2):
                            for j in range(4):
                                P.dma(out_d[(4 * qc + j) * 128:(4 * qc + j + 1) * 128, 0:256], O[:, j, :], reads=["O"], is_output=True)
                        apply_wout(qc, O, "O", WO, SC, MS, wk)
                    P.barrier()

        def swa_mixer(l, HT, CS64):
            with ExitStack() as sa:
                SA = sbuf(sa, "SA", [128, 4, S], BF16)
                VW = sbuf(sa, "VW", [128, NT, 2, 65], BF16)
                memset("pool", VW[:], 1.0, ["VW"])
                with ExitStack() as st:
                    W = sbuf(st, "W", [128, 8, 512], BF16)
                    stg = [sbuf(st, "stg%d" % j, [128, 8, 256], F32) for j in range(2)]
                    tC = sbuf(st, "tC", [128, 512], F32)
                    tS = sbuf(st, "tS", [128, 512], F32)
                    R = sbuf(st, "R", [128, 512], BF16)
                    pb = [psum(st, "pb%d" % j, [128, 512], F32) for j in range(2)]
                    pT = [psum_tb(st, "pTn%d" % j)[:, 0:4, :] for j in range(2)]
                    load_w_cols(W, "W", dr["w_in"][l][:, SWA0:SWA0 + 512], 512, stg, "stg")
                    cols_for_tile(0, HT, W, 512, [pb[0]], [("pb", 0)])
                    for i in range(NT):
                        b = pb[i % 2]
                        bk = ("pb", i % 2)
                        if i + 1 < NT:
                            cols_for_tile(i + 1, HT, W, 512, [pb[(i + 1) % 2]], [("pb", (i + 1) % 2)])
                        cs = CS64[:, i, :, :]
                        rope(v4(R[:, 0:256], 4, 32), v4(b[:, 0:256], 4, 32), 4, 32, cs, tC, tS, [bk], ["R"])
                        Rk = R[:, 256:512].rearrange("p (g r d) -> p g r d", r=2, d=64)
                        rope(Rk[:, :, 0, :].rearrange("p g (two h) -> p g two h", two=2), v4(b[:, 256:384], 2, 32), 2, 32, cs, tC, tS, [bk], ["R"])
                        cp("pool", Rk[:, :, 1, :], Rk[:, :, 0, :], ["R"], ["R"])
                        cp("dve", VW[:, i, :, 0:64], b[:, 384:512].rearrange("p (g d) -> p g d", g=2), [bk], ["VW"])
                        p_ = pT[i % 2]
                        for k in range(4):
                            tr(p_[:, k, :], R[:, k * 128:(k + 1) * 128], identb[:], ["R", "identb"], [("pTn", i % 2)])
                        cp("act", SA[:, :, i * 128:(i + 1) * 128], p_[:], [("pTn", i % 2)], [("sa", i)])
                    P.barrier()
                SAR = [("sa", i) for i in range(NT)]
                with ExitStack() as st:
                    ACC, SC, MS, PEX, wk = attn_scope(st)
                    WO = load_wout(st, l, 3)
                    O = sbuf(st, "O", [128, 4, 256], F32)
                    sm_ = sbuf(st, "ssm", [128, 16], F32)
                    esk = sbuf(st, "esk", [128, 4], F32)
                    P.dma(esk[:], dr["swa_sinks"][l:l + 1, :].to_broadcast([128, 4]), writes=["esk"])
                    act(esk[:], esk[:], AF.Exp, ["esk"], ["esk"])
                    scale = 64 ** -0.5
                    kr, ed = window(1)
                    for qc in range(4):
                        units = []
                        for h in range(4):
                            hp, blk = 64 * (h % 2), h // 2
                            units.append(dict(
                                parts=[((lambda kt, hp=hp, blk=blk: SA[hp:hp + 64, 2 + blk, kt * 128:(kt + 1) * 128]),
                                        (lambda c0, n, hp=hp, blk=blk: SA[hp:hp + 64, blk, c0:c0 + n]))],
                                vf=(lambda kt, g=h // 2: VW[:, kt, g, :]), R=SAR + ["VW"]))
                        flash(qc, units, kr, ed, scale, ACC, SC, PEX)
                        for h in range(4):
                            a3 = ACC[h][:, 0:260].rearrange("p (j c) -> p j c", j=4)
                            rd = sm_[:, 4 * h:4 * h + 4]
                            ts("dve", rd, a3[:, :, 64], esk[:, h:h + 1], None, ALU.add, None, [("acc", h), "esk"], [("rd", h)])
                            recip(rd, rd, [("rd", h)], [("rd", h)])
                            tt("dve", O[:, :, h * 64:(h + 1) * 64], a3[:, :, 0:64], rd.unsqueeze(2).to_broadcast([128, 4, 64]), ALU.mult,
                               [("acc", h), ("rd", h)], ["O"])
                        if dbg == ("o", 3):
                            for j in range(4):
                                P.dma(out_d[(4 * qc + j) * 128:(4 * qc + j + 1) * 128, 0:256], O[:, j, :], reads=["O"], is_output=True)
                        apply_wout(qc, O, "O", WO, SC, MS, wk)
                    P.barrier()

        def ffn_phase(l):
            load_mod(l, 1)
            with ExitStack() as sf:
                ST = sbuf(sf, "ST", [128, 4, 8, 4, 128], BF16)
                LNR = sbuf(sf, "LNR", [128, 4, 8], F32)
                H2T = sbuf(sf, "H2T", [128, 8, 512], BF16)
                SKs = sbuf(sf, "SKs", [128, 2, 128], F32)
                SK = sbuf(sf, "SK", [128, 2, 128], BF16)
                P.dma(SKs[:], dr["skT"][l].rearrange("c d k -> d c k"), writes=["SKs"])
                cp("dve", SK[:], SKs[:], ["SKs"], ["SK"])
                for g in range(4):
                    with ExitStack() as st:
                        junk = sbuf(st, "junk", [128, D], BF16)
                        t1 = sbuf(st, "t1", [128, D], F32)
                        hb = sbuf(st, "hb", [128, D], BF16)
                        pT = psum_tb(st, "pT")
                        QP = sbuf(st, "QP", [128, 16, 512], BF16)
                        wqs = [sbuf(st, "wqs%d" % j, [128, 8, 128], F32) for j in range(2)]
                        wqb = [sbuf(st, "wqb%d" % j, [128, 8, 128], BF16) for j in range(2)]
                        SSs = sbuf(st, "SSs", [128, 16, 128], F32)
                        pq = [psum(st, "pq%d" % j, [128, 512], F32) for j in range(2)]
                        pss = [psum(st, "pss%d" % j, [128, 512], F32) for j in range(4)]
                        m1 = sbuf(st, "m1", [128, 16], F32)
                        m2 = sbuf(st, "m2", [128, 16], F32)
                        mc = sbuf(st, "mc", [128, 16], F32)
                        rr = sbuf(st, "rr", [128, 256], F32)
                        cand = sbuf(st, "cand", [128, 256], F32)
                        sm_ = sbuf(st, "psm", [128, 32], F32)
                        am = sbuf(st, "am", [128, 128], F32)
                        sfl = sbuf(st, "sfl", [128, 2, 128], F32)
                        shl = sbuf(st, "shl", [128, 4, 128], BF16)
                        pst_ = psum_tb(st, "pst")
                        for tt_ in range(4):
                            norm_to_T(4 * g + tt_, H2T, tt_ * 128, ("h2t", tt_), (junk, t1, hb, pT))
                        H2R = [("h2t", j) for j in range(4)]
                        for n in range(16):
                            s_ = wqs[n % 2]
                            P.dma(s_[:], dr["peer_w_q"][l][:, n * 128:(n + 1) * 128].rearrange("(k p) n -> p k n", p=128), writes=[("wqs", n % 2)])
                            cp("pool", wqb[n % 2][:], s_[:], [("wqs", n % 2)], [("wqb", n % 2)])
                            for k in range(8):
                                mm(pq[n % 2][:], wqb[n % 2][:, k, :], H2T[:, k, :], k == 0, k == 7, [("wqb", n % 2)] + H2R, [("pq", n % 2)])
                            cp("act", QP[:, n, :], pq[n % 2][:], [("pq", n % 2)], [("qp", n)])
                        for tt_ in range(4):
                            for n in range(16):
                                b = pss[n // 4]
                                mm(b[:, (n % 4) * 128:(n % 4 + 1) * 128], QP[:, n, tt_ * 128:(tt_ + 1) * 128], SK[:, n % 2, :], True, True,
                                   [("qp", n), "SK"], [("pss", n // 4)])
                            for q4 in range(4):
                                cp("act", SSs[:, 4 * q4:4 * q4 + 4, :], pss[q4][:].rearrange("p (a b) -> p a b", a=4), [("pss", q4)], [("sss", q4)])
                            for h in range(8):
                                s1 = SSs[:, 2 * h, :]
                                s2 = SSs[:, 2 * h + 1, :]
                                sk_ = [("sss", h // 2)]
                                for (sx, mx) in ((s1, m1), (s2, m2)):
                                    P.op("dve", lambda e, sx=sx, mx=mx: e.max(mx[:, 0:8], sx), reads=sk_, writes=["mx"])
                                    P.op("dve", lambda e, sx=sx, mx=mx: e.match_replace(rr[:, 0:128], mx[:, 0:8], sx, -1.0e30), reads=sk_ + ["mx"], writes=["rr"])
                                    P.op("dve", lambda e, mx=mx: e.max(mx[:, 8:16], rr[:, 0:128]), reads=["rr"], writes=["mx"])
                                c3 = cand[:].rearrange("p (a b) -> p a b", a=16)
                                tt("dve", c3, m1[:].unsqueeze(2).to_broadcast([128, 16, 16]), m2[:].unsqueeze(1).to_broadcast([128, 16, 16]), ALU.add,
                                   ["mx"], ["cand"])
                                P.op("dve", lambda e: e.max(mc[:, 0:8], cand[:]), reads=["cand"], writes=["mc"])
                                P.op("dve", lambda e: e.match_replace(rr[:], mc[:, 0:8], cand[:], -1.0e30), reads=["cand", "mc"], writes=["rr"])
                                P.op("dve", lambda e: e.max(mc[:, 8:16], rr[:]), reads=["rr"], writes=["mc"])
                                negM = sm_[:, 0:1]
                                Z = sm_[:, 1:2]
                                nthr = sm_[:, 2:3]
                                ts("dve", negM, mc[:, 0:1], -1.0, None, ALU.mult, None, ["mc"], ["psm"])
                                act(sm_[:, 8:24], mc[:], AF.Exp, ["mc", "psm"], ["psm2", "psmZ"], bias=negM, accum=Z)
                                act(Z, Z, AF.Ln, ["psmZ"], ["psmZ"])
                                ts("dve", nthr, mc[:, 15:16], -1.0, 1.0e-3, ALU.mult, ALU.add, ["mc"], ["psm"])
                                tt("dve", Z, negM, Z, ALU.subtract, ["psm", "psmZ"], ["psmZ"])
                                tt("dve", LNR[:, tt_, h:h + 1], Z, nthr, ALU.subtract, ["psm", "psmZ"], [("lnr", tt_)])
                                ts("dve", am[:], s1, m1[:, 15:16], -1.0, ALU.is_ge, ALU.add, sk_ + ["mx"], ["am"])
                                stt(am[:], am[:], BIG, s1, ALU.mult, ALU.add, ["am"] + sk_, ["am"])
                                ts("dve", sfl[:, 0, :], am[:], nthr, None, ALU.add, None, ["am", "psm"], ["sfl"])
                                ts("dve", am[:], s2, m2[:, 15:16], -1.0, ALU.is_ge, ALU.add, sk_ + ["mx"], ["am"])
                                stt(sfl[:, 1, :], am[:], BIG, s2, ALU.mult, ALU.add, ["am"] + sk_, ["sfl"])
                                shl6 = shl[:].rearrange("p (c hf) (q j) -> p c hf q j", hf=2, q=2)
                                sfl4 = sfl[:].rearrange("p c (hf j) -> p c hf j", hf=2)
                                for c_ in range(2):
                                    cp("pool", shl6[:, c_, :, 0, :], sfl4[:, c_, :, :], ["sfl"], ["shl"])
                                    tt("pool", shl6[:, c_, :, 1, :], sfl4[:, c_, :, :], shl6[:, c_, :, 0, :], ALU.subtract, ["sfl", "shl"], ["shl"])
                                for q_ in range(4):
                                    tr(pst_[:, q_, :], shl[:, q_, :], identb[:], ["shl", "identb"], ["pst"])
                                cp("act", ST[:, tt_, h, :, :], pst_[:, 0:4, :], ["pst"], [("st", tt_)])
                        P.barrier()
                    with ExitStack() as st:
                        Us = sbuf(st, "Us", [128, 8, 256], F32)
                        Vs = sbuf(st, "Vs", [128, 2, D], F32)
                        UB = [sbuf(st, "UB%d" % j, [128, 8, 512], BF16) for j in range(2)]
                        VB = [sbuf(st, "VB%d" % j, [128, 4, D], BF16) for j in range(2)]
                        NR = 6
                        Rb = [sbuf(st, "Rb%d" % j, [128, 512], BF16) for j in range(NR)]
                        Wb = [sbuf(st, "Wb%d" % j, [128, 512], BF16) for j in range(NR)]
                        GtT = sbuf(st, "GtT", [128, 8, 512], BF16)
                        Gt = [GtT[:, j, :] for j in range(8)]
                        WAT = sbuf(st, "WAT", [128, 4, 512], BF16)
                        wT = [psum(st, "wT%d" % j, [128, 512], F32) for j in range(2)]
                        MC = [psum(st, "MC%d" % j, [128, 512], F32) for j in range(4)]
                        PP = [psum(st, "PP%d" % j, [128, 512], F32) for j in range(2)]
                        H2R = [("h2t", j) for j in range(4)]
                        ppc = [0]

                        def next_pp():
                            j = ppc[0] % 2
                            ppc[0] += 1
                            return PP[j], ("pp", j)

                        def load_steps(ck):
                            e0 = ck * 512
                            ub, vbf = UB[ck % 2], VB[ck % 2]
                            uk, vk = ("ub", ck % 2), ("vb", ck % 2)

                            def dma_h(hh):
                                P.dma(Us[:], dr["uT"][l][:, e0 + hh * 256:e0 + (hh + 1) * 256].rearrange("(k p) e -> p k e", p=128), writes=["Us"])
                                P.dma(Vs[:], dr["pv"][l][e0 + hh * 256:e0 + (hh + 1) * 256, :].rearrange("(s p) d -> p s d", p=128), writes=["Vs"])

                            def cast_h(hh):
                                cp("act", ub[:, :, hh * 256:(hh + 1) * 256], Us[:], ["Us"], [uk])
                                tt("pool", vbf[:, 2 * hh:2 * hh + 2, :], Vs[:], MOD[:, 2, :].unsqueeze(1).to_broadcast([128, 2, D]), ALU.mult, ["Vs", "mod"], [vk])
                            return [lambda: dma_h(0), lambda: (cast_h(0), dma_h(1)), lambda: cast_h(1)]

                        def front(ck, subs=(0, 1, 2, 3)):
                            ub, uk = UB[ck % 2], ("ub", ck % 2)
                            for s_ in subs:
                                p_, pk_ = next_pp()
                                for k in range(8):
                                    mm(p_[:], ub[:, k, s_ * 128:(s_ + 1) * 128], H2T[:, k, :], k == 0, k == 7, H2R + [uk], [pk_])
                                gj = (ck % 2) * 4 + s_
                                act(Gt[gj], p_[:], GELU, [pk_], [("gt", gj)])

                        def tail_v(ck):
                            vbf, vk = VB[ck % 2], ("vb", ck % 2)
                            outl = []
                            for tt_ in range(4):
                                for hf in range(2):
                                    def vgrp(tt_=tt_, hf=hf, vbf=vbf, vk=vk):
                                        i = 4 * g + tt_
                                        o_, ok = next_pp()
                                        for s_ in range(4):
                                            mm(o_[:], WAT[:, s_, tt_ * 128:(tt_ + 1) * 128], vbf[:, s_, hf * 512:(hf + 1) * 512], s_ == 0, s_ == 3, [("wat", tt_), vk], [ok])
                                        tt("dve", X[:, i, hf * 512:(hf + 1) * 512], X[:, i, hf * 512:(hf + 1) * 512], o_[:], ALU.add, [("x", i), ok], [("x", i)])
                                    outl.append(vgrp)
                            return outl

                        cn = 0
                        for f_ in load_steps(0):
                            f_()
                        front(0)
                        deferred = []
                        sel2 = identb2[:, :].unsqueeze(1).to_broadcast([128, 4, 64])
                        wtc = 0
                        pendq = []
                        midq = []
                        watq = [None]
                        for ck in range(32):
                            un_i = 0
                            half_, ckk = ck // 16, ck % 16
                            sel1 = identb2[:, 4 * ckk:4 * ckk + 4].unsqueeze(2).to_broadcast([128, 4, 128])
                            for tt_ in range(4):
                                wtb = wT[wtc % 2]
                                wtk = ("wt", wtc % 2)
                                wtc += 1
                                wt3 = wtb[:].rearrange("p (s t) -> p s t", s=4)
                                for h in range(8):
                                    kk = cn % NR
                                    rb, wb = Rb[kk], Wb[kk]
                                    mcb = MC[cn % 4]
                                    mk_ = ("mc", cn % 4)
                                    mc3 = mcb[:].rearrange("p (a b) -> p a b", a=4)
                                    stk = [("st", tt_), "identb2"]
                                    mm(mc3, ST[:, tt_, h, half_, :], sel1, True, False, stk, [mk_])
                                    mm(mc3[:, :, 0:64], ST[:, tt_, h, 2, :], sel2, False, False, stk, [mk_])
                                    mm(mc3[:, :, 64:128], ST[:, tt_, h, 3, :], sel2, False, False, stk, [mk_], sig=True)
                                    ts("dve", rb[:], mcb[:], 1.0e6, 0.0, ALU.mult, ALU.min, [mk_], [("rb", kk)])
                                    cn += 1

                                    def fin_unit(kk=kk, h=h, wb=wb, wt3=wt3, wtk=wtk):
                                        for s_ in range(4):
                                            mm(wt3[:, s_, :], wb[:, s_ * 128:(s_ + 1) * 128], identb[:],
                                               (h == 0 and s_ == 0), (h == 7 and s_ == 3), [("wb", kk), "identb"], [wtk], sig=(s_ == 3))

                                    def mid_unit(kk=kk, tt_=tt_, h=h, rb=rb, wb=wb, mcb=mcb, mk_=mk_, fin_unit=fin_unit):
                                        mm(mcb[:], identb[:], rb[:], False, True, ["identb", ("rb", kk)], [mk_])
                                        act(wb[:], mcb[:], AF.Exp, [mk_, ("lnr", tt_)], [("wb", kk)], bias=LNR[:, tt_, h:h + 1])
                                        pendq.append(fin_unit)
                                        if len(pendq) > 2:
                                            pendq.pop(0)()
                                    midq.append(mid_unit)
                                    if len(midq) > 1:
                                        midq.pop(0)()
                                    if h == 3 and watq[0] is not None:
                                        watq[0]()
                                        watq[0] = None
                                    if deferred:
                                        deferred.pop(0)()
                                    un_i += 1
                                    if ck + 1 < 32:
                                        if un_i == 9:
                                            while deferred:
                                                deferred.pop(0)()
                                            lsteps = load_steps(ck + 1)
                                            lsteps[0]()
                                        elif un_i == 16:
                                            lsteps[1]()
                                        elif un_i == 24:
                                            lsteps[2]()
                                        elif un_i == 27:
                                            front(ck + 1, (0, 1))
                                        elif un_i == 31:
                                            front(ck + 1, (2, 3))
                                    if h == 7:
                                        def wat_fn(tt_=tt_, gts=(ck % 2) * 4, wt3=wt3, wtk=wtk):
                                            tt("dve", WAT[:, :, tt_ * 128:(tt_ + 1) * 128], GtT[:, gts:gts + 4, tt_ * 128:(tt_ + 1) * 128], wt3, ALU.mult,
                                               [("gt", gts + s_) for s_ in range(4)] + [wtk], [("wat", tt_)])
                                        watq[0] = wat_fn
                            while deferred:
                                deferred.pop(0)()
                            deferred = tail_v(ck)
                        while midq:
                            midq.pop(0)()
                        while pendq:
                            pendq.pop(0)()
                        watq[0]()
                        while deferred:
                            deferred.pop(0)()
                        P.barrier()

        for l in layers:
            mixer_phase(l)
            if xmid_d is not None:
                for i in range(NT):
                    P.dma(xmid_d[i * 128:(i + 1) * 128, :], X[:, i, :], reads=[("x", i)], is_output=True)
            if do_ffn:
                ffn_phase(l)

        with ExitStack() as st:
            fgb = sbuf(st, "fgb", [128, D], F32)
            junk = sbuf(st, "junk", [128, D], BF16)
            ob = [sbuf(st, "ob%d" % j, [128, D], F32) for j in range(2)]
            P.dma(fgb[:], dr["final_g"].to_broadcast([128, D]), writes=["fgb"])
            if dbg is None or dbg == "final":
                for i in range(NT):
                    xi = X[:, i, :]
                    act(junk[:], xi, AF.Square, [("x", i)], ["junk", ("ssq", i)], accum=SSQ[:, i:i + 1])
                    act(RSTD[:, i:i + 1], SSQ[:, i:i + 1], AF.Sqrt, [("ssq", i), "epsc"], [("rstd", i)], bias=epsc[:], scale=1.0 / D)
                    recip(RSTD[:, i:i + 1], RSTD[:, i:i + 1], [("rstd", i)], [("rstd", i)])
                    stt(ob[i % 2][:], xi, RSTD[:, i:i + 1], fgb[:], ALU.mult, ALU.mult, [("x", i), ("rstd", i), "fgb"], [("ob", i % 2)])
                    P.dma(out_d[i * 128:(i + 1) * 128, :], ob[i % 2][:], reads=[("ob", i % 2)], is_output=True)
            elif dbg == "x":
                for i in range(NT):
                    P.dma(out_d[i * 128:(i + 1) * 128, :], X[:, i, :], reads=[("x", i)], is_output=True)
            P.barrier()
        P.finish()
        print("instructions:", P.n_inst, {k: len(v) for k, v in P.q.items()}, "dma sems:", len(P.dma_sem), flush=True)
    return nc


def _consts():
    c = {}
    c["ident"] = np.eye(128, dtype=np.float32)
    k = np.arange(128)[:, None]
    q = np.arange(128)[None, :]
    c["tri"] = np.stack([(k <= q), (k > q)], axis=1).astype(np.float32)

    def rt(dim):
        inv = (1.0 / (np.float32(10000.0) ** (np.arange(0, dim, 2, dtype=np.float32) / np.float32(dim)))).astype(np.float32)
        ang = (np.arange(S, dtype=np.float32)[:, None] * inv[None, :]).astype(np.float32)
        return np.stack([np.cos(ang), np.sin(ang)], axis=1).astype(np.float32)
    c["cs64"] = rt(64)
    c["cs32"] = rt(32)
    cidx_last = np.arange(127) * 16 + 31
    cm = np.zeros((128, S), np.float32)
    cm[:127] = (cidx_last[:, None] <= np.arange(S)[None, :])
    c["cmaskT"] = cm
    t = np.arange(S)
    qblk = t // 64
    jj = np.arange(32)
    allowed = jj[None, :] <= qblk[:, None]
    forced = (jj[None, :] == 0) | (jj[None, :] == qblk[:, None]) | (jj[None, :] == qblk[:, None] - 1)
    M1 = (allowed & ~forced).astype(np.float32)
    M2 = np.where(allowed, np.where(forced, 1e4, 0.0), -1e30).astype(np.float32)
    c["selM"] = np.stack([M1, M2, allowed.astype(np.float32)], axis=1)
    ex = np.zeros((32, S), np.float32)
    ex[np.arange(S) // 64, np.arange(S)] = 1.0
    c["expand"] = ex
    cidx = np.arange(127)[:, None] * 16 + np.arange(32)[None, :]
    ov = np.zeros((128, 33), np.float32)
    ov[:127, 0] = 1.0
    for cc in range(127):
        for p in cidx[cc]:
            ov[cc, 1 + p // 64] += 1.0 / 32.0
    c["ovl1"] = ov
    return c


def _prep_shared(inp):
    f = lambda a: np.ascontiguousarray(np.asarray(a, dtype=np.float32))
    w_in = f(inp["w_in"])
    perm = np.concatenate([np.arange(0, 256), np.arange(256, 320), np.arange(384, 448), np.arange(512, 576),
                           np.arange(320, 384), np.arange(448, 512), np.arange(576, 640), np.arange(640, 652),
                           np.arange(652, 2348)])
    sh = {
        "ada_w": f(inp["ada_w"]), "ada_b": f(inp["ada_b"]),
        "gmix": f(inp["norm_mix_g"]), "gffn": f(inp["norm_ffn_g"]), "final_g": f(inp["final_g"]).reshape(1, D),
        "w_in": f(w_in[:, :, perm]), "w_out": f(inp["w_out"]),
        "wkD": f(np.concatenate([inp["nsa_cmp_wk"], inp["nsa_cmp_wk"]], axis=-1)), "wv": f(inp["nsa_cmp_wv"]),
        "poskT": f(np.transpose(inp["nsa_cmp_pos_k"], (0, 2, 1))), "posvT": f(np.transpose(inp["nsa_cmp_pos_v"], (0, 2, 1))),
        "lam4": f(np.stack([inp["diff_lam_q1"], inp["diff_lam_k1"], inp["diff_lam_q2"], inp["diff_lam_k2"]], axis=1)),
        "sub_g": f(inp["diff_sub_g"]),
        "mla_qg": f(inp["mla_q_norm_g"]), "mla_w_uq": f(inp["mla_w_uq"]), "mla_kvg": f(inp["mla_kv_norm_g"]), "mla_w_ukv": f(inp["mla_w_ukv"]),
        "swa_sinks": f(inp["swa_sinks"]),
        "peer_w_q": f(inp["peer_w_q"]),
        "skT": f(np.stack([np.transpose(inp["peer_sub_k1"], (0, 2, 1)), np.transpose(inp["peer_sub_k2"], (0, 2, 1))], axis=1)),
        "uT": f(np.transpose(inp["peer_u"], (0, 2, 1))), "pv": f(inp["peer_v"]),
    }
    sh.update(_consts())
    return sh


def make_in_maps(inp):
    sh = _prep_shared(inp)
    x = np.asarray(inp["x"], dtype=np.float32)
    c = np.asarray(inp["c"], dtype=np.float32)
    maps = []
    for b in range(8):
        m = dict(sh)
        m["x"] = np.ascontiguousarray(x[b])
        m["cT"] = np.ascontiguousarray(c[b].reshape(8, 128).T)
        maps.append(m)
    return maps


_NC_CACHE = {}


def kernel(**inputs):
    if "nc" not in _NC_CACHE:
        _NC_CACHE["nc"] = build()
    nc = _NC_CACHE["nc"]
    in_maps = make_in_maps(inputs)
    res = run_bass_kernel_spmd(nc, in_maps, core_ids=list(range(8)))
    return np.stack([np.asarray(r["out"], dtype=np.float32) for r in res.results], axis=0)
```

```python
import math
from contextlib import ExitStack
import numpy as np
import concourse.bass as bass
import concourse.mybir as mybir
from concourse.bass_utils import run_bass_kernel_spmd

F32 = mybir.dt.float32
BF16 = mybir.dt.bfloat16
ALU = mybir.AluOpType
AF = mybir.ActivationFunctionType
AX = mybir.AxisListType

COMPUTE = ("pe", "dve", "act", "pool")
L = 2
S = 2048
D = 1024
NT = 16
EPS = 1e-6
BIG = 1.0e4
GELU = AF.Gelu_apprx_tanh
RELAX = 1 << 30

NSA0, DIF0, MLA0, SWA0 = 0, 652, 1420, 1836


class Prog:
    def __init__(self, nc, es):
        self.nc = nc
        self.es = es
        self.engs = {"pe": nc.tensor, "dve": nc.vector, "act": nc.scalar, "pool": nc.gpsimd, "sp": nc.sync}
        self.q = {k: [] for k in self.engs}
        self.cnt = {k: 0 for k in COMPUTE}
        self.sem = {k: es.enter_context(nc.semaphore("s_" + k)) for k in COMPUTE}
        self.seen = {k: {} for k in self.engs}
        self.lastw = {}
        self.readers = {}
        self.dma_sem = {}
        self.n_inst = 0
        self.out_waits = []

    def _deps(self, reads, writes):
        deps = []
        for k in reads:
            t = self.lastw.get(k)
            if t is not None:
                deps.append(t)
        for k in writes:
            t = self.lastw.get(k)
            if t is not None:
                deps.append(t)
            deps.extend(self.readers.get(k, ()))
        return deps

    def _emit_waits(self, eng, deps, self_sync=True):
        best = {}
        for (name, sem, val) in deps:
            if name == eng and (not self_sync or self.cnt[eng] - val >= RELAX):
                continue
            if name not in best or best[name][1] < val:
                best[name] = (sem, val)
        for name, (sem, val) in best.items():
            if self.seen[eng].get(name, 0) >= val:
                continue
            self.seen[eng][name] = val
            self.q[eng].append(lambda e, sem=sem, val=val: e.wait_ge(sem, val))

    def _commit(self, tok, reads, writes):
        for k in writes:
            self.lastw[k] = tok
            self.readers[k] = []
        for k in reads:
            if k in writes:
                continue
            self.readers.setdefault(k, []).append(tok)

    def op(self, eng, fn, reads=(), writes=(), self_sync=True, sig=True):
        deps = self._deps(reads, writes)
        self._emit_waits(eng, deps, self_sync)
        sem = self.sem[eng]
        if sig:
            self.cnt[eng] += 1
            val = self.cnt[eng]
            self.q[eng].append(lambda e, fn=fn, sem=sem: fn(e).then_inc(sem, 1))
        else:
            val = self.cnt[eng] + 1
            self.q[eng].append(lambda e, fn=fn: fn(e))
        self._commit((eng, sem, val), reads, writes)
        self.n_inst += 1

    def dma(self, out, in_, reads=(), writes=(), queue="sp", is_output=False):
        deps = self._deps(reads, writes)
        self._emit_waits(queue, deps)
        key = writes[0] if writes else ("rd", reads[0])
        if key not in self.dma_sem:
            self.dma_sem[key] = [self.es.enter_context(self.nc.semaphore("d%d" % len(self.dma_sem))), 0]
        ent = self.dma_sem[key]
        ent[1] += 16
        sem, val = ent[0], ent[1]
        self.q[queue].append(lambda e, out=out, in_=in_, sem=sem: e.dma_start(out=out, in_=in_).then_inc(sem, 16))
        tok = ("dma:%s" % str(key), sem, val)
        self._commit(tok, reads, writes)
        if is_output:
            self.out_waits.append(tok)
        self.n_inst += 1

    def barrier(self):
        deps = [(k, self.sem[k], self.cnt[k]) for k in COMPUTE if self.cnt[k] > 0]
        for key, (sem, val) in self.dma_sem.items():
            if val > 0:
                deps.append(("dma:%s" % str(key), sem, val))
        for eng in self.engs:
            self._emit_waits(eng, deps, self_sync=True)
        self.lastw = {}
        self.readers = {}

    def finish(self):
        self._emit_waits("sp", self.out_waits)
        with self.nc.Block() as block:
            @block.sync
            def _(e):
                for f in self.q["sp"]:
                    f(e)

            @block.tensor
            def _(e):
                for f in self.q["pe"]:
                    f(e)

            @block.vector
            def _(e):
                for f in self.q["dve"]:
                    f(e)

            @block.scalar
            def _(e):
                for f in self.q["act"]:
                    f(e)

            @block.gpsimd
            def _(e):
                for f in self.q["pool"]:
                    f(e)


INPUT_SPECS = [
    ("x", [S, D]), ("cT", [128, 8]),
    ("ada_w", [L, D, 6 * D]), ("ada_b", [L, 6 * D]),
    ("gmix", [L, D]), ("gffn", [L, D]), ("final_g", [1, D]),
    ("w_in", [L, D, 2348]), ("w_out", [L, D, D]),
    ("wkD", [L, 2048, 128]), ("wv", [L, 2048, 64]), ("poskT", [L, 64, 32]), ("posvT", [L, 64, 32]),
    ("lam4", [L, 4, 32]), ("sub_g", [L, 64]),
    ("mla_qg", [L, 256]), ("mla_w_uq", [L, 256, 384]), ("mla_kvg", [L, 128]), ("mla_w_ukv", [L, 128, 512]),
    ("swa_sinks", [L, 4]),
    ("peer_w_q", [L, D, 2048]), ("skT", [L, 2, 128, 128]),
    ("uT", [L, D, 16384]), ("pv", [L, 16384, D]),
    ("ident", [128, 128]), ("tri", [128, 2, 128]),
    ("cs64", [S, 2, 32]), ("cs32", [S, 2, 16]),
    ("cmaskT", [128, S]), ("selM", [S, 3, 32]), ("expand", [32, S]), ("ovl1", [128, 33]),
]


def build(layers=(0, 1), dbg=None, do_mixers=(0, 1, 2, 3), do_ffn=True):
    nc = bass.Bass("TRN2", target_bir_lowering=False)
    dr = {}
    for name, shape in INPUT_SPECS:
        dr[name] = nc.dram_tensor(name, shape, F32, kind="ExternalInput").ap()
    out_d = nc.dram_tensor("out", [S, D], F32, kind="ExternalOutput").ap()
    xmid_d = nc.dram_tensor("xmid", [S, D], F32, kind="ExternalOutput").ap() if dbg == "x" else None

    with ExitStack() as es:
        P = Prog(nc, es)

        uid = [0]

        def sbuf(st, name, shape, dt):
            uid[0] += 1
            return st.enter_context(nc.sbuf_tensor("%s_s%d" % (name, uid[0]), shape, dt))

        def psum(st, name, shape, dt):
            uid[0] += 1
            return st.enter_context(nc.psum_tensor("%s_p%d" % (name, uid[0]), shape, dt))

        def psum_tb(st, name):
            t_ = psum(st, name, [128, 512], F32)
            return t_[:].bitcast(BF16).rearrange("p (k t) -> p k t", t=128)

        def mm(o, l, r, st, sp, R, W, sig=None):
            P.op("pe", lambda e: e.matmul(o, l, r, start=st, stop=sp, skip_group_check=True), reads=R, writes=W, self_sync=False,
                 sig=(sp if sig is None else sig))

        def tr(o, i, idn, R, W):
            P.op("pe", lambda e: e.transpose(o, i, idn), reads=R, writes=W, self_sync=False)

        def tt(eng, o, a, b, op, R, W):
            P.op(eng, lambda e: e.tensor_tensor(o, a, b, op), reads=R, writes=W)

        def ts(eng, o, a, s1, s2, op0, op1, R, W):
            if s2 is None:
                P.op(eng, lambda e: e.tensor_scalar(o, a, s1, None, op0), reads=R, writes=W)
            else:
                P.op(eng, lambda e: e.tensor_scalar(o, a, s1, s2, op0, op1), reads=R, writes=W)

        def stt(o, a, s, b, op0, op1, R, W):
            P.op("dve", lambda e: e.scalar_tensor_tensor(o, a, s, b, op0, op1), reads=R, writes=W)

        def act(o, i, func, R, W, bias=None, scale=None, accum=None):
            kw = {}
            if bias is not None:
                kw["bias"] = bias
            if scale is not None:
                kw["scale"] = scale
            if accum is not None:
                kw["accum_out"] = accum
            P.op("act", lambda e: e.activation(o, i, func, **kw), reads=R, writes=W)

        def cp(eng, o, i, R, W):
            if eng == "act":
                P.op("act", lambda e: e.copy(o, i), reads=R, writes=W)
            else:
                P.op(eng, lambda e: e.tensor_copy(o, i), reads=R, writes=W)

        def memset(eng, o, val, W):
            P.op(eng, lambda e: e.memset(o, val), writes=W)

        def red(o, i, op, R, W):
            P.op("dve", lambda e: e.tensor_reduce(o, i, AX.X, op), reads=R, writes=W)

        def recip(o, i, R, W):
            P.op("dve", lambda e: e.reciprocal(o, i), reads=R, writes=W)

        X = sbuf(es, "X", [128, NT, D], F32)
        MOD = sbuf(es, "MOD", [128, 3, D], F32)
        identf = sbuf(es, "identf", [128, 128], F32)
        identb = sbuf(es, "identb", [128, 128], BF16)
        tri = sbuf(es, "tri", [128, 2, 128], BF16)
        identb2 = sbuf(es, "identb2", [128, 64], BF16)
        zrow = sbuf(es, "zrow", [1, 512], BF16)
        onesb = sbuf(es, "onesb", [1, 128], BF16)
        scb = sbuf(es, "scb", [128, 8, 128], F32)
        SSQ = sbuf(es, "SSQ", [128, NT], F32)
        RSTD = sbuf(es, "RSTD", [128, NT], F32)
        epsc = sbuf(es, "epsc", [128, 1], F32)

        with ExitStack() as st0:
            trif = sbuf(st0, "trif", [128, 2, 128], F32)
            ct = sbuf(st0, "ct", [128, 8], F32)
            P.dma(identf[:], dr["ident"], writes=["identf"])
            P.dma(trif[:], dr["tri"], writes=["trif"])
            P.dma(ct[:], dr["cT"], writes=["ct"])
            for i in range(NT):
                P.dma(X[:, i, :], dr["x"][i * 128:(i + 1) * 128, :], writes=[("x", i)])
            cp("dve", identb[:], identf[:], ["identf"], ["identb"])
            cp("dve", tri[:], trif[:], ["trif"], ["tri"])
            tt("pool", identb2[:], identb[:, 0:64], identb[:, 64:128], ALU.add, ["identb"], ["identb2"])
            memset("pool", zrow[:], 0.0, ["zrow"])
            memset("pool", onesb[:], 1.0, ["onesb"])
            memset("pool", epsc[:], EPS, ["epsc"])
            act(ct[:], ct[:], AF.Silu, ["ct"], ["ct"])
            cp("dve", scb[:], ct[:].unsqueeze(2).to_broadcast([128, 8, 128]), ["ct"], ["scb"])
            P.barrier()

        def load_mod(l, half):
            with ExitStack() as st:
                AW = [sbuf(st, "AW%d" % j, [128, 8, 512], F32) for j in range(2)]
                gbc = sbuf(st, "gbc", [128, D], F32)
                pm = [psum(st, "pm%d" % j, [128, 512], F32) for j in range(2)]
                MODf = MOD[:].rearrange("p a d -> p (a d)")
                c0 = half * 3 * D
                P.dma(MODf, dr["ada_b"][l:l + 1, c0:c0 + 3 * D].to_broadcast([128, 3 * D]), writes=["mod"])
                gsrc = dr["gmix"] if half == 0 else dr["gffn"]
                P.dma(gbc[:], gsrc[l:l + 1, :].to_broadcast([128, D]), writes=["gbc"])
                for n in range(6):
                    stg = AW[n % 2]
                    P.dma(stg[:], dr["ada_w"][l][:, c0 + n * 512:c0 + (n + 1) * 512].rearrange("(k p) n -> p k n", p=128),
                          writes=[("AW", n % 2)])
                    for k in range(8):
                        mm(pm[n % 2][:], scb[:, k, :], stg[:, k, :], k == 0, k == 7, [("AW", n % 2), "scb"], [("pm", n % 2)])
                    tt("dve", MODf[:, n * 512:(n + 1) * 512], pm[n % 2][:], MODf[:, n * 512:(n + 1) * 512], ALU.add,
                       [("pm", n % 2), "mod"], ["mod"])
                stt(MOD[:, 1, :], MOD[:, 1, :], 1.0, gbc[:], ALU.add, ALU.mult, ["mod", "gbc"], ["mod"])
                P.barrier()

        def norm_to_T(i, dstT, col0, dkey, wk):
            junk, t1, hb, pT = wk
            xi = X[:, i, :]
            act(junk[:], xi, AF.Square, [("x", i)], ["junk", ("ssq", i)], accum=SSQ[:, i:i + 1])
            act(RSTD[:, i:i + 1], SSQ[:, i:i + 1], AF.Sqrt, [("ssq", i), "epsc"], [("rstd", i)], bias=epsc[:], scale=1.0 / D)
            recip(RSTD[:, i:i + 1], RSTD[:, i:i + 1], [("rstd", i)], [("rstd", i)])
            stt(t1[:], xi, RSTD[:, i:i + 1], MOD[:, 1, :], ALU.mult, ALU.mult, [("x", i), ("rstd", i), "mod"], ["t1"])
            tt("pool", hb[:], t1[:], MOD[:, 0, :], ALU.add, ["t1", "mod"], ["hb"])
            for k in range(8):
                tr(pT[:, k, :], hb[:, k * 128:(k + 1) * 128], identb[:], ["hb", "identb"], ["pT"])
            cp("act", dstT[:, :, col0:col0 + 128], pT[:], ["pT"], [dkey])

        def load_w_cols(W, wkey, src2d, ncols, stg, sname):
            j = 0
            for c0 in range(0, ncols, 256):
                n = min(256, ncols - c0)
                s_ = stg[j % len(stg)]
                sk = (sname, j % len(stg))
                P.dma(s_[:, :, 0:n], src2d[:, c0:c0 + n].rearrange("(k p) n -> p k n", p=128), writes=[sk])
                cp("act" if j % 2 == 0 else "pool", W[:, :, c0:c0 + n], s_[:, :, 0:n], [sk], [wkey])
                j += 1

        def rope(dst4, src4, G, half, cs_i, tC, tS, R, W):
            cb = cs_i[:, 0, :].unsqueeze(1).unsqueeze(1).to_broadcast([128, G, 2, half])
            sb_ = cs_i[:, 1, :].unsqueeze(1).unsqueeze(1).to_broadcast([128, G, 2, half])
            tC4 = tC[:, 0:G * 2 * half].rearrange("p (g two h) -> p g two h", two=2, h=half)
            tS4 = tS[:, 0:G * 2 * half].rearrange("p (g two h) -> p g two h", two=2, h=half)
            tt("dve", tC4, src4, cb, ALU.mult, R + ["cs"], ["tC"])
            tt("dve", tS4, src4, sb_, ALU.mult, R + ["cs"], ["tS"])
            tt("pool", dst4[:, :, 0, :], tC4[:, :, 0, :], tS4[:, :, 1, :], ALU.subtract, ["tC", "tS"], W)
            tt("pool", dst4[:, :, 1, :], tS4[:, :, 0, :], tC4[:, :, 1, :], ALU.add, ["tC", "tS"], W)

        def v4(ap, G, half):
            return ap.rearrange("p (g two h) -> p g two h", two=2, h=half)

        def flash(qc, units, kt_range, edge, scale, ACC, SC, PEX, shared_mask=None):
            qts = [4 * qc + j for j in range(4)]
            lo = min(kt_range(q)[0] for q in qts)
            hi = max(kt_range(q)[1] for q in qts)
            for u in range(len(units)):
                mm(ACC[u][:, 0:512], onesb[0:1, 0:128], zrow[0:1, 0:512], True, False, ["onesb", "zrow"], [("acc", u)])
            steps = []
            for kt in range(lo, hi + 1):
                need = [j for j, q in enumerate(qts) if kt_range(q)[0] <= kt <= kt_range(q)[1]]
                j0, j1 = need[0], need[-1]
                c0 = (4 * qc + j0) * 128
                n = (j1 - j0 + 1) * 128
                for u, un in enumerate(units):
                    steps.append((kt, u, un, need, j0, c0, n))
            smc = {}

            def emit_scores(idx):
                kt, u, un, need, j0, c0, n = steps[idx]
                if shared_mask is not None and kt not in smc:
                    smc[kt] = shared_mask(kt, c0, n, j0)
                sc = SC[idx % len(SC)]
                sk = ("sc", idx % len(SC))
                np_ = len(un["parts"])
                for pi, (kf, qf) in enumerate(un["parts"]):
                    mm(sc[:, 0:n], kf(kt), qf(c0, n), pi == 0, pi == np_ - 1, un["R"], [sk])

            def emit_rest(idx):
                kt, u, un, need, j0, c0, n = steps[idx]
                sc = SC[idx % len(SC)]
                sk = ("sc", idx % len(SC))
                pe = PEX[idx % len(PEX)]
                pk = ("pex", idx % len(PEX))
                act(pe[:, 0:n], sc[:, 0:n], AF.Exp, [sk], [pk], scale=scale)
                if shared_mask is not None:
                    sm = smc[kt]
                    tt("dve", pe[:, 0:n], pe[:, 0:n], sm[0], ALU.mult, [pk, sm[1]], [pk])
                else:
                    for j in need:
                        ed = edge(kt, 4 * qc + j)
                        if ed is not None:
                            sl = pe[:, (j - j0) * 128:(j - j0 + 1) * 128]
                            tt("pool", sl, sl, tri[:, ed, :], ALU.mult, [pk, "tri"], [pk])
                for j in need:
                    last = (kt == kt_range(4 * qc + j)[1])
                    mm(ACC[u][:, j * 65:(j + 1) * 65], pe[:, (j - j0) * 128:(j - j0 + 1) * 128], un["vf"](kt),
                       False, last, [pk] + un["R"], [("acc", u)])

            LOOK = len(SC) - 1
            for i0 in range(min(LOOK, len(steps))):
                emit_scores(i0)
            for idx in range(len(steps)):
                if idx + LOOK < len(steps):
                    emit_scores(idx + LOOK)
                emit_rest(idx)

        def apply_wout(qc, O, okey, WO, SC, MS, wk):
            OT, tmpx = wk
            for j in range(4):
                i = 4 * qc + j
                pT = MS[j % 2]
                pk = ("ms", 0)
                pTf = pT[:, 0:256].rearrange("p (k t) -> p k t", k=2)
                for k in range(2):
                    tr(pTf[:, k, :], O[:, j, k * 128:(k + 1) * 128], identf[:], [okey, "identf"], [pk])
                cp("act", OT[:], pTf, [pk], ["OT"])
                for hf in range(2):
                    for k in range(2):
                        mm(SC[hf][:, 0:512], OT[:, k, :], WO[:, k, hf * 512:(hf + 1) * 512], k == 0, k == 1, ["OT", "WO"], [("sc", hf)])
                for hf in range(2):
                    tt("dve", tmpx[:, hf * 512:(hf + 1) * 512], SC[hf][:, 0:512], MOD[:, 2, hf * 512:(hf + 1) * 512], ALU.mult,
                       [("sc", hf), "mod"], ["tmpx"])
                tt("pool", X[:, i, :], X[:, i, :], tmpx[:], ALU.add, [("x", i), "tmpx"], [("x", i)])

        def load_wout(st, l, m):
            WOs = sbuf(st, "WOs", [128, 2, 512], F32)
            WO = sbuf(st, "WO", [128, 2, D], BF16)
            for hf in range(2):
                P.dma(WOs[:], dr["w_out"][l][m * 256:(m + 1) * 256, hf * 512:(hf + 1) * 512].rearrange("(k p) n -> p k n", p=128),
                      writes=["WOs"])
                cp("act", WO[:, :, hf * 512:(hf + 1) * 512], WOs[:], ["WOs"], ["WO"])
            return WO

        def causal(qt):
            return (0, qt)

        def causal_edge(kt, qt):
            return 0 if kt == qt else None

        def window(wt):
            return (lambda qt: (max(0, qt - wt), qt)), (lambda kt, qt: 0 if kt == qt else (1 if kt == qt - wt else None))

        def mixer_phase(l):
            load_mod(l, 0)
            with ExitStack() as sm:
                HT = sbuf(sm, "HT", [128, 8, S], BF16)
                CS64 = sbuf(sm, "CS64", [128, NT, 2, 32], F32)
                CS32 = sbuf(sm, "CS32", [128, NT, 2, 16], F32)
                P.dma(CS64[:], dr["cs64"].rearrange("(i p) c h -> p i c h", p=128), writes=["cs"])
                P.dma(CS32[:], dr["cs32"].rearrange("(i p) c h -> p i c h", p=128), writes=["cs"])
                with ExitStack() as st:
                    junk = sbuf(st, "junk", [128, D], BF16)
                    t1 = sbuf(st, "t1", [128, D], F32)
                    hb = sbuf(st, "hb", [128, D], BF16)
                    pT = psum_tb(st, "pT")
                    for i in range(NT):
                        norm_to_T(i, HT, i * 128, ("hT", i), (junk, t1, hb, pT))
                    P.barrier()
                if 0 in do_mixers:
                    nsa_mixer(l, HT, CS64)
                if 1 in do_mixers:
                    diff_mixer(l, HT, CS32)
                if 2 in do_mixers:
                    mla_mixer(l, HT, CS32)
                if 3 in do_mixers:
                    swa_mixer(l, HT, CS64)
                P.barrier()

        def cols_for_tile(i, HT, W, ncols, banks, bkeys):
            for b, c0 in enumerate(range(0, ncols, 512)):
                n = min(512, ncols - c0)
                for k in range(8):
                    mm(banks[b][:, 0:n], HT[:, k, i * 128:(i + 1) * 128], W[:, k, c0:c0 + n], k == 0, k == 7,
                       [("hT", i), "W"], [bkeys[b]])

        def attn_scope(st, npex=3):
            ACC = [psum(st, "ACC%d" % j, [128, 512], F32) for j in range(4)]
            SC = [psum(st, "SC%d" % j, [128, 512], F32) for j in range(3)]
            ms0 = psum(st, "MS0", [128, 512], F32)
            MS = [ms0, ms0]
            PEX = [sbuf(st, "PEX%d" % j, [128, 512], BF16) for j in range(npex + 1)]
            OT = sbuf(st, "OT", [128, 2, 128], BF16)
            tmpx = sbuf(st, "tmpx", [128, D], F32)
            return ACC, SC, MS, PEX, (OT, tmpx)

        def nsa_mixer(l, HT, CS64):
            with ExitStack() as sa:
                NA = sbuf(sa, "NA", [128, 6, S], BF16)
                VS = sbuf(sa, "VS", [128, NT, 2, 65], BF16)
                GT = sbuf(sa, "GT", [128, NT, 12], F32)
                memset("pool", VS[:], 1.0, ["VS"])
                with ExitStack() as st:
                    W = sbuf(st, "W", [128, 8, 768], BF16)
                    stg = [sbuf(st, "stg%d" % j, [128, 8, 256], F32) for j in range(2)]
                    tC = sbuf(st, "tC", [128, 512], F32)
                    tS = sbuf(st, "tS", [128, 512], F32)
                    R = sbuf(st, "R", [128, 768], BF16)
                    pb = [psum(st, "pb%d" % j, [128, 512], F32) for j in range(4)]
                    pT = [psum_tb(st, "pTn%d" % j)[:, 0:6, :] for j in range(2)]
                    load_w_cols(W, "W", dr["w_in"][l][:, NSA0:NSA0 + 652], 652, stg, "stg")
                    cols_for_tile(0, HT, W, 652, [pb[0], pb[1]], [("pb", 0), ("pb", 1)])
                    for i in range(NT):
                        b = [pb[(2 * i) % 4], pb[(2 * i + 1) % 4]]
                        bk = [("pb", (2 * i) % 4), ("pb", (2 * i + 1) % 4)]
                        if i + 1 < NT:
                            cols_for_tile(i + 1, HT, W, 652, [pb[(2 * i + 2) % 4], pb[(2 * i + 3) % 4]], [("pb", (2 * i + 2) % 4), ("pb", (2 * i + 3) % 4)])
                        cs = CS64[:, i, :, :]
                        rope(v4(R[:, 0:256], 4, 32), v4(b[0][:, 0:256], 4, 32), 4, 32, cs, tC, tS, [bk[0]], ["R"])
                        Rk = R[:, 256:640].rearrange("p (g r d) -> p g r d", r=2, d=64)
                        rope(Rk[:, :, 0, :].rearrange("p g (two h) -> p g two h", two=2), v4(b[0][:, 256:448], 3, 32), 3, 32, cs, tC, tS, [bk[0]], ["R"])
                        cp("pool", Rk[:, :, 1, :], Rk[:, :, 0, :], ["R"], ["R"])
                        cp("dve", R[:, 640:768].rearrange("p (r d) -> p r d", r=2), b[0][:, 448:512].unsqueeze(1).to_broadcast([128, 2, 64]), [bk[0]], ["R"])
                        cp("dve", VS[:, i, :, 0:64], b[1][:, 0:128].rearrange("p (g d) -> p g d", g=2), [bk[1]], ["VS"])
                        act(GT[:, i, :], b[1][:, 128:140], AF.Sigmoid, [bk[1]], ["GT"])
                        p_ = pT[i % 2]
                        for k in range(6):
                            tr(p_[:, k, :], R[:, k * 128:(k + 1) * 128], identb[:], ["R", "identb"], [("pTn", i % 2)])
                        cp("act", NA[:, :, i * 128:(i + 1) * 128], p_[:], [("pTn", i % 2)], [("na", i)])
                    P.barrier()
                NAR = [("na", i) for i in range(NT)]
                with ExitStack() as st:
                    ACC, SC, MS, PEX, wk = attn_scope(st)
                    WO = load_wout(st, l, 0)
                    KCMP = sbuf(st, "KCMP", [128, 128], BF16)
                    VC1 = sbuf(st, "VC1", [128, 97], BF16)
                    CM = sbuf(st, "CM", [128, S], BF16)
                    SELM = sbuf(st, "SELM", [128, NT, 3, 32], F32)
                    EXP = sbuf(st, "EXP", [32, S], BF16)
                    selT = sbuf(st, "selT", [32, 512], BF16)
                    P.dma(SELM[:], dr["selM"].rearrange("(i p) c j -> p i c j", p=128), writes=["SELM"])
                    with ExitStack() as sp_:
                        wstg = sbuf(sp_, "wstg", [64, 8, 128], F32)
                        WKb = sbuf(sp_, "WKb", [64, 32, 128], BF16)
                        WVb = sbuf(sp_, "WVb", [64, 32, 64], BF16)
                        posf = sbuf(sp_, "posf", [64, 2, 32], F32)
                        posb = sbuf(sp_, "posb", [64, 2, 32], BF16)
                        ovf = sbuf(sp_, "ovf", [128, 33], F32)
                        kb = sbuf(sp_, "kb", [128, 1], F32)
                        vb = sbuf(sp_, "vb", [1, 64], BF16)
                        CMf = sbuf(sp_, "CMf", [128, 512], F32)
                        EXs = sbuf(sp_, "EXs", [32, 512], F32)
                        wk3 = dr["wkD"][l].rearrange("(j d) o -> d j o", d=64)
                        wv3 = dr["wv"][l].rearrange("(j d) o -> d j o", d=64)
                        for c in range(4):
                            P.dma(wstg[:], wk3[:, 8 * c:8 * c + 8, :], writes=["wstg"])
                            cp("act", WKb[:, 8 * c:8 * c + 8, :], wstg[:], ["wstg"], ["WKb"])
                        for c in range(4):
                            P.dma(wstg[:, :, 0:64], wv3[:, 8 * c:8 * c + 8, :], writes=["wstg"])
                            cp("act", WVb[:, 8 * c:8 * c + 8, :], wstg[:, :, 0:64], ["wstg"], ["WVb"])
                        P.dma(posf[:, 0, :], dr["poskT"][l], writes=["posf"])
                        P.dma(posf[:, 1, :], dr["posvT"][l], writes=["posf"])
                        P.dma(ovf[:], dr["ovl1"], writes=["ovf"])
                        for c in range(4):
                            P.dma(CMf[:], dr["cmaskT"][:, c * 512:(c + 1) * 512], writes=["CMf"])
                            cp("pool", CM[:, c * 512:(c + 1) * 512], CMf[:], ["CMf"], ["CM"])
                            P.dma(EXs[:], dr["expand"][:, c * 512:(c + 1) * 512], writes=["EXs"])
                            cp("pool", EXP[:, c * 512:(c + 1) * 512], EXs[:], ["EXs"], ["EXP"])
                        cp("dve", posb[:], posf[:], ["posf"], ["posb"])
                        memset("pool", VC1[:], 0.0, ["VC1"])
                        memset("pool", KCMP[:], 0.0, ["KCMP"])
                        cp("dve", VC1[:, 64:97], ovf[:], ["ovf", "VC1"], ["VC1"])
                        for j in range(32):
                            mm(MS[0][:, 0:1], WKb[0:64, j, :], posb[0:64, 0, j:j + 1], j == 0, j == 31, ["WKb", "posb"], [("ms", 0)])
                        cp("dve", kb[:], MS[0][:, 0:1], [("ms", 0)], ["kb"])
                        for j in range(32):
                            mm(MS[1][0:1, 0:64], posb[0:64, 1, j:j + 1], WVb[0:64, j, :], j == 0, j == 31, ["WVb", "posb"], [("ms", 0)])
                        cp("dve", vb[:], MS[1][0:1, 0:64], [("ms", 0)], ["vb"])
                        for j in range(32):
                            mm(SC[0][:, 0:127], WKb[0:64, j, :], NA[0:64, 2, j:j + 16 * 126 + 1:16], j == 0, j == 31, ["WKb"] + NAR, [("sc", 0)])
                        ts("dve", KCMP[:, 0:127], SC[0][:, 0:127], kb[:, 0:1], None, ALU.add, None, [("sc", 0), "kb", "KCMP"], ["KCMP"])
                        for j in range(32):
                            mm(SC[1][0:127, 0:64], NA[0:64, 5, j:j + 16 * 126 + 1:16], WVb[0:64, j, :], j == 0, False, ["WVb"] + NAR, [("sc", 1)])
                        mm(SC[1][0:127, 0:64], onesb[0:1, 0:127], vb[0:1, :], False, True, ["onesb", "vb"], [("sc", 1)])
                        cp("dve", VC1[0:127, 0:64], SC[1][0:127, 0:64], [("sc", 1), "VC1"], ["VC1"])
                        P.barrier()
                    MK = [sbuf(st, "MK%d" % j, [128, 512], BF16) for j in range(2)]
                    O = sbuf(st, "O", [128, 4, 256], F32)
                    sm_ = sbuf(st, "nsa_small", [128, 256], F32)
                    impw = sbuf(st, "impw", [128, 4, 32], F32)
                    imr = sbuf(st, "imr", [128, 32], F32)
                    selb = sbuf(st, "selb", [128, 32], BF16)
                    PC = [sbuf(st, "PC%d" % j, [128, 512], BF16) for j in range(4)]
                    scale = 64 ** -0.5
                    for qc in range(4):
                        q0 = qc * 512
                        pcs = []
                        for h in range(4):
                            hp, blk = 64 * (h % 2), h // 2
                            sc = SC[h % 2]
                            mm(sc[0:127, :], KCMP[hp:hp + 64, 0:127], NA[hp:hp + 64, blk, q0:q0 + 512], True, True, ["KCMP"] + NAR, [("sc", h % 2)])
                            pc = PC[h]
                            act(pc[0:127, :], sc[0:127, :], AF.Exp, [("sc", h % 2)], [("pc", h)], scale=scale)
                            tt("dve", pc[0:127, :], pc[0:127, :], CM[0:127, q0:q0 + 512], ALU.mult, [("pc", h), "CM"], [("pc", h)])
                            pcs.append(pc)
                        for j in range(4):
                            i = 4 * qc + j
                            psc = MS[j % 2]
                            pck = ("ms", 0)
                            ps3 = psc[:, 0:388].rearrange("p (h c) -> p h c", h=4)
                            for h in range(4):
                                mm(ps3[:, h, :], pcs[h][0:127, j * 128:(j + 1) * 128], VC1[0:127, :], True, True, [("pc", h), "VC1"], [pck])
                            den = sm_[:, 0:4]
                            g1 = sm_[:, 4:8]
                            ts("dve", den, ps3[:, :, 64], 1e-30, None, ALU.max, None, [pck], ["nsm"])
                            recip(den, den, ["nsm"], ["nsm"])
                            tt("dve", g1, den, GT[:, i, 0:12:3], ALU.mult, ["nsm", "GT"], ["nsm"])
                            tt("dve", O[:, j, :].rearrange("p (h d) -> p h d", h=4), ps3[:, :, 0:64],
                               g1.unsqueeze(2).to_broadcast([128, 4, 64]), ALU.mult, [pck, "nsm"], ["O"])
                            tt("dve", impw[:], ps3[:, :, 65:97], den.unsqueeze(2).to_broadcast([128, 4, 32]), ALU.mult, [pck, "nsm"], ["impw"])
                            red(imr[:], impw[:].rearrange("p h j -> p j h"), ALU.add, ["impw"], ["imr"])
                            tt("dve", imr[:], imr[:], SELM[:, i, 0, :], ALU.mult, ["imr", "SELM"], ["imr"])
                            tt("dve", imr[:], imr[:], SELM[:, i, 1, :], ALU.add, ["imr", "SELM"], ["imr"])
                            m8 = sm_[:, 8:24]
                            imr2 = sm_[:, 32:64]
                            P.op("dve", lambda e, m8=m8: e.max(m8[:, 0:8], imr[:]), reads=["imr"], writes=["nsm"])
                            P.op("dve", lambda e, m8=m8, imr2=imr2: e.match_replace(imr2, m8[:, 0:8], imr[:], -3.0e38), reads=["imr", "nsm"], writes=["nsm2"])
                            P.op("dve", lambda e, m8=m8, imr2=imr2: e.max(m8[:, 8:16], imr2), reads=["nsm2"], writes=["nsm"])
                            stt(selb[:], imr[:], m8[:, 15:16], SELM[:, i, 2, :], ALU.is_ge, ALU.mult, ["imr", "nsm", "SELM"], ["selb"])
                            pst = MS[j % 2][:, 256:384].bitcast(BF16)
                            tr(pst[0:32, 0:128], selb[:], identb[:], ["selb", "identb"], [pck])
                            cp("act", selT[:, j * 128:(j + 1) * 128], pst[0:32, 0:128], [pck], ["selT"])

                        def smask(kt, c0, n, j0, qc=qc, q0=q0):
                            mk = MK[kt % 2]
                            mkk = ("mk", kt % 2)
                            pm_ = MS[kt % 2]
                            mm(pm_[:, 0:n], EXP[0:32, kt * 128:(kt + 1) * 128], selT[0:32, c0 - q0:c0 - q0 + n], True, True, ["EXP", "selT"], [("ms", 0)])
                            cp("act", mk[:, 0:n], pm_[:, 0:n], [("ms", 0)], [mkk])
                            if kt >= 4 * qc:
                                tt("pool", mk[:, 0:128], mk[:, 0:128], tri[:, 0, :], ALU.mult, [mkk, "tri"], [mkk])
                            return (mk[:, 0:n], mkk)

                        units = []
                        for h in range(4):
                            hp, blk = 64 * (h % 2), h // 2
                            units.append(dict(
                                parts=[((lambda kt, hp=hp: NA[hp:hp + 64, 3, kt * 128:(kt + 1) * 128]),
                                        (lambda c0, n, hp=hp, blk=blk: NA[hp:hp + 64, blk, c0:c0 + n]))],
                                vf=(lambda kt: VS[:, kt, 0, :]), R=NAR + ["VS"]))
                        flash(qc, units, causal, causal_edge, scale, ACC, SC, PEX, shared_mask=smask)
                        nsa_fin(qc, ACC, GT, O, sm_, 1)
                        kr, ed = window(4)
                        units = []
                        for h in range(4):
                            hp, blk = 64 * (h % 2), h // 2
                            units.append(dict(
                                parts=[((lambda kt, hp=hp: NA[hp:hp + 64, 4, kt * 128:(kt + 1) * 128]),
                                        (lambda c0, n, hp=hp, blk=blk: NA[hp:hp + 64, blk, c0:c0 + n]))],
                                vf=(lambda kt: VS[:, kt, 1, :]), R=NAR + ["VS"]))
                        flash(qc, units, kr, ed, scale, ACC, SC, PEX)
                        nsa_fin(qc, ACC, GT, O, sm_, 2)
                        if dbg == ("o", 0):
                            for j in range(4):
                                P.dma(out_d[(4 * qc + j) * 128:(4 * qc + j + 1) * 128, 0:256], O[:, j, :], reads=["O"], is_output=True)
                        apply_wout(qc, O, "O", WO, SC, MS, wk)
                    P.barrier()

        def nsa_fin(qc, ACC, GT, O, sm_, gi):
            for h in range(4):
                a3 = ACC[h][:, 0:260].rearrange("p (j c) -> p j c", j=4)
                rd = sm_[:, 64 + 4 * h:68 + 4 * h]
                recip(rd, a3[:, :, 64], [("acc", h)], [("rd", h)])
                tt("dve", rd, rd, GT[:, 4 * qc:4 * qc + 4, 3 * h + gi], ALU.mult, [("rd", h), "GT"], [("rd", h)])
                for j in range(4):
                    stt(O[:, j, h * 64:(h + 1) * 64], a3[:, j, 0:64], rd[:, j:j + 1], O[:, j, h * 64:(h + 1) * 64], ALU.mult, ALU.add,
                        [("acc", h), ("rd", h), "O"], ["O"])

        def diff_mixer(l, HT, CS32):
            lam_init = 0.8 - 0.6 * math.exp(-0.3 * l)
            with ExitStack() as sa:
                DA = sbuf(sa, "DA", [128, 6, S], BF16)
                VD = sbuf(sa, "VD", [128, NT, 4, 65], BF16)
                memset("pool", VD[:], 1.0, ["VD"])
                with ExitStack() as st:
                    W = sbuf(st, "W", [128, 8, 768], BF16)
                    stg = [sbuf(st, "stg%d" % j, [128, 8, 256], F32) for j in range(2)]
                    tC = sbuf(st, "tC", [128, 512], F32)
                    tS = sbuf(st, "tS", [128, 512], F32)
                    R = sbuf(st, "R", [128, 768], BF16)
                    RT = sbuf(st, "RT", [128, 512], BF16)
                    memset("pool", R[:], 0.0, ["R"])
                    pb = [psum(st, "pb%d" % j, [128, 512], F32) for j in range(4)]
                    pT = [psum_tb(st, "pTn%d" % j)[:, 0:6, :] for j in range(2)]
                    load_w_cols(W, "W", dr["w_in"][l][:, DIF0:DIF0 + 768], 768, stg, "stg")
                    cols_for_tile(0, HT, W, 768, [pb[0], pb[1]], [("pb", 0), ("pb", 1)])
                    for i in range(NT):
                        b = [pb[(2 * i) % 4], pb[(2 * i + 1) % 4]]
                        bk = [("pb", (2 * i) % 4), ("pb", (2 * i + 1) % 4)]
                        if i + 1 < NT:
                            cols_for_tile(i + 1, HT, W, 768, [pb[(2 * i + 2) % 4], pb[(2 * i + 3) % 4]], [("pb", (2 * i + 2) % 4), ("pb", (2 * i + 3) % 4)])
                        rope(v4(RT[:, 0:512], 16, 16), v4(b[0][:, 0:512], 16, 16), 16, 16, CS32[:, i, :, :], tC, tS, [bk[0]], ["RT"])
                        for side in range(2):
                            for gb_ in range(3):
                                nu = 3 if gb_ < 2 else 2
                                cp("pool" if gb_ % 2 == 0 else "act", R[:, (3 * side + gb_) * 128:(3 * side + gb_) * 128 + 32 * nu],
                                   RT[:, side * 256 + gb_ * 96:side * 256 + gb_ * 96 + 32 * nu], ["RT"], ["R"])
                        cp("dve", VD[:, i, :, 0:64], b[1][:, 0:256].rearrange("p (g d) -> p g d", g=4), [bk[1]], ["VD"])
                        p_ = pT[i % 2]
                        for k in range(6):
                            tr(p_[:, k, :], R[:, k * 128:(k + 1) * 128], identb[:], ["R", "identb"], [("pTn", i % 2)])
                        cp("act", DA[:, :, i * 128:(i + 1) * 128], p_[:], [("pTn", i % 2)], [("da", i)])
                    P.barrier()
                DAR = [("da", i) for i in range(NT)]
                with ExitStack() as st:
                    ACC, SC, MS, PEX, wk = attn_scope(st)
                    WO = load_wout(st, l, 1)
                    O = sbuf(st, "O", [128, 4, 256], F32)
                    O2 = sbuf(st, "O2", [128, 4, 256], F32)
                    lamt = sbuf(st, "lamt", [128, 4, 32], F32)
                    sgb = sbuf(st, "sgb", [128, 64], F32)
                    sm_ = sbuf(st, "dsm", [128, 64], F32)
                    P.dma(lamt[:].rearrange("p a b -> p (a b)"), dr["lam4"][l:l + 1].rearrange("o a b -> o (a b)").to_broadcast([128, 128]), writes=["lamt"])
                    P.dma(sgb[:], dr["sub_g"][l:l + 1, :].to_broadcast([128, 64]), writes=["sgb"])
                    tt("dve", lamt[:, 0, :], lamt[:, 0, :], lamt[:, 1, :], ALU.mult, ["lamt"], ["lamt"])
                    tt("dve", lamt[:, 2, :], lamt[:, 2, :], lamt[:, 3, :], ALU.mult, ["lamt"], ["lamt"])
                    red(sm_[:, 0:1], lamt[:, 0, :], ALU.add, ["lamt"], ["dl"])
                    red(sm_[:, 1:2], lamt[:, 2, :], ALU.add, ["lamt"], ["dl"])
                    act(sm_[:, 0:2], sm_[:, 0:2], AF.Exp, ["dl"], ["dl"])
                    stt(sm_[:, 0:1], sm_[:, 1:2], -lam_init, sm_[:, 0:1], ALU.add, ALU.subtract, ["dl"], ["dl"])
                    ts("dve", sgb[:], sgb[:], 1.0 - lam_init, None, ALU.mult, None, ["sgb"], ["sgb"])
                    scale = 32 ** -0.5
                    for qc in range(4):
                        for grp in range(2):
                            units = []
                            for uu in range(4):
                                u = grp * 4 + uu
                                blk, po = u // 3, 32 * (u % 3)
                                h = u // 2
                                units.append(dict(
                                    parts=[((lambda kt, po=po, blk=blk: DA[po:po + 32, 3 + blk, kt * 128:(kt + 1) * 128]),
                                            (lambda c0, n, po=po, blk=blk: DA[po:po + 32, blk, c0:c0 + n]))],
                                    vf=(lambda kt, h=h: VD[:, kt, h, :]), R=DAR + ["VD"]))
                            flash(qc, units, causal, causal_edge, scale, ACC, SC, PEX)
                            for uu in range(4):
                                u = grp * 4 + uu
                                h, c = u // 2, u % 2
                                a3 = ACC[uu][:, 0:260].rearrange("p (j c) -> p j c", j=4)
                                rd = sm_[:, 8 + 4 * uu:12 + 4 * uu]
                                recip(rd, a3[:, :, 64], [("acc", uu)], [("rd", uu)])
                                if c == 1:
                                    ts("dve", rd, rd, sm_[:, 0:1], None, ALU.mult, None, [("rd", uu), "dl"], [("rd", uu)])
                                dst = (O if c == 0 else O2)[:, :, h * 64:(h + 1) * 64]
                                tt("dve", dst, a3[:, :, 0:64], rd.unsqueeze(2).to_broadcast([128, 4, 64]), ALU.mult,
                                   [("acc", uu), ("rd", uu)], ["O" if c == 0 else "O2"])
                        tt("pool", O[:], O[:], O2[:], ALU.add, ["O", "O2"], ["O"])
                        tt("pool", O2[:], O[:], O[:], ALU.mult, ["O"], ["O2"])
                        ss16 = sm_[:, 32:48]
                        red(ss16, O2[:].rearrange("p j (h d) -> p (j h) d", h=4), ALU.add, ["O2"], ["ss16"])
                        act(ss16, ss16, AF.Sqrt, ["ss16", "epsc"], ["ss16"], bias=epsc[:], scale=1.0 / 64)
                        recip(ss16, ss16, ["ss16"], ["ss16"])
                        O3 = O[:].rearrange("p j (h d) -> p (j h) d", h=4)
                        tt("dve", O3, O3, ss16.unsqueeze(2).to_broadcast([128, 16, 64]), ALU.mult, ["O", "ss16"], ["O"])
                        tt("dve", O3, O3, sgb[:].unsqueeze(1).to_broadcast([128, 16, 64]), ALU.mult, ["O", "sgb"], ["O"])
                        if dbg == ("o", 1):
                            for j in range(4):
                                P.dma(out_d[(4 * qc + j) * 128:(4 * qc + j + 1) * 128, 0:256], O[:, j, :], reads=["O"], is_output=True)
                        apply_wout(qc, O, "O", WO, SC, MS, wk)
                    P.barrier()

        def mla_mixer(l, HT, CS32):
            with ExitStack() as sa:
                MA = sbuf(sa, "MA", [128, 7, S], BF16)
                VM = sbuf(sa, "VM", [128, NT, 4, 65], BF16)
                memset("pool", VM[:], 1.0, ["VM"])
                with ExitStack() as st:
                    W = sbuf(st, "W", [128, 8, 416], BF16)
                    stg = [sbuf(st, "stg%d" % j, [128, 8, 256], F32) for j in range(2)]
                    tC = sbuf(st, "tC", [128, 512], F32)
                    tS = sbuf(st, "tS", [128, 512], F32)
                    R = sbuf(st, "R", [128, 896], BF16)
                    memset("pool", R[:], 0.0, ["R"])
                    WUQ = sbuf(st, "WUQ", [128, 2, 384], BF16)
                    WUKV = sbuf(st, "WUKV", [128, 512], BF16)
                    wst = sbuf(st, "wst", [128, 2, 512], F32)
                    qgb = sbuf(st, "qgb", [128, 384], F32)
                    cqn = sbuf(st, "cqn", [128, 384], BF16)
                    CT = sbuf(st, "CT", [128, 3, 128], BF16)
                    junk = sbuf(st, "junk", [128, 256], BF16)
                    kr = sbuf(st, "kr", [128, 32], BF16)
                    msm = sbuf(st, "msm", [128, 8], F32)
                    pb = [psum(st, "pb%d" % j, [128, 512], F32) for j in range(2)]
                    pu = [psum(st, "pu%d" % j, [128, 512], F32) for j in range(4)]
                    pT = [psum_tb(st, "pTn%d" % j)[:, 0:7, :] for j in range(2)]
                    load_w_cols(W, "W", dr["w_in"][l][:, MLA0:MLA0 + 416], 416, stg, "stg")
                    P.dma(wst[:, :, 0:384], dr["mla_w_uq"][l].rearrange("(k p) n -> p k n", p=128), writes=["wst"])
                    cp("act", WUQ[:], wst[:, :, 0:384], ["wst"], ["WUQ"])
                    P.dma(wst[:, 0, :], dr["mla_w_ukv"][l], writes=["wst"])
                    cp("act", WUKV[:], wst[:, 0, :], ["wst"], ["WUKV"])
                    P.dma(qgb[:, 0:256], dr["mla_qg"][l:l + 1, :].to_broadcast([128, 256]), writes=["qgb"])
                    P.dma(qgb[:, 256:384], dr["mla_kvg"][l:l + 1, :].to_broadcast([128, 128]), writes=["qgb"])
                    cols_for_tile(0, HT, W, 416, [pb[0]], [("pb", 0)])
                    for i in range(NT):
                        b = pb[i % 2]
                        bk = ("pb", i % 2)
                        if i + 1 < NT:
                            cols_for_tile(i + 1, HT, W, 416, [pb[(i + 1) % 2]], [("pb", (i + 1) % 2)])
                        act(junk[:, 0:256], b[:, 0:256], AF.Square, [bk], ["junk", "msm"], accum=msm[:, 0:1])
                        act(junk[:, 0:128], b[:, 256:384], AF.Square, [bk], ["junk", "msm"], accum=msm[:, 1:2])
                        act(msm[:, 0:1], msm[:, 0:1], AF.Sqrt, ["msm", "epsc"], ["msm"], bias=epsc[:], scale=1.0 / 256)
                        act(msm[:, 1:2], msm[:, 1:2], AF.Sqrt, ["msm", "epsc"], ["msm"], bias=epsc[:], scale=1.0 / 128)
                        recip(msm[:, 0:2], msm[:, 0:2], ["msm"], ["msm"])
                        stt(cqn[:, 0:256], b[:, 0:256], msm[:, 0:1], qgb[:, 0:256], ALU.mult, ALU.mult, [bk, "msm", "qgb"], ["cqn"])
                        stt(cqn[:, 256:384], b[:, 256:384], msm[:, 1:2], qgb[:, 256:384], ALU.mult, ALU.mult, [bk, "msm", "qgb"], ["cqn"])
                        p_ = pT[i % 2]
                        pk = ("pTn", i % 2)
                        for k in range(3):
                            tr(p_[:, k, :], cqn[:, k * 128:(k + 1) * 128], identb[:], ["cqn", "identb"], [pk])
                        cp("act", CT[:], p_[:, 0:3, :], [pk], ["CT"])
                        pq = pu[(2 * i) % 4]
                        pkv = pu[(2 * i + 1) % 4]
                        pqk, pkvk = ("pu", (2 * i) % 4), ("pu", (2 * i + 1) % 4)
                        mm(pq[:, 0:384], CT[:, 0, :], WUQ[:, 0, :], True, False, ["CT", "WUQ"], [pqk])
                        mm(pq[:, 0:384], CT[:, 1, :], WUQ[:, 1, :], False, True, ["CT", "WUQ"], [pqk])
                        mm(pkv[:, 0:512], CT[:, 2, :], WUKV[:], True, True, ["CT", "WUKV"], [pkvk])
                        q3 = pq[:, 0:384].rearrange("p (g x) -> p g x", x=96)
                        kv3 = pkv[:, 0:512].rearrange("p (g x) -> p g x", x=128)
                        cs = CS32[:, i, :, :]
                        cp("dve", R[:, 0:256].rearrange("p (g d) -> p g d", g=4), q3[:, :, 0:64], [pqk], ["R"])
                        rope(v4(R[:, 256:352], 3, 16), q3[:, 0:3, 64:96].rearrange("p g (two h) -> p g two h", two=2), 3, 16, cs, tC, tS, [pqk], ["R"])
                        rope(v4(R[:, 768:800], 1, 16), q3[:, 3:4, 64:96].rearrange("p g (two h) -> p g two h", two=2), 1, 16, cs, tC, tS, [pqk], ["R"])
                        cp("dve", R[:, 384:640].rearrange("p (g d) -> p g d", g=4), kv3[:, :, 0:64], [pkvk], ["R"])
                        rope(v4(kr[:, 0:32], 1, 16), v4(b[:, 384:416], 1, 16), 1, 16, cs, tC, tS, [bk], ["kr"])
                        cp("pool", R[:, 640:768].rearrange("p (g d) -> p g d", g=4), kr[:].unsqueeze(1).to_broadcast([128, 4, 32]), ["kr"], ["R"])
                        cp("dve", VM[:, i, :, 0:64], kv3[:, :, 64:128], [pkvk], ["VM"])
                        for k in range(7):
                            tr(p_[:, k, :], R[:, k * 128:(k + 1) * 128], identb[:], ["R", "identb"], [pk])
                        cp("act", MA[:, :, i * 128:(i + 1) * 128], p_[:], [pk], [("ma", i)])
                    P.barrier()
                MAR = [("ma", i) for i in range(NT)]
                with ExitStack() as st:
                    ACC, SC, MS, PEX, wk = attn_scope(st)
                    WO = load_wout(st, l, 2)
                    O = sbuf(st, "O", [128, 4, 256], F32)
                    sm_ = sbuf(st, "msm2", [128, 16], F32)
                    scale = 96 ** -0.5
                    for qc in range(4):
                        units = []
                        for h in range(4):
                            hp, blk = 64 * (h % 2), h // 2
                            units.append(dict(
                                parts=[((lambda kt, hp=hp, blk=blk: MA[hp:hp + 64, 3 + blk, kt * 128:(kt + 1) * 128]),
                                        (lambda c0, n, hp=hp, blk=blk: MA[hp:hp + 64, blk, c0:c0 + n])),
                                       ((lambda kt, h=h: MA[32 * (h % 3):32 * (h % 3) + 32, 5, kt * 128:(kt + 1) * 128]),
                                        (lambda c0, n, h=h: MA[32 * (h % 3):32 * (h % 3) + 32, 2 if h < 3 else 6, c0:c0 + n]))],
                                vf=(lambda kt, h=h: VM[:, kt, h, :]), R=MAR + ["VM"]))
                        flash(qc, units, causal, causal_edge, scale, ACC, SC, PEX)
                        for h in range(4):
                            a3 = ACC[h][:, 0:260].rearrange("p (j c) -> p j c", j=4)
                            rd = sm_[:, 4 * h:4 * h + 4]
                            recip(rd, a3[:, :, 64], [("acc", h)], [("rd", h)])
                            tt("dve", O[:, :, h * 64:(h + 1) * 64], a3[:, :, 0:64], rd.unsqueeze(2).to_broadcast([128, 4, 64]), ALU.mult,
                               [("acc", h), ("rd", h)], ["O"])
                        if dbg == ("o", 2):
                            for j in range(4):
                                P.dma(out_d[(4 * qc + j) * 128:(4 * qc + j + 1) * 128, 0:256], O[:, j, :], reads=["O"], is_output=True)
                        apply_wout(qc, O, "O", WO, SC, MS, wk)
                    P.barrier()

        def swa_mixer(l, HT, CS64):
            with ExitStack() as sa:
                SA = sbuf(sa, "SA", [128, 4, S], BF16)
                VW = sbuf(sa, "VW", [128, NT, 2, 65], BF16)
                memset("pool", VW[:], 1.0, ["VW"])
                with ExitStack() as st:
                    W = sbuf(st, "W", [128, 8, 512], BF16)
                    stg = [sbuf(st, "stg%d" % j, [128, 8, 256], F32) for j in range(2)]
                    tC = sbuf(st, "tC", [128, 512], F32)
                    tS = sbuf(st, "tS", [128, 512], F32)
                    R = sbuf(st, "R", [128, 512], BF16)
                    pb = [psum(st, "pb%d" % j, [128, 512], F32) for j in range(2)]
                    pT = [psum_tb(st, "pTn%d" % j)[:, 0:4, :] for j in range(2)]
                    load_w_cols(W, "W", dr["w_in"][l][:, SWA0:SWA0 + 512], 512, stg, "stg")
                    cols_for_tile(0, HT, W, 512, [pb[0]], [("pb", 0)])
                    for i in range(NT):
                        b = pb[i % 2]
                        bk = ("pb", i % 2)
                        if i + 1 < NT:
                            cols_for_tile(i + 1, HT, W, 512, [pb[(i + 1) % 2]], [("pb", (i + 1) % 2)])
                        cs = CS64[:, i, :, :]
                        rope(v4(R[:, 0:256], 4, 32), v4(b[:, 0:256], 4, 32), 4, 32, cs, tC, tS, [bk], ["R"])
                        Rk = R[:, 256:512].rearrange("p (g r d) -> p g r d", r=2, d=64)
                        rope(Rk[:, :, 0, :].rearrange("p g (two h) -> p g two h", two=2), v4(b[:, 256:384], 2, 32), 2, 32, cs, tC, tS, [bk], ["R"])
                        cp("pool", Rk[:, :, 1, :], Rk[:, :, 0, :], ["R"], ["R"])
                        cp("dve", VW[:, i, :, 0:64], b[:, 384:512].rearrange("p (g d) -> p g d", g=2), [bk], ["VW"])
                        p_ = pT[i % 2]
                        for k in range(4):
                            tr(p_[:, k, :], R[:, k * 128:(k + 1) * 128], identb[:], ["R", "identb"], [("pTn", i % 2)])
                        cp("act", SA[:, :, i * 128:(i + 1) * 128], p_[:], [("pTn", i % 2)], [("sa", i)])
                    P.barrier()
                SAR = [("sa", i) for i in range(NT)]
                with ExitStack() as st:
                    ACC, SC, MS, PEX, wk = attn_scope(st)
                    WO = load_wout(st, l, 3)
                    O = sbuf(st, "O", [128, 4, 256], F32)
                    sm_ = sbuf(st, "ssm", [128, 16], F32)
                    esk = sbuf(st, "esk", [128, 4], F32)
                    P.dma(esk[:], dr["swa_sinks"][l:l + 1, :].to_broadcast([128, 4]), writes=["esk"])
                    act(esk[:], esk[:], AF.Exp, ["esk"], ["esk"])
                    scale = 64 ** -0.5
                    kr, ed = window(1)
                    for qc in range(4):
                        units = []
                        for h in range(4):
                            hp, blk = 64 * (h % 2), h // 2
                            units.append(dict(
                                parts=[((lambda kt, hp=hp, blk=blk: SA[hp:hp + 64, 2 + blk, kt * 128:(kt + 1) * 128]),
                                        (lambda c0, n, hp=hp, blk=blk: SA[hp:hp + 64, blk, c0:c0 + n]))],
                                vf=(lambda kt, g=h // 2: VW[:, kt, g, :]), R=SAR + ["VW"]))
                        flash(qc, units, kr, ed, scale, ACC, SC, PEX)
                        for h in range(4):
                            a3 = ACC[h][:, 0:260].rearrange("p (j c) -> p j c", j=4)
                            rd = sm_[:, 4 * h:4 * h + 4]
                            ts("dve", rd, a3[:, :, 64], esk[:, h:h + 1], None, ALU.add, None, [("acc", h), "esk"], [("rd", h)])
                            recip(rd, rd, [("rd", h)], [("rd", h)])
                            tt("dve", O[:, :, h * 64:(h + 1) * 64], a3[:, :, 0:64], rd.unsqueeze(2).to_broadcast([128, 4, 64]), ALU.mult,
                               [("acc", h), ("rd", h)], ["O"])
                        if dbg == ("o", 3):
                            for j in range(4):
                                P.dma(out_d[(4 * qc + j) * 128:(4 * qc + j + 1) * 128, 0:256], O[:, j, :], reads=["O"], is_output=True)
                        apply_wout(qc, O, "O", WO, SC, MS, wk)
                    P.barrier()

        def ffn_phase(l):
            load_mod(l, 1)
            with ExitStack() as sf:
                ST = sbuf(sf, "ST", [128, 4, 8, 4, 128], BF16)
                LNR = sbuf(sf, "LNR", [128, 4, 8], F32)
                H2T = sbuf(sf, "H2T", [128, 8, 512], BF16)
                SKs = sbuf(sf, "SKs", [128, 2, 128], F32)
                SK = sbuf(sf, "SK", [128, 2, 128], BF16)
                P.dma(SKs[:], dr["skT"][l].rearrange("c d k -> d c k"), writes=["SKs"])
                cp("dve", SK[:], SKs[:], ["SKs"], ["SK"])
                for g in range(4):
                    with ExitStack() as st:
                        junk = sbuf(st, "junk", [128, D], BF16)
                        t1 = sbuf(st, "t1", [128, D], F32)
                        hb = sbuf(st, "hb", [128, D], BF16)
                        pT = psum_tb(st, "pT")
                        QP = sbuf(st, "QP", [128, 16, 512], BF16)
                        wqs = [sbuf(st, "wqs%d" % j, [128, 8, 128], F32) for j in range(2)]
                        wqb = [sbuf(st, "wqb%d" % j, [128, 8, 128], BF16) for j in range(2)]
                        SSs = sbuf(st, "SSs", [128, 16, 128], F32)
                        pq = [psum(st, "pq%d" % j, [128, 512], F32) for j in range(2)]
                        pss = [psum(st, "pss%d" % j, [128, 512], F32) for j in range(4)]
                        m1 = sbuf(st, "m1", [128, 16], F32)
                        m2 = sbuf(st, "m2", [128, 16], F32)
                        mc = sbuf(st, "mc", [128, 16], F32)
                        rr = sbuf(st, "rr", [128, 256], F32)
                        cand = sbuf(st, "cand", [128, 256], F32)
                        sm_ = sbuf(st, "psm", [128, 32], F32)
                        am = sbuf(st, "am", [128, 128], F32)
                        sfl = sbuf(st, "sfl", [128, 2, 128], F32)
                        shl = sbuf(st, "shl", [128, 4, 128], BF16)
                        pst_ = psum_tb(st, "pst")
                        for tt_ in range(4):
                            norm_to_T(4 * g + tt_, H2T, tt_ * 128, ("h2t", tt_), (junk, t1, hb, pT))
                        H2R = [("h2t", j) for j in range(4)]
                        for n in range(16):
                            s_ = wqs[n % 2]
                            P.dma(s_[:], dr["peer_w_q"][l][:, n * 128:(n + 1) * 128].rearrange("(k p) n -> p k n", p=128), writes=[("wqs", n % 2)])
                            cp("act", wqb[n % 2][:], s_[:], [("wqs", n % 2)], [("wqb", n % 2)])
                            for k in range(8):
                                mm(pq[n % 2][:], wqb[n % 2][:, k, :], H2T[:, k, :], k == 0, k == 7, [("wqb", n % 2)] + H2R, [("pq", n % 2)])
                            cp("act", QP[:, n, :], pq[n % 2][:], [("pq", n % 2)], [("qp", n)])
                        for tt_ in range(4):
                            for n in range(16):
                                b = pss[n // 4]
                                mm(b[:, (n % 4) * 128:(n % 4 + 1) * 128], QP[:, n, tt_ * 128:(tt_ + 1) * 128], SK[:, n % 2, :], True, True,
                                   [("qp", n), "SK"], [("pss", n // 4)])
                            for q4 in range(4):
                                cp("act", SSs[:, 4 * q4:4 * q4 + 4, :], pss[q4][:].rearrange("p (a b) -> p a b", a=4), [("pss", q4)], [("sss", q4)])
                            for h in range(8):
                                s1 = SSs[:, 2 * h, :]
                                s2 = SSs[:, 2 * h + 1, :]
                                sk_ = [("sss", h // 2)]
                                for (sx, mx) in ((s1, m1), (s2, m2)):
                                    P.op("dve", lambda e, sx=sx, mx=mx: e.max(mx[:, 0:8], sx), reads=sk_, writes=["mx"])
                                    P.op("dve", lambda e, sx=sx, mx=mx: e.match_replace(rr[:, 0:128], mx[:, 0:8], sx, -1.0e30), reads=sk_ + ["mx"], writes=["rr"])
                                    P.op("dve", lambda e, mx=mx: e.max(mx[:, 8:16], rr[:, 0:128]), reads=["rr"], writes=["mx"])
                                c3 = cand[:].rearrange("p (a b) -> p a b", a=16)
                                tt("dve", c3, m1[:].unsqueeze(2).to_broadcast([128, 16, 16]), m2[:].unsqueeze(1).to_broadcast([128, 16, 16]), ALU.add,
                                   ["mx"], ["cand"])
                                P.op("dve", lambda e: e.max(mc[:, 0:8], cand[:]), reads=["cand"], writes=["mc"])
                                P.op("dve", lambda e: e.match_replace(rr[:], mc[:, 0:8], cand[:], -1.0e30), reads=["cand", "mc"], writes=["rr"])
                                P.op("dve", lambda e: e.max(mc[:, 8:16], rr[:]), reads=["rr"], writes=["mc"])
                                negM = sm_[:, 0:1]
                                Z = sm_[:, 1:2]
                                nthr = sm_[:, 2:3]
                                ts("dve", negM, mc[:, 0:1], -1.0, None, ALU.mult, None, ["mc"], ["psm"])
                                act(sm_[:, 8:24], mc[:], AF.Exp, ["mc", "psm"], ["psm2", "psmZ"], bias=negM, accum=Z)
                                act(Z, Z, AF.Ln, ["psmZ"], ["psmZ"])
                                ts("dve", nthr, mc[:, 15:16], -1.0, 1.0e-3, ALU.mult, ALU.add, ["mc"], ["psm"])
                                tt("dve", Z, negM, Z, ALU.subtract, ["psm", "psmZ"], ["psmZ"])
                                tt("dve", LNR[:, tt_, h:h + 1], Z, nthr, ALU.subtract, ["psm", "psmZ"], [("lnr", tt_)])
                                ts("dve", am[:], s1, m1[:, 15:16], -1.0, ALU.is_ge, ALU.add, sk_ + ["mx"], ["am"])
                                stt(am[:], am[:], BIG, s1, ALU.mult, ALU.add, ["am"] + sk_, ["am"])
                                ts("dve", sfl[:, 0, :], am[:], nthr, None, ALU.add, None, ["am", "psm"], ["sfl"])
                                ts("dve", am[:], s2, m2[:, 15:16], -1.0, ALU.is_ge, ALU.add, sk_ + ["mx"], ["am"])
                                stt(sfl[:, 1, :], am[:], BIG, s2, ALU.mult, ALU.add, ["am"] + sk_, ["sfl"])
                                shl6 = shl[:].rearrange("p (c hf) (q j) -> p c hf q j", hf=2, q=2)
                                sfl4 = sfl[:].rearrange("p c (hf j) -> p c hf j", hf=2)
                                for c_ in range(2):
                                    cp("pool", shl6[:, c_, :, 0, :], sfl4[:, c_, :, :], ["sfl"], ["shl"])
                                    tt("pool", shl6[:, c_, :, 1, :], sfl4[:, c_, :, :], shl6[:, c_, :, 0, :], ALU.subtract, ["sfl", "shl"], ["shl"])
                                for q_ in range(4):
                                    tr(pst_[:, q_, :], shl[:, q_, :], identb[:], ["shl", "identb"], ["pst"])
                                cp("act", ST[:, tt_, h, :, :], pst_[:, 0:4, :], ["pst"], [("st", tt_)])
                        P.barrier()
                    with ExitStack() as st:
                        Us = sbuf(st, "Us", [128, 8, 256], F32)
                        Vs = sbuf(st, "Vs", [128, 2, D], F32)
                        UB = [sbuf(st, "UB%d" % j, [128, 8, 512], BF16) for j in range(2)]
                        VB = [sbuf(st, "VB%d" % j, [128, 4, D], BF16) for j in range(2)]
                        NR = 6
                        Rb = [sbuf(st, "Rb%d" % j, [128, 512], BF16) for j in range(NR)]
                        Wb = [sbuf(st, "Wb%d" % j, [128, 512], BF16) for j in range(NR)]
                        GtT = sbuf(st, "GtT", [128, 8, 512], BF16)
                        Gt = [GtT[:, j, :] for j in range(8)]
                        WAT = sbuf(st, "WAT", [128, 4, 512], BF16)
                        wT = [psum(st, "wT%d" % j, [128, 512], F32) for j in range(2)]
                        MC = [psum(st, "MC%d" % j, [128, 512], F32) for j in range(4)]
                        PP = [psum(st, "PP%d" % j, [128, 512], F32) for j in range(2)]
                        H2R = [("h2t", j) for j in range(4)]
                        ppc = [0]

                        def next_pp():
                            j = ppc[0] % 2
                            ppc[0] += 1
                            return PP[j], ("pp", j)

                        def load_steps(ck):
                            e0 = ck * 512
                            ub, vbf = UB[ck % 2], VB[ck % 2]
                            uk, vk = ("ub", ck % 2), ("vb", ck % 2)

                            def dma_h(hh):
                                P.dma(Us[:], dr["uT"][l][:, e0 + hh * 256:e0 + (hh + 1) * 256].rearrange("(k p) e -> p k e", p=128), writes=["Us"])
                                P.dma(Vs[:], dr["pv"][l][e0 + hh * 256:e0 + (hh + 1) * 256, :].rearrange("(s p) d -> p s d", p=128), writes=["Vs"])

                            def cast_h(hh):
                                cp("act", ub[:, :, hh * 256:(hh + 1) * 256], Us[:], ["Us"], [uk])
                                tt("pool", vbf[:, 2 * hh:2 * hh + 2, :], Vs[:], MOD[:, 2, :].unsqueeze(1).to_broadcast([128, 2, D]), ALU.mult, ["Vs", "mod"], [vk])
                            return [lambda: dma_h(0), lambda: (cast_h(0), dma_h(1)), lambda: cast_h(1)]

                        def front(ck, subs=(0, 1, 2, 3)):
                            ub, uk = UB[ck % 2], ("ub", ck % 2)
                            for s_ in subs:
                                p_, pk_ = next_pp()
                                for k in range(8):
                                    mm(p_[:], ub[:, k, s_ * 128:(s_ + 1) * 128], H2T[:, k, :], k == 0, k == 7, H2R + [uk], [pk_])
                                gj = (ck % 2) * 4 + s_
                                act(Gt[gj], p_[:], GELU, [pk_], [("gt", gj)])

                        def tail_v(ck):
                            vbf, vk = VB[ck % 2], ("vb", ck % 2)
                            outl = []
                            for tt_ in range(4):
                                for hf in range(2):
                                    def vgrp(tt_=tt_, hf=hf, vbf=vbf, vk=vk):
                                        i = 4 * g + tt_
                                        o_, ok = next_pp()
                                        for s_ in range(4):
                                            mm(o_[:], WAT[:, s_, tt_ * 128:(tt_ + 1) * 128], vbf[:, s_, hf * 512:(hf + 1) * 512], s_ == 0, s_ == 3, [("wat", tt_), vk], [ok])
                                        tt("dve", X[:, i, hf * 512:(hf + 1) * 512], X[:, i, hf * 512:(hf + 1) * 512], o_[:], ALU.add, [("x", i), ok], [("x", i)])
                                    outl.append(vgrp)
                            return outl

                        cn = 0
                        for f_ in load_steps(0):
                            f_()
                        front(0)
                        deferred = []
                        sel2 = identb2[:, :].unsqueeze(1).to_broadcast([128, 4, 64])
                        wtc = 0
                        pendq = []
                        midq = []
                        watq = [None]
                        for ck in range(32):
                            un_i = 0
                            half_, ckk = ck // 16, ck % 16
                            sel1 = identb2[:, 4 * ckk:4 * ckk + 4].unsqueeze(2).to_broadcast([128, 4, 128])
                            for tt_ in range(4):
                                wtb = wT[wtc % 2]
                                wtk = ("wt", wtc % 2)
                                wtc += 1
                                wt3 = wtb[:].rearrange("p (s t) -> p s t", s=4)
                                for h in range(8):
                                    kk = cn % NR
                                    rb, wb = Rb[kk], Wb[kk]
                                    mcb = MC[cn % 4]
                                    mk_ = ("mc", cn % 4)
                                    mc3 = mcb[:].rearrange("p (a b) -> p a b", a=4)
                                    stk = [("st", tt_), "identb2"]
                                    mm(mc3, ST[:, tt_, h, half_, :], sel1, True, False, stk, [mk_])
                                    mm(mc3[:, :, 0:64], ST[:, tt_, h, 2, :], sel2, False, False, stk, [mk_])
                                    mm(mc3[:, :, 64:128], ST[:, tt_, h, 3, :], sel2, False, False, stk, [mk_], sig=True)
                                    ts("dve", rb[:], mcb[:], 1.0e6, 0.0, ALU.mult, ALU.min, [mk_], [("rb", kk)])
                                    cn += 1

                                    def fin_unit(kk=kk, h=h, wb=wb, wt3=wt3, wtk=wtk):
                                        for s_ in range(4):
                                            mm(wt3[:, s_, :], wb[:, s_ * 128:(s_ + 1) * 128], identb[:],
                                               (h == 0 and s_ == 0), (h == 7 and s_ == 3), [("wb", kk), "identb"], [wtk], sig=(s_ == 3))

                                    def mid_unit(kk=kk, tt_=tt_, h=h, rb=rb, wb=wb, mcb=mcb, mk_=mk_, fin_unit=fin_unit):
                                        mm(mcb[:], identb[:], rb[:], False, True, ["identb", ("rb", kk)], [mk_])
                                        act(wb[:], mcb[:], AF.Exp, [mk_, ("lnr", tt_)], [("wb", kk)], bias=LNR[:, tt_, h:h + 1])
                                        pendq.append(fin_unit)
                                        if len(pendq) > 2:
                                            pendq.pop(0)()
                                    midq.append(mid_unit)
                                    if len(midq) > 1:
                                        midq.pop(0)()
                                    if h == 3 and watq[0] is not None:
                                        watq[0]()
                                        watq[0] = None
                                    if deferred:
                                        deferred.pop(0)()
                                    un_i += 1
                                    if ck + 1 < 32:
                                        if un_i == 9:
                                            while deferred:
                                                deferred.pop(0)()
                                            lsteps = load_steps(ck + 1)
                                            lsteps[0]()
                                        elif un_i == 16:
                                            lsteps[1]()
                                        elif un_i == 24:
                                            lsteps[2]()
                                        elif un_i == 27:
                                            front(ck + 1, (0, 1))
                                        elif un_i == 31:
                                            front(ck + 1, (2, 3))
                                    if h == 7:
                                        def wat_fn(tt_=tt_, gts=(ck % 2) * 4, wt3=wt3, wtk=wtk):
                                            tt("dve", WAT[:, :, tt_ * 128:(tt_ + 1) * 128], GtT[:, gts:gts + 4, tt_ * 128:(tt_ + 1) * 128], wt3, ALU.mult,
                                               [("gt", gts + s_) for s_ in range(4)] + [wtk], [("wat", tt_)])
                                        watq[0] = wat_fn
                            while deferred:
                                deferred.pop(0)()
                            deferred = tail_v(ck)
                        while midq:
                            midq.pop(0)()
                        while pendq:
                            pendq.pop(0)()
                        watq[0]()
                        while deferred:
                            deferred.pop(0)()
                        P.barrier()

        for l in layers:
            mixer_phase(l)
            if xmid_d is not None:
                for i in range(NT):
                    P.dma(xmid_d[i * 128:(i + 1) * 128, :], X[:, i, :], reads=[("x", i)], is_output=True)
            if do_ffn:
                ffn_phase(l)

        with ExitStack() as st:
            fgb = sbuf(st, "fgb", [128, D], F32)
            junk = sbuf(st, "junk", [128, D], BF16)
            ob = [sbuf(st, "ob%d" % j, [128, D], F32) for j in range(2)]
            P.dma(fgb[:], dr["final_g"].to_broadcast([128, D]), writes=["fgb"])
            if dbg is None or dbg == "final":
                for i in range(NT):
                    xi = X[:, i, :]
                    act(junk[:], xi, AF.Square, [("x", i)], ["junk", ("ssq", i)], accum=SSQ[:, i:i + 1])
                    act(RSTD[:, i:i + 1], SSQ[:, i:i + 1], AF.Sqrt, [("ssq", i), "epsc"], [("rstd", i)], bias=epsc[:], scale=1.0 / D)
                    recip(RSTD[:, i:i + 1], RSTD[:, i:i + 1], [("rstd", i)], [("rstd", i)])
                    stt(ob[i % 2][:], xi, RSTD[:, i:i + 1], fgb[:], ALU.mult, ALU.mult, [("x", i), ("rstd", i), "fgb"], [("ob", i % 2)])
                    P.dma(out_d[i * 128:(i + 1) * 128, :], ob[i % 2][:], reads=[("ob", i % 2)], is_output=True)
            elif dbg == "x":
                for i in range(NT):
                    P.dma(out_d[i * 128:(i + 1) * 128, :], X[:, i, :], reads=[("x", i)], is_output=True)
            P.barrier()
        P.finish()
        print("instructions:", P.n_inst, {k: len(v) for k, v in P.q.items()}, "dma sems:", len(P.dma_sem), flush=True)
    return nc


def _consts():
    c = {}
    c["ident"] = np.eye(128, dtype=np.float32)
    k = np.arange(128)[:, None]
    q = np.arange(128)[None, :]
    c["tri"] = np.stack([(k <= q), (k > q)], axis=1).astype(np.float32)

    def rt(dim):
        inv = (1.0 / (np.float32(10000.0) ** (np.arange(0, dim, 2, dtype=np.float32) / np.float32(dim)))).astype(np.float32)
        ang = (np.arange(S, dtype=np.float32)[:, None] * inv[None, :]).astype(np.float32)
        return np.stack([np.cos(ang), np.sin(ang)], axis=1).astype(np.float32)
    c["cs64"] = rt(64)
    c["cs32"] = rt(32)
    cidx_last = np.arange(127) * 16 + 31
    cm = np.zeros((128, S), np.float32)
    cm[:127] = (cidx_last[:, None] <= np.arange(S)[None, :])
    c["cmaskT"] = cm
    t = np.arange(S)
    qblk = t // 64
    jj = np.arange(32)
    allowed = jj[None, :] <= qblk[:, None]
    forced = (jj[None, :] == 0) | (jj[None, :] == qblk[:, None]) | (jj[None, :] == qblk[:, None] - 1)
    M1 = (allowed & ~forced).astype(np.float32)
    M2 = np.where(allowed, np.where(forced, 1e4, 0.0), -1e30).astype(np.float32)
    c["selM"] = np.stack([M1, M2, allowed.astype(np.float32)], axis=1)
    ex = np.zeros((32, S), np.float32)
    ex[np.arange(S) // 64, np.arange(S)] = 1.0
    c["expand"] = ex
    cidx = np.arange(127)[:, None] * 16 + np.arange(32)[None, :]
    ov = np.zeros((128, 33), np.float32)
    ov[:127, 0] = 1.0
    for cc in range(127):
        for p in cidx[cc]:
            ov[cc, 1 + p // 64] += 1.0 / 32.0
    c["ovl1"] = ov
    return c


def _prep_shared(inp):
    f = lambda a: np.ascontiguousarray(np.asarray(a, dtype=np.float32))
    w_in = f(inp["w_in"])
    perm = np.concatenate([np.arange(0, 256), np.arange(256, 320), np.arange(384, 448), np.arange(512, 576),
                           np.arange(320, 384), np.arange(448, 512), np.arange(576, 640), np.arange(640, 652),
                           np.arange(652, 2348)])
    sh = {
        "ada_w": f(inp["ada_w"]), "ada_b": f(inp["ada_b"]),
        "gmix": f(inp["norm_mix_g"]), "gffn": f(inp["norm_ffn_g"]), "final_g": f(inp["final_g"]).reshape(1, D),
        "w_in": f(w_in[:, :, perm]), "w_out": f(inp["w_out"]),
        "wkD": f(np.concatenate([inp["nsa_cmp_wk"], inp["nsa_cmp_wk"]], axis=-1)), "wv": f(inp["nsa_cmp_wv"]),
        "poskT": f(np.transpose(inp["nsa_cmp_pos_k"], (0, 2, 1))), "posvT": f(np.transpose(inp["nsa_cmp_pos_v"], (0, 2, 1))),
        "lam4": f(np.stack([inp["diff_lam_q1"], inp["diff_lam_k1"], inp["diff_lam_q2"], inp["diff_lam_k2"]], axis=1)),
        "sub_g": f(inp["diff_sub_g"]),
        "mla_qg": f(inp["mla_q_norm_g"]), "mla_w_uq": f(inp["mla_w_uq"]), "mla_kvg": f(inp["mla_kv_norm_g"]), "mla_w_ukv": f(inp["mla_w_ukv"]),
        "swa_sinks": f(inp["swa_sinks"]),
        "peer_w_q": f(inp["peer_w_q"]),
        "skT": f(np.stack([np.transpose(inp["peer_sub_k1"], (0, 2, 1)), np.transpose(inp["peer_sub_k2"], (0, 2, 1))], axis=1)),
        "uT": f(np.transpose(inp["peer_u"], (0, 2, 1))), "pv": f(inp["peer_v"]),
    }
    sh.update(_consts())
    return sh


def make_in_maps(inp):
    sh = _prep_shared(inp)
    x = np.asarray(inp["x"], dtype=np.float32)
    c = np.asarray(inp["c"], dtype=np.float32)
    maps = []
    for b in range(8):
        m = dict(sh)
        m["x"] = np.ascontiguousarray(x[b])
        m["cT"] = np.ascontiguousarray(c[b].reshape(8, 128).T)
        maps.append(m)
    return maps


_NC_CACHE = {}


def kernel(**inputs):
    if "nc" not in _NC_CACHE:
        _NC_CACHE["nc"] = build()
    nc = _NC_CACHE["nc"]
    in_maps = make_in_maps(inputs)
    res = run_bass_kernel_spmd(nc, in_maps, core_ids=list(range(8)))
    return np.stack([np.asarray(r["out"], dtype=np.float32) for r in res.results], axis=0)
```
